# Optimizing a Trainium2 kernel written in Bass

```python
import math
import jax, jax.numpy as jnp
from jax import lax
import numpy as np

D_MODEL = 1024
BATCH = 4
SEQ = 8192
DEPTH = 1

CONV_CH = 768
CONV_K = 3
SSM_CH = 256
SSM_GROUP = 16
SSM_GROUPS = SSM_CH // SSM_GROUP
SSM_STATE = 64
DT_MIN = 1e-3
DT_MAX = 1e-1
N_BRANCH = 2
IN_COLS = 3 * CONV_CH + SSM_CH + N_BRANCH * D_MODEL
PEER_HEADS = 8
PEER_KEYS = 128
PEER_N = PEER_KEYS * PEER_KEYS
PEER_QDIM = 256
PEER_HALF = PEER_QDIM // 2
PEER_TOPK = 16
PEER_CHUNK = 128
RMS_EPS = 1e-6

kernel_name = "hybrid_conv_s5_peer_block"


def rmsnorm(x, g):
    xf = x.astype(jnp.float32)
    y = xf * lax.rsqrt(jnp.mean(xf * xf, axis=-1, keepdims=True) + RMS_EPS)
    return (y * g.astype(jnp.float32)).astype(x.dtype)


def short_conv_mixer(b, c, h, conv_w, conv_b):
    v = c * h
    s = v.shape[1]
    vp = jnp.pad(v, ((0, 0), (CONV_K - 1, 0), (0, 0)))
    y = conv_b
    for k in range(CONV_K):
        y = y + conv_w[k] * vp[:, k:k + s]
    return b * y


def _complex_scan_op(e1, e2):
    a1r, a1i, b1r, b1i = e1
    a2r, a2i, b2r, b2i = e2
    ar = a2r * a1r - a2i * a1i
    ai = a2r * a1i + a2i * a1r
    br = a2r * b1r - a2i * b1i + b2r
    bi = a2r * b1i + a2i * b1r + b2i
    return (ar, ai, br, bi)


def s5_mixer(u, lam_re, lam_im, log_dt, b_re, b_im, c_re, c_im, d_skip, w_glu, b_glu):
    f32 = jnp.float32
    bsz, s, _ = u.shape
    uf = u.astype(f32).reshape(bsz, s, SSM_GROUPS, SSM_GROUP)
    lr = lam_re.astype(f32)
    li = lam_im.astype(f32)
    dt = jnp.exp(log_dt.astype(f32))[:, None]
    mag = jnp.exp(lr * dt)
    ang = li * dt
    a_re = mag * jnp.cos(ang)
    a_im = mag * jnp.sin(ang)
    den = lr * lr + li * li
    nr = a_re - 1.0
    ni = a_im
    k_re = (nr * lr + ni * li) / den
    k_im = (ni * lr - nr * li) / den
    bu_re = jnp.einsum('bsgh,gph->bsgp', uf, b_re.astype(f32))
    bu_im = jnp.einsum('bsgh,gph->bsgp', uf, b_im.astype(f32))
    x_re = k_re * bu_re - k_im * bu_im
    x_im = k_re * bu_im + k_im * bu_re
    A_re = jnp.broadcast_to(a_re, x_re.shape)
    A_im = jnp.broadcast_to(a_im, x_im.shape)
    _, _, s_re, s_im = lax.associative_scan(_complex_scan_op, (A_re, A_im, x_re, x_im), axis=1)
    y = (jnp.einsum('bsgp,ghp->bsgh', s_re, c_re.astype(f32))
         - jnp.einsum('bsgp,ghp->bsgh', s_im, c_im.astype(f32))
         + d_skip.astype(f32) * uf)
    y = y.reshape(bsz, s, SSM_CH)
    z = jax.nn.gelu(y, approximate=False)
    out = z * jax.nn.sigmoid(z @ w_glu.astype(f32) + b_glu.astype(f32))
    return out.astype(u.dtype)


def peer_ffn(x, w_q, sub_k1, sub_k2, expert_u, expert_v):
    f32 = jnp.float32
    bsz, s, d = x.shape
    q = (x @ w_q).astype(f32).reshape(bsz, s, PEER_HEADS, 2, PEER_HALF)
    s1 = jnp.einsum('bshd,nd->bshn', q[..., 0, :], sub_k1.astype(f32))
    s2 = jnp.einsum('bshd,nd->bshn', q[..., 1, :], sub_k2.astype(f32))
    v1, i1 = lax.top_k(s1, PEER_TOPK)
    v2, i2 = lax.top_k(s2, PEER_TOPK)
    cand = (v1[..., :, None] + v2[..., None, :]).reshape(bsz, s, PEER_HEADS, PEER_TOPK * PEER_TOPK)
    cidx = (i1[..., :, None] * PEER_KEYS + i2[..., None, :]).reshape(bsz, s, PEER_HEADS, PEER_TOPK * PEER_TOPK)
    best, pos = lax.top_k(cand, PEER_TOPK)
    eidx = jnp.take_along_axis(cidx, pos, axis=-1)
    gate = jax.nn.softmax(best, axis=-1)
    n_chunks = (bsz * s) // PEER_CHUNK
    hk = PEER_HEADS * PEER_TOPK
    xs = x.reshape(n_chunks, PEER_CHUNK, d)
    es = eidx.reshape(n_chunks, PEER_CHUNK, hk)
    gs = gate.reshape(n_chunks, PEER_CHUNK, hk).astype(x.dtype)

    def block(args):
        xc, ec, gc = args
        u = jnp.take(expert_u, ec, axis=0)
        v = jnp.take(expert_v, ec, axis=0)
        h = jax.nn.gelu(jnp.einsum('cd,ckd->ck', xc, u), approximate=False)
        return jnp.einsum('ck,ckd->cd', gc * h, v)

    out = lax.map(block, (xs, es, gs))
    return out.reshape(bsz, s, d)


def setup_inputs(seed: int = 0) -> dict:
    key = jax.random.key(seed)
    ks = jax.random.split(key, 32)
    f32 = jnp.float32
    L = DEPTH
    D = D_MODEL

    def nrm(k, shape, scale):
        return jax.random.normal(k, shape, f32) * scale

    n_idx = jnp.arange(SSM_STATE, dtype=f32)
    lam_re = -0.5 + nrm(ks[6], (L, SSM_GROUPS, SSM_STATE), 0.01)
    lam_im = math.pi * n_idx[None, None, :] + nrm(ks[7], (L, SSM_GROUPS, SSM_STATE), 0.01)
    log_dt = jax.random.uniform(ks[8], (L, SSM_GROUPS), f32, math.log(DT_MIN), math.log(DT_MAX))
    return {
        "x": nrm(ks[0], (BATCH, SEQ, D), 1.0),
        "g_mix": 1.0 + nrm(ks[1], (L, D), 0.01),
        "w_in": nrm(ks[2], (L, D, IN_COLS), D ** -0.5),
        "b_gate": nrm(ks[3], (L, N_BRANCH * D), 0.01),
        "conv_w": nrm(ks[4], (L, CONV_K, CONV_CH), CONV_K ** -0.5),
        "conv_b": nrm(ks[5], (L, CONV_CH), 0.01),
        "lam_re": lam_re,
        "lam_im": lam_im,
        "log_dt": log_dt,
        "ssm_b_re": nrm(ks[9], (L, SSM_GROUPS, SSM_STATE, SSM_GROUP), (2 * SSM_GROUP) ** -0.5),
        "ssm_b_im": nrm(ks[10], (L, SSM_GROUPS, SSM_STATE, SSM_GROUP), (2 * SSM_GROUP) ** -0.5),
        "ssm_c_re": nrm(ks[11], (L, SSM_GROUPS, SSM_GROUP, SSM_STATE), SSM_STATE ** -0.5),
        "ssm_c_im": nrm(ks[12], (L, SSM_GROUPS, SSM_GROUP, SSM_STATE), SSM_STATE ** -0.5),
        "ssm_d": nrm(ks[13], (L, SSM_GROUPS, SSM_GROUP), 1.0),
        "w_glu": nrm(ks[14], (L, SSM_CH, SSM_CH), SSM_CH ** -0.5),
        "b_glu": nrm(ks[15], (L, SSM_CH), 0.01),
        "w_branch_a": nrm(ks[16], (L, CONV_CH, D), CONV_CH ** -0.5),
        "w_branch_b": nrm(ks[17], (L, SSM_CH, D), SSM_CH ** -0.5),
        "w_out": nrm(ks[18], (L, D, D), D ** -0.5),
        "g_ffn": 1.0 + nrm(ks[19], (L, D), 0.01),
        "w_q": nrm(ks[20], (L, D, PEER_HEADS * PEER_QDIM), D ** -0.5),
        "sub_keys_1": nrm(ks[21], (L, PEER_KEYS, PEER_HALF), PEER_HALF ** -0.5),
        "sub_keys_2": nrm(ks[22], (L, PEER_KEYS, PEER_HALF), PEER_HALF ** -0.5),
        "expert_u": nrm(ks[23], (L, PEER_N, D), D ** -0.5),
        "expert_v": nrm(ks[24], (L, PEER_N, D), PEER_HEADS ** -0.5),
        "g_final": 1.0 + nrm(ks[25], (D,), 0.01),
    }


def reference(x, g_mix, w_in, b_gate, conv_w, conv_b, lam_re, lam_im, log_dt,
              ssm_b_re, ssm_b_im, ssm_c_re, ssm_c_im, ssm_d, w_glu, b_glu,
              w_branch_a, w_branch_b, w_out, g_ffn, w_q, sub_keys_1, sub_keys_2,
              expert_u, expert_v, g_final):
    bsz, s, d = x.shape
    splits = [CONV_CH, 2 * CONV_CH, 3 * CONV_CH, 3 * CONV_CH + SSM_CH]
    for l in range(DEPTH):
        h = rmsnorm(x, g_mix[l])
        proj = h @ w_in[l]
        cb, cc, cx, su, gl = jnp.split(proj, splits, axis=-1)
        ya = short_conv_mixer(cb, cc, cx, conv_w[l], conv_b[l]) @ w_branch_a[l]
        yb = s5_mixer(su, lam_re[l], lam_im[l], log_dt[l], ssm_b_re[l], ssm_b_im[l],
                      ssm_c_re[l], ssm_c_im[l], ssm_d[l], w_glu[l], b_glu[l]) @ w_branch_b[l]
        g = jax.nn.sigmoid(gl + b_gate[l]).reshape(bsz, s, N_BRANCH, d)
        merged = g[:, :, 0] * ya + g[:, :, 1] * yb
        x = x + merged @ w_out[l]
        x = x + peer_ffn(rmsnorm(x, g_ffn[l]), w_q[l], sub_keys_1[l], sub_keys_2[l],
                         expert_u[l], expert_v[l])
    return rmsnorm(x, g_final)
```

```python
import math
import numpy as np
import concourse.bass as bass
import concourse.mybir as mybir
from concourse.bass_utils import run_bass_kernel_spmd

F32 = mybir.dt.float32
BF16 = mybir.dt.bfloat16
U32 = mybir.dt.uint32
I32 = mybir.dt.int32
ALU = mybir.AluOpType
AF = mybir.ActivationFunctionType
AX = mybir.AxisListType

D = 1024
CONV_CH = 768
SSM_CH = 256
G = 16
P = 64
IN_COLS = 4608
NQ = 2048
NE = 16384
TB = 128
EPS = 1e-6
TWO_PI = 2.0 * math.pi
SKIP_PRECAST = False


class Buf:
    __slots__ = ("name", "w", "r")

    def __init__(self, name=""):
        self.name = name
        self.w = None
        self.r = []


class Sched:
    ENG = ("pe", "dve", "act", "pool", "sp")

    def __init__(self, nc, n_dma_sems=20):
        self.nc = nc
        self.stack = []
        self.streams = {e: [] for e in self.ENG}
        self.cnt = {e: 0 for e in self.ENG}
        self.sem = {}
        for e in self.ENG:
            g = nc.semaphore("s_" + e)
            self.sem[e] = g.__enter__()
            self.stack.append(g)
        self.known = {e: {} for e in self.ENG}
        self.dma_sems = {}
        for q in ("sp", "pool", "act"):
            self.dma_sems[q] = []
            for i in range({"sp": n_dma_sems, "pool": 48, "act": 8}[q]):
                g = nc.semaphore("d%s_%d" % (q, i))
                self.sem["d%s%d" % (q, i)] = g.__enter__()
                self.stack.append(g)
                self.dma_sems[q].append("d%s%d" % (q, i))
        self.dma_cnt = {k: 0 for q in self.dma_sems for k in self.dma_sems[q]}
        self.dma_rr = {"sp": 0, "pool": 0, "act": 0}
        self.n_inst = 0

    def _need(self, eng, deps):
        out = []
        for key, val in deps:
            if val <= 0:
                continue
            if self.known[eng].get(key, 0) >= val:
                continue
            self.known[eng][key] = val
            out.append((key, val))
        return out

    def _deps(self, reads, writes):
        deps = {}
        for b in reads:
            if b.w is not None:
                k, v = b.w
                deps[k] = max(deps.get(k, 0), v)
        for b in writes:
            if b.w is not None:
                k, v = b.w
                deps[k] = max(deps.get(k, 0), v)
            for k, v in b.r:
                deps[k] = max(deps.get(k, 0), v)
        return list(deps.items())

    def op(self, eng, fn, reads=(), writes=()):
        deps = self._deps(reads, writes)
        if eng == "pe":
            deps = [(k, v) for k, v in deps if k != "pe"]
        waits = self._need(eng, deps)
        self.cnt[eng] += 1
        n = self.cnt[eng]
        self.streams[eng].append((waits, fn, (eng, 1)))
        for b in reads:
            b.r.append((eng, n))
            if len(b.r) > 64:
                b.r = b.r[-64:] if False else self._compact(b.r)
        for b in writes:
            b.w = (eng, n)
            b.r = []
        self.n_inst += 1

    @staticmethod
    def _compact(r):
        m = {}
        for k, v in r:
            m[k] = max(m.get(k, 0), v)
        return list(m.items())

    def dma(self, queue, fn, reads=(), writes=()):
        deps = self._deps(reads, writes)
        pool_ = self.dma_sems[queue]
        key = pool_[self.dma_rr[queue] % len(pool_)]
        self.dma_rr[queue] += 1
        prev = self.dma_cnt[key]
        if prev > 0:
            deps.append((key, prev))
        waits = self._need(queue, deps)
        val = prev + 16
        self.dma_cnt[key] = val
        self.streams[queue].append((waits, fn, (key, 16)))
        for b in reads:
            b.r.append((key, val))
            if len(b.r) > 64:
                b.r = self._compact(b.r)
        for b in writes:
            b.w = (key, val)
            b.r = []
        self.n_inst += 1

    def barrier(self, dma=True):
        deps = [(e, self.cnt[e]) for e in self.ENG] + (list(self.dma_cnt.items()) if dma else [])
        for e in self.ENG:
            waits = self._need(e, [d for d in deps if d[0] != e])
            if waits:
                self.streams[e].append((waits, None, None))

    def final_wait(self, eng="sp"):
        deps = [(e, self.cnt[e]) for e in self.ENG if e != eng] + list(self.dma_cnt.items())
        waits = self._need(eng, deps)
        self.streams[eng].append((waits, None, None))

    def emit(self):
        nc = self.nc
        sems = self.sem
        streams = self.streams

        def run(engine, items):
            for waits, fn, inc in items:
                for key, val in waits:
                    engine.wait_ge(sems[key], val)
                if fn is not None:
                    fn(engine).then_inc(sems[inc[0]], inc[1])

        with nc.Block() as block:
            @block.tensor
            def _(e):
                run(e, streams["pe"])

            @block.vector
            def _(e):
                run(e, streams["dve"])

            @block.scalar
            def _(e):
                run(e, streams["act"])

            @block.gpsimd
            def _(e):
                run(e, streams["pool"])

            @block.sync
            def _(e):
                run(e, streams["sp"])

    def close(self):
        for g in reversed(self.stack):
            g.__exit__(None, None, None)


class Alloc:
    def __init__(self, nc):
        self.nc = nc
        self.stack = []

    def sb(self, name, shape, dt=F32, side=None):
        g = self.nc.sbuf_tensor(name, list(shape), dt, side=side) if side else self.nc.sbuf_tensor(name, list(shape), dt)
        t = g.__enter__()
        self.stack.append(g)
        return t

    def ps(self, name, shape, dt=F32):
        g = self.nc.psum_tensor(name, list(shape), dt)
        t = g.__enter__()
        self.stack.append(g)
        return t

    def close(self):
        for g in reversed(self.stack):
            g.__exit__(None, None, None)
        self.stack = []


def build_nc(S_half, debug=False, do_b=True):
    NB = S_half // TB
    nc = bass.Bass("TRN2", target_bir_lowering=False)

    def din(name, shape, dt=F32):
        return nc.dram_tensor(name, list(shape), dt, kind="ExternalInput").ap()

    x_main = din("x_main", [S_half, D])
    x_prev = din("x_prev", [S_half, D])
    w_in = din("w_in", [D, IN_COLS])
    w_a = din("w_a", [CONV_CH, D])
    w_b = din("w_b", [SSM_CH, D])
    w_out = din("w_out", [D, D])
    w_glu = din("w_glu", [SSM_CH, SSM_CH])
    w_q = din("w_q", [D, NQ])
    keysT = din("keysT", [2, 128, 128])
    g_rep = din("g_rep", [3, 128, D])
    colp = din("colp", [128, 48])
    ssm_pt = din("ssm_pt", [3, 128, G])
    ssm_row = din("ssm_row", [3, 128, G * P])
    bt_in = din("bt_in", [2, 128, G * P])
    ct_in = din("ct_in", [128, G * 128])
    exp_u = din("exp_u", [NE, D])
    exp_v = din("exp_v", [NE, D])
    out_d = nc.dram_tensor("out", [S_half, D], F32, kind="ExternalOutput").ap()
    if debug:
        x1_d = nc.dram_tensor("x1_d", [S_half, D], F32, kind="ExternalOutput").ap()
        dbg_d = nc.dram_tensor("dbg", [128, 2 * S_half], F32, kind="ExternalOutput").ap()
    else:
        x1_d = nc.dram_tensor("x1_d", [S_half, D], F32, kind="Internal").ap()
        dbg_d = None

    uv_d = nc.dram_tensor("uv_d", [NE, 2 * D], BF16, kind="Internal").ap()
    B_uv = Buf("uv_d")
    B_x1d = Buf("x1_d")

    S = Sched(nc)
    op, dma = S.op, S.dma

    P0 = Alloc(nc)
    identf = P0.sb("identf", [128, 128])
    ident = P0.sb("ident", [128, 128], BF16)
    junk = P0.sb("junk", [128, D], BF16)
    stat = P0.sb("stat", [128, 8])
    B_junk = Buf("junk")
    B_stat = Buf("stat")
    ps_tr = P0.ps("ps_tr", [128, 8, 128], BF16)
    B_ptr = Buf("ps_tr")
    NPS = 5
    ps_acc = [P0.ps("ps_acc%d" % i, [128, 512]) for i in range(2)]
    B_pacc = Buf("ps_acc")
    ps_mm = [P0.ps("ps_mm%d" % i, [128, 512]) for i in range(NPS)]
    B_ps = [Buf("ps%d" % i) for i in range(NPS)]
    ps_rr = [0]
    ps_pool = list(range(NPS))

    def next_ps():
        i = ps_pool[ps_rr[0] % len(ps_pool)]
        ps_rr[0] += 1
        return ps_mm[i], B_ps[i]

    def mm_group(out_ap, b_out, pairs, reads):
        n = len(pairs)
        for i, (l, r) in enumerate(pairs):
            op("pe", lambda e, l=l, r=r, i=i: e.matmul(out_ap, lhsT=l, rhs=r, start=(i == 0), stop=(i == n - 1)),
               reads=reads, writes=[b_out])

    def rstd_of(src, b_src, col):
        op("act", lambda e: e.activation(out=junk[:], in_=src, func=AF.Square, accum_out=stat[:, col:col + 1]),
           reads=[b_src, B_stat], writes=[B_junk, B_stat])
        op("dve", lambda e: e.tensor_scalar(out=stat[:, col + 1:col + 2], in0=stat[:, col:col + 1], scalar1=1.0 / D, scalar2=EPS,
                                            op0=ALU.mult, op1=ALU.add), reads=[B_stat], writes=[B_stat])
        op("act", lambda e: e.activation(out=stat[:, col + 2:col + 3], in_=stat[:, col + 1:col + 2], func=AF.Sqrt),
           reads=[B_stat], writes=[B_stat])
        op("dve", lambda e: e.reciprocal(out=stat[:, col + 3:col + 4], in_=stat[:, col + 2:col + 3]), reads=[B_stat], writes=[B_stat])

    def norm_tile(src, b_src, g_ap, b_gconst, dst, b_dst, col):
        rstd_of(src, b_src, col)
        op("dve", lambda e: e.scalar_tensor_tensor(out=dst, in0=src, scalar=stat[:, col + 3:col + 4], in1=g_ap,
                                                   op0=ALU.mult, op1=ALU.mult),
           reads=[b_src, B_stat, b_gconst], writes=[b_dst])

    def transposes(src_bf, b_src, dstT, b_dstT):
        for k in range(8):
            op("pe", lambda e, k=k: e.transpose(out=ps_tr[:, k, :], in_=src_bf[:, k * 128:(k + 1) * 128], identity=ident[:]),
               reads=[b_src, B_id], writes=[B_ptr])
        op("act", lambda e: e.activation(out=dstT[:, :, :], in_=ps_tr[:], func=AF.Copy), reads=[B_ptr], writes=[b_dstT])

    A = Alloc(nc)
    sb = A.sb
    w_in_sb = sb("w_in_sb", [128, 8, IN_COLS], BF16)
    w_a_sb = sb("w_a_sb", [128, 6, D], BF16)
    w_b_sb = sb("w_b_sb", [128, 2, D], BF16)
    w_out_sb = sb("w_out_sb", [128, 8, D], BF16)
    w_glu_sb = sb("w_glu_sb", [128, 2, SSM_CH], BF16)
    B_w = Buf("weights")

    def load_weights():
        for kc in range(8):
            for c in range(3):
                dma("pool", lambda e, kc=kc, c=c: e.dma_start(
                    out=w_in_sb[:, kc, c * 1536:(c + 1) * 1536],
                    in_=w_in[kc * 128:(kc + 1) * 128, c * 1536:(c + 1) * 1536]), writes=[B_w])
        for kc in range(6):
            dma("pool", lambda e, kc=kc: e.dma_start(out=w_a_sb[:, kc, :], in_=w_a[kc * 128:(kc + 1) * 128, :]), writes=[B_w])
        for kc in range(2):
            dma("pool", lambda e, kc=kc: e.dma_start(out=w_b_sb[:, kc, :], in_=w_b[kc * 128:(kc + 1) * 128, :]), writes=[B_w])
            dma("pool", lambda e, kc=kc: e.dma_start(out=w_glu_sb[:, kc, :], in_=w_glu[kc * 128:(kc + 1) * 128, :]), writes=[B_w])
        for kc in range(8):
            dma("pool", lambda e, kc=kc: e.dma_start(out=w_out_sb[:, kc, :], in_=w_out[kc * 128:(kc + 1) * 128, :]), writes=[B_w])


    g_sb = sb("g_sb", [128, D])
    colp_sb = sb("colp_sb", [128, 48])
    B_c = Buf("consts")
    dma("sp", lambda e: e.dma_start(out=g_sb[:], in_=g_rep[0]), writes=[B_c])
    dma("sp", lambda e: e.dma_start(out=colp_sb[:], in_=colp[:, :]), writes=[B_c])
    CW = lambda c, k: colp_sb[:, 16 + c * 3 + k:17 + c * 3 + k]
    CBIAS = lambda c: colp_sb[:, 34 + c:35 + c]

    B_id = Buf("ident")
    op("pool", lambda e: e.iota(identf[:], [[1, 128]], base=0, channel_multiplier=-1,
                                allow_small_or_imprecise_dtypes=True), writes=[B_id])
    op("dve", lambda e: e.tensor_single_scalar(out=ident[:], in_=identf[:], scalar=0.0, op=ALU.is_equal),
       reads=[B_id], writes=[B_id])

    r_p = sb("r_p", [128, G])
    ctab = sb("ctab", [128, G, TB + 1])
    stab = sb("stab", [128, G, TB + 1])
    nsT = sb("nsT", [128, G])
    bpq = sb("bpq", [128, G, 2, 128], BF16)
    ctb = sb("ctb", [128, G, 128], BF16)
    B_s = Buf("ssmc")
    LP = 2 if NB % 2 == 0 else 1
    AXr = Alloc(nc)
    PT = AXr.sb("PT", [128, LP, G, 128], BF16, side="right")
    CRI = AXr.sb("CRI", [128, 2, G * P], F32, side="right")
    Mst = AXr.sb("Mst", [128, G, 128], F32, side="right")
    A00 = Alloc(nc)
    ctf = A00.sb("ctf", [128, G * 128])
    dma("sp", lambda e: e.dma_start(out=ctf[:], in_=ct_in[:, :]), writes=[B_s])
    op("dve", lambda e: e.tensor_scalar(out=ctf[64:128, :], in0=ctf[64:128, :], scalar1=-1.0, scalar2=None, op0=ALU.mult), reads=[B_s], writes=[B_s])
    op("dve", lambda e: e.tensor_copy(out=ctb[:].rearrange("p g m -> p (g m)"), in_=ctf[:]), reads=[B_s], writes=[B_s])
    S.barrier(dma=False)
    A00.close()
    A0 = Alloc(nc)
    sb0 = A0.sb
    spt = sb0("spt", [128, 3, G])
    HW = 8 * P
    srow = sb0("srow", [128, 3, HW])
    bt = sb0("bt", [128, 2, HW])
    ecol = sb0("ecol", [128, 4])
    dma("sp", lambda e: e.dma_start(out=spt[:], in_=ssm_pt.rearrange("a p g -> p a g")), writes=[B_s])
    for l in range(LP):
        op("pool", lambda e, l=l: e.iota(ecol[:, l:l + 1], [[0, 1]], base=128 * l + 127, channel_multiplier=-1,
                                         allow_small_or_imprecise_dtypes=True), writes=[B_s])

    thp_p = sb0("thp_p", [128, G])
    tmp_p = sb0("tmp_p", [128, 4, G])
    iota_t = sb0("iota_t", [128, TB + 1])
    ytmp = sb0("ytmp", [128, 3, TB + 1])
    yti = sb0("yti", [128, TB + 1], I32)
    rti = sb0("rti", [128, HW], I32)
    rt2 = sb0("rt2", [128, HW])
    R = [sb0("rw%d" % i, [128, HW]) for i in range(8)]

    def expm1_poly(eng, out, z, t):
        op(eng, lambda e: e.tensor_scalar(out=out, in0=z, scalar1=1.0 / 720, scalar2=1.0 / 120, op0=ALU.mult, op1=ALU.add), reads=[B_s], writes=[B_s])
        for c in (1.0 / 24, 1.0 / 6, 0.5, 1.0):
            op(eng, lambda e: e.tensor_tensor(out=t, in0=out, in1=z, op=ALU.mult), reads=[B_s], writes=[B_s])
            op(eng, lambda e, c=c: e.tensor_scalar(out=out, in0=t, scalar1=c, scalar2=None, op0=ALU.add), reads=[B_s], writes=[B_s])
        op(eng, lambda e: e.tensor_tensor(out=t, in0=out, in1=z, op=ALU.mult), reads=[B_s], writes=[B_s])
        op(eng, lambda e: e.tensor_copy(out=out, in_=t), reads=[B_s], writes=[B_s])

    def sin_turns(out, y, off, t, ti, t2):
        op("dve", lambda e: e.tensor_scalar(out=t, in0=y, scalar1=8.0 + off, scalar2=None, op0=ALU.add), reads=[B_s], writes=[B_s])
        op("dve", lambda e: e.tensor_copy(out=ti, in_=t), reads=[B_s], writes=[B_s])
        op("dve", lambda e: e.tensor_copy(out=t2, in_=ti), reads=[B_s], writes=[B_s])
        op("dve", lambda e: e.tensor_tensor(out=t, in0=t, in1=t2, op=ALU.subtract), reads=[B_s], writes=[B_s])
        op("dve", lambda e: e.tensor_single_scalar(out=t2, in_=t, scalar=0.5, op=ALU.is_gt), reads=[B_s], writes=[B_s])
        op("dve", lambda e: e.tensor_tensor(out=t, in0=t, in1=t2, op=ALU.subtract), reads=[B_s], writes=[B_s])
        op("act", lambda e: e.activation(out=out, in_=t, func=AF.Sin, scale=TWO_PI), reads=[B_s], writes=[B_s])

    dt_p = tmp_p[:, 0, :]
    z_p = tmp_p[:, 1, :]
    e_p = tmp_p[:, 2, :]
    t_p = tmp_p[:, 3, :]
    op("act", lambda e: e.activation(out=dt_p, in_=spt[:, 2, :], func=AF.Exp), reads=[B_s], writes=[B_s])
    op("dve", lambda e: e.tensor_tensor(out=z_p, in0=spt[:, 0, :], in1=dt_p, op=ALU.mult), reads=[B_s], writes=[B_s])
    expm1_poly("dve", e_p, z_p, t_p)
    op("dve", lambda e: e.tensor_scalar(out=r_p[:], in0=e_p, scalar1=1.0, scalar2=None, op0=ALU.add), reads=[B_s], writes=[B_s])
    op("dve", lambda e: e.tensor_tensor(out=t_p, in0=spt[:, 1, :], in1=dt_p, op=ALU.mult), reads=[B_s], writes=[B_s])
    op("dve", lambda e: e.tensor_scalar(out=thp_p[:], in0=t_p, scalar1=1.0 / TWO_PI, scalar2=None, op0=ALU.mult), reads=[B_s], writes=[B_s])
    op("pool", lambda e: e.iota(iota_t[:], [[1, TB + 1]], base=0, channel_multiplier=0,
                                allow_small_or_imprecise_dtypes=True), writes=[B_s])
    load_weights()
    for g in range(G):
        op("dve", lambda e, g=g: e.tensor_scalar(out=ytmp[:, 0, :], in0=iota_t[:], scalar1=thp_p[:, g:g + 1], scalar2=None, op0=ALU.mult), reads=[B_s], writes=[B_s])
        sin_turns(stab[:, g, :], ytmp[:, 0, :], 0.0, ytmp[:, 1, :], yti[:], ytmp[:, 2, :])
        sin_turns(ctab[:, g, :], ytmp[:, 0, :], 0.25, ytmp[:, 1, :], yti[:], ytmp[:, 2, :])
    op("dve", lambda e: e.tensor_scalar(out=nsT[:], in0=stab[:, :, TB], scalar1=-1.0, scalar2=None, op0=ALU.mult), reads=[B_s], writes=[B_s])

    for hh in range(2):
        gs = slice(8 * hh, 8 * hh + 8)
        for a in range(3):
            dma("sp", lambda e, a=a, hh=hh: e.dma_start(out=srow[:, a, :], in_=ssm_row[a][:, hh * HW:(hh + 1) * HW]), reads=[B_s], writes=[B_s])
        for a in range(2):
            dma("sp", lambda e, a=a, hh=hh: e.dma_start(out=bt[:, a, :], in_=bt_in[a][:, hh * HW:(hh + 1) * HW]), reads=[B_s], writes=[B_s])
        lr_r, li_r = srow[:, 0, :], srow[:, 1, :]
        dt_r, z_r, em1_r, t_r, thp_r, sn_r, cs_r, sh_r = [R[i][:] for i in range(8)]
        op("act", lambda e: e.activation(out=dt_r, in_=srow[:, 2, :], func=AF.Exp), reads=[B_s], writes=[B_s])
        op("dve", lambda e: e.tensor_tensor(out=z_r, in0=lr_r, in1=dt_r, op=ALU.mult), reads=[B_s], writes=[B_s])
        expm1_poly("dve", em1_r, z_r, t_r)
        op("dve", lambda e: e.tensor_tensor(out=t_r, in0=li_r, in1=dt_r, op=ALU.mult), reads=[B_s], writes=[B_s])
        op("dve", lambda e: e.tensor_scalar(out=thp_r, in0=t_r, scalar1=1.0 / TWO_PI, scalar2=None, op0=ALU.mult), reads=[B_s], writes=[B_s])
        y_r, mg_r = R[5][:], R[0][:]
        for l in range(LP):
            op("dve", lambda e, l=l: e.tensor_scalar(out=y_r, in0=thp_r, scalar1=ecol[:, l:l + 1], scalar2=None, op0=ALU.mult), reads=[B_s], writes=[B_s])
            sin_turns(R[7][:], y_r, 0.0, t_r, rti[:], rt2[:])
            sin_turns(R[6][:], y_r, 0.25, t_r, rti[:], rt2[:])
            op("act", lambda e, l=l: e.activation(out=mg_r, in_=z_r, func=AF.Exp, scale=ecol[:, l:l + 1]), reads=[B_s], writes=[B_s])
            op("dve", lambda e, l=l, gs=gs: e.tensor_tensor(out=PT[:, l, gs, 0:64], in0=mg_r.rearrange("p (g s) -> p g s", g=8), in1=R[6][:].rearrange("p (g s) -> p g s", g=8), op=ALU.mult), reads=[B_s], writes=[B_s])
            op("dve", lambda e, l=l, gs=gs: e.tensor_tensor(out=PT[:, l, gs, 64:128], in0=mg_r.rearrange("p (g s) -> p g s", g=8), in1=R[7][:].rearrange("p (g s) -> p g s", g=8), op=ALU.mult), reads=[B_s], writes=[B_s])
        op("dve", lambda e: e.tensor_scalar(out=y_r, in0=thp_r, scalar1=float(128 * LP), scalar2=None, op0=ALU.mult), reads=[B_s], writes=[B_s])
        sin_turns(R[7][:], y_r, 0.0, t_r, rti[:], rt2[:])
        sin_turns(R[6][:], y_r, 0.25, t_r, rti[:], rt2[:])
        op("act", lambda e: e.activation(out=mg_r, in_=z_r, func=AF.Exp, scale=float(128 * LP)), reads=[B_s], writes=[B_s])
        op("dve", lambda e, hh=hh: e.tensor_tensor(out=CRI[:, 0, hh * HW:(hh + 1) * HW], in0=mg_r, in1=R[6][:], op=ALU.mult), reads=[B_s], writes=[B_s])
        op("dve", lambda e, hh=hh: e.tensor_tensor(out=CRI[:, 1, hh * HW:(hh + 1) * HW], in0=mg_r, in1=R[7][:], op=ALU.mult), reads=[B_s], writes=[B_s])
        op("act", lambda e: e.activation(out=dt_r, in_=srow[:, 2, :], func=AF.Exp), reads=[B_s], writes=[B_s])
        sin_turns(sn_r, thp_r, 0.0, t_r, rti[:], rt2[:])
        sin_turns(cs_r, thp_r, 0.25, t_r, rti[:], rt2[:])
        op("dve", lambda e: e.tensor_scalar(out=dt_r, in0=thp_r, scalar1=0.5, scalar2=None, op0=ALU.mult), reads=[B_s], writes=[B_s])
        sin_turns(sh_r, dt_r, 0.0, t_r, rti[:], rt2[:])
        op("dve", lambda e: e.tensor_tensor(out=z_r, in0=em1_r, in1=cs_r, op=ALU.mult), reads=[B_s], writes=[B_s])
        op("dve", lambda e: e.tensor_tensor(out=t_r, in0=sh_r, in1=sh_r, op=ALU.mult), reads=[B_s], writes=[B_s])
        op("dve", lambda e: e.scalar_tensor_tensor(out=z_r, in0=t_r, scalar=-2.0, in1=z_r, op0=ALU.mult, op1=ALU.add), reads=[B_s], writes=[B_s])
        op("dve", lambda e: e.scalar_tensor_tensor(out=sn_r, in0=em1_r, scalar=1.0, in1=sn_r, op0=ALU.add, op1=ALU.mult), reads=[B_s], writes=[B_s])
        nr_r, ni_r = z_r, sn_r
        op("dve", lambda e: e.tensor_tensor(out=cs_r, in0=lr_r, in1=lr_r, op=ALU.mult), reads=[B_s], writes=[B_s])
        op("dve", lambda e: e.tensor_tensor(out=t_r, in0=li_r, in1=li_r, op=ALU.mult), reads=[B_s], writes=[B_s])
        op("dve", lambda e: e.tensor_tensor(out=cs_r, in0=cs_r, in1=t_r, op=ALU.add), reads=[B_s], writes=[B_s])
        op("dve", lambda e: e.reciprocal(out=cs_r, in_=cs_r), reads=[B_s], writes=[B_s])
        rden = cs_r
        op("dve", lambda e: e.tensor_tensor(out=em1_r, in0=nr_r, in1=lr_r, op=ALU.mult), reads=[B_s], writes=[B_s])
        op("dve", lambda e: e.tensor_tensor(out=t_r, in0=ni_r, in1=li_r, op=ALU.mult), reads=[B_s], writes=[B_s])
        op("dve", lambda e: e.tensor_tensor(out=em1_r, in0=em1_r, in1=t_r, op=ALU.add), reads=[B_s], writes=[B_s])
        op("dve", lambda e: e.tensor_tensor(out=em1_r, in0=em1_r, in1=rden, op=ALU.mult), reads=[B_s], writes=[B_s])
        op("dve", lambda e: e.tensor_tensor(out=sh_r, in0=ni_r, in1=lr_r, op=ALU.mult), reads=[B_s], writes=[B_s])
        op("dve", lambda e: e.tensor_tensor(out=t_r, in0=nr_r, in1=li_r, op=ALU.mult), reads=[B_s], writes=[B_s])
        op("dve", lambda e: e.tensor_tensor(out=sh_r, in0=sh_r, in1=t_r, op=ALU.subtract), reads=[B_s], writes=[B_s])
        op("dve", lambda e: e.tensor_tensor(out=sh_r, in0=sh_r, in1=rden, op=ALU.mult), reads=[B_s], writes=[B_s])
        k_re, k_im = em1_r, sh_r
        op("dve", lambda e: e.tensor_tensor(out=z_r, in0=k_re, in1=bt[:, 0, :], op=ALU.mult), reads=[B_s], writes=[B_s])
        op("dve", lambda e: e.tensor_tensor(out=t_r, in0=k_im, in1=bt[:, 1, :], op=ALU.mult), reads=[B_s], writes=[B_s])
        op("dve", lambda e: e.tensor_tensor(out=z_r, in0=z_r, in1=t_r, op=ALU.subtract), reads=[B_s], writes=[B_s])
        op("dve", lambda e: e.tensor_tensor(out=sn_r, in0=k_re, in1=bt[:, 1, :], op=ALU.mult), reads=[B_s], writes=[B_s])
        op("dve", lambda e: e.tensor_tensor(out=t_r, in0=k_im, in1=bt[:, 0, :], op=ALU.mult), reads=[B_s], writes=[B_s])
        op("dve", lambda e: e.tensor_tensor(out=sn_r, in0=sn_r, in1=t_r, op=ALU.add), reads=[B_s], writes=[B_s])
        bre3 = R[1][:].rearrange("p (g s) -> p g s", g=8)
        bim3 = R[5][:].rearrange("p (g s) -> p g s", g=8)
        op("dve", lambda e, gs=gs: e.tensor_copy(out=bpq[:, gs, 0, 0:64], in_=bre3), reads=[B_s], writes=[B_s])
        op("dve", lambda e, gs=gs: e.tensor_copy(out=bpq[:, gs, 0, 64:128], in_=bim3), reads=[B_s], writes=[B_s])
        op("dve", lambda e, gs=gs: e.tensor_copy(out=bpq[:, gs, 1, 0:64], in_=bim3), reads=[B_s], writes=[B_s])
        op("dve", lambda e, gs=gs: e.tensor_scalar(out=bpq[:, gs, 1, 64:128], in0=bre3, scalar1=-1.0, scalar2=None, op0=ALU.mult), reads=[B_s], writes=[B_s])

    S.barrier()
    A0.close()
    x_tok = [sb("x_tok%d" % i, [128, D]) for i in range(3)]
    B_x = [Buf("x_tok%d" % i) for i in range(3)]
    h_bf = sb("h_bf", [128, 1, D], BF16)
    B_h = [Buf("h0")]
    hT2 = [sb("hT%d" % i, [128, 8, TB], BF16) for i in range(2)]
    B_hT2 = [Buf("hT0"), Buf("hT1")]
    v_sb = sb("v_sb", [128, 6, TB + 2])
    B_v = [Buf("v%d" % c) for c in range(6)]
    cc_sb = sb("cc_sb", [128, 2, TB])
    B_cc = [Buf("cc0"), Buf("cc1")]
    init = sb("init", [128, G, 2])
    B_init = [Buf("init%d" % g) for g in range(G)]
    op("pool", lambda e: e.memset(v_sb[:], 0.0), writes=B_v)
    op("pool", lambda e: e.memset(init[:], 0.0), writes=B_init)

    blocks = [("pre", i) for i in range(NB)] + [("main", i) for i in range(NB)]

    def proj(par, mt):
        ps, b_ps = next_ps()
        mm_group(ps[:, 0:TB], b_ps,
                 [(w_in_sb[:, k, mt * 128:(mt + 1) * 128], hT2[par][:, k, :]) for k in range(8)], [B_w, B_hT2[par]])
        return ps, b_ps

    def alloc_main():
        g = {}
        g["ct1"] = sb("ct1", [128, 2, TB]); g["ct2"] = sb("ct2", [128, 2, TB])
        g["outA2"] = [sb("outA%d" % i, [128, 6, TB], BF16) for i in range(2)]
        g["uT2"] = [sb("uT%d" % i, [128, 2, TB], BF16) for i in range(2)]
        g["u_f2"] = [sb("u_f%d" % i, [128, 2, TB]) for i in range(2)]
        g["gates2"] = [sb("gates%d" % i, [128, 16, TB], BF16) for i in range(2)]
        g["st"] = [[sb("st%d_%d" % (i, j), [128, TB]) for j in range(NST)] for i in range(NSB)]
        g["s_bf"] = sb("s_bf", [128, NSB, TB], BF16)
        g["itmp"] = sb("itmp", [128, G, 2])
        g["ctmp"] = sb("ctmp", [128, 4, G])
        g["y_sb"] = sb("y_sb", [128, 2, TB]); g["z_f"] = sb("z_f", [128, 2, TB]); g["z_b"] = sb("z_b", [128, 2, TB], BF16)
        g["sg"] = sb("sg", [128, 2, TB]); g["outB"] = sb("outB", [128, 2, TB], BF16)
        g["m1"] = sb("m1", [128, 1, TB]); g["m2"] = sb("m2", [128, 1, TB]); g["merged"] = sb("merged", [128, 8, TB], BF16)
        g["stg32"] = [sb("stg32_%d" % i, [128, 512]) for i in range(2)]
        g["stgb"] = [sb("stgb_%d" % i, [128, 512], BF16) for i in range(2)]
        return g

    NSB = 3
    NST = 10
    B_ct = [Buf("ct0"), Buf("ct1")]
    B_outA2 = [Buf("outA0"), Buf("outA1")]
    B_u2 = [Buf("u0"), Buf("u1")]
    B_g2 = [Buf("gates0"), Buf("gates1")]
    B_st = [[Buf("st%d_%d" % (i, j)) for j in range(NST)] for i in range(NSB)]
    B_sbf = [Buf("sbf%d" % i) for i in range(NSB)]
    B_y = [Buf("y0"), Buf("y1")]
    B_outB = Buf("outB")
    B_m = [Buf("m0"), Buf("m1")]
    B_mg = Buf("merged")
    B_wl = Buf("wl")
    B_ctmp = Buf("ctmp")
    B_yacc = [Buf("yacc0"), Buf("yacc1")]
    B_stg = [Buf("stg0"), Buf("stg1")]
    pre_chunks = [(tb, ch, hf) for tb in range(2) for ch in range(NE // 128) for hf in range(2)]
    pre_i = [0]
    n_pre_per_blk = -(-len(pre_chunks) // NB)
    MA = {}
    B_stg32 = [Buf("stg32_0"), Buf("stg32_1")]

    def precast_gen(n):
        if SKIP_PRECAST:
            return
        stg32, stgb = MA["stg32"], MA["stgb"]

        def load(i):
            tb, ch, hf = pre_chunks[i]
            src = (exp_u if tb == 0 else exp_v)[ch * 128:(ch + 1) * 128, hf * 512:(hf + 1) * 512]
            dma("act", lambda e, src=src, j=i % 2: e.dma_start(out=stg32[j][:], in_=src), writes=[B_stg32[i % 2]])

        for _ in range(n):
            i = pre_i[0]
            if i >= len(pre_chunks):
                return
            pre_i[0] += 1
            if i == 0:
                load(0)
            if i + 1 < len(pre_chunks):
                pass
            tb, ch, hf = pre_chunks[i]
            j = i % 2
            dst = uv_d[ch * 128:(ch + 1) * 128, tb * D + hf * 512:tb * D + (hf + 1) * 512]
            op("act", lambda e, j=j: e.activation(out=stgb[j][:], in_=stg32[j][:], func=AF.Copy), reads=[B_stg32[j]], writes=[B_stg[j]])
            if i + 1 < len(pre_chunks):
                load(i + 1)
            dma("act", lambda e, dst=dst, j=j: e.dma_start(out=dst, in_=stgb[j][:]), reads=[B_stg[j]], writes=[B_uv])
            yield

    def load_norm_T(bi):
        kind, blk = blocks[bi]
        par = bi % 2
        xsrc = x_main if kind == "main" else x_prev
        xt, b_xt = x_tok[bi % 3], B_x[bi % 3]
        dma("sp", lambda e: e.dma_start(out=xt[:], in_=xsrc[blk * TB:(blk + 1) * TB, :]), writes=[b_xt])
        norm_tile(xt[:], b_xt, g_sb[:], B_c, h_bf[:, 0, :], B_h[0], 4 * par)
        transposes(h_bf[:, 0, :], B_h[0], hT2[par], B_hT2[par])

    def front(bi):
        kind, blk = blocks[bi]
        par = bi % 2
        load_norm_T(bi)
        yield
        for m in range(2):
            ps_u, b_u = proj(par, 18 + m)
            op("act", lambda e, m=m, ps_u=ps_u: e.activation(out=u_f2[par][:, m, :], in_=ps_u[:, 0:TB], func=AF.Copy), reads=[b_u], writes=[B_u2[par]])
            op("act", lambda e, m=m, ps_u=ps_u: e.activation(out=uT2[par][:, m, :], in_=ps_u[:, 0:TB], func=AF.Copy), reads=[b_u], writes=[B_u2[par]])
            yield

    def delayed(gen, n):
        for _ in range(n):
            yield
        for _ in gen:
            yield

    def convgate(bi):
        kind, blk = blocks[bi]
        par = bi % 2
        is_main = kind == "main"
        last_pre = (kind == "pre" and blk == NB - 1)
        if is_main:
            outA, B_outA = outA2[par], B_outA2[par]
            gates, B_g = gates2[par], B_g2[par]
        if is_main or last_pre:
            for c in range(6):
                j = c % 2
                ps_c, b_c = proj(par, 6 + c)
                ps_x, b_xx = proj(par, 12 + c)
                op("act", lambda e, ps_c=ps_c, j=j: e.activation(out=cc_sb[:, j, :], in_=ps_c[:, 0:TB], func=AF.Copy),
                   reads=[b_c], writes=[B_cc[j]])
                op("dve", lambda e, ps_x=ps_x, j=j, c=c: e.tensor_tensor(out=v_sb[:, c, 2:TB + 2], in0=cc_sb[:, j, :], in1=ps_x[:, 0:TB], op=ALU.mult),
                   reads=[B_cc[j], b_xx], writes=[B_v[c]])
                yield
                if is_main:
                    ps_b, b_b = proj(par, c)
                    op("pool", lambda e, c=c, j=j: e.tensor_scalar(out=ct1[:, j, :], in0=v_sb[:, c, 2:TB + 2], scalar1=CW(c, 2), scalar2=CBIAS(c),
                                                                   op0=ALU.mult, op1=ALU.add), reads=[B_v[c], B_c], writes=[B_ct[j]])
                    op("dve", lambda e, c=c, j=j: e.scalar_tensor_tensor(out=ct2[:, j, :], in0=v_sb[:, c, 1:TB + 1], scalar=CW(c, 1), in1=ct1[:, j, :],
                                                                         op0=ALU.mult, op1=ALU.add), reads=[B_v[c], B_c, B_ct[j]], writes=[B_ct[j]])
                    op("dve", lambda e, c=c, j=j: e.scalar_tensor_tensor(out=ct1[:, j, :], in0=v_sb[:, c, 0:TB], scalar=CW(c, 0), in1=ct2[:, j, :],
                                                                         op0=ALU.mult, op1=ALU.add), reads=[B_v[c], B_c, B_ct[j]], writes=[B_ct[j]])
                    op("dve", lambda e, c=c, j=j, ps_b=ps_b: e.tensor_tensor(out=outA[:, c, :], in0=ct1[:, j, :], in1=ps_b[:, 0:TB], op=ALU.mult),
                       reads=[B_ct[j], b_b], writes=[B_outA])
                op("pool", lambda e, c=c: e.tensor_copy(out=v_sb[:, c, 0:2], in_=v_sb[:, c, TB:TB + 2]), reads=[B_v[c]], writes=[B_v[c]])
                yield
        if is_main:
            for t in range(16):
                ps_g, b_gg = proj(par, 20 + t)
                op("act", lambda e, t=t, ps_g=ps_g: e.activation(out=gates[:, t, :], in_=ps_g[:, 0:TB], func=AF.Sigmoid,
                                                                 bias=colp_sb[:, t:t + 1], scale=1.0), reads=[b_gg, B_c], writes=[B_g])
                if t % 2 == 1:
                    yield

    def ssm(bi):
        kind, blk = blocks[bi]
        par = bi % 2
        is_main = kind == "main"
        uT, B_u = uT2[par], B_u2[par]
        pq = {}

        def stage0(g):
            ps_p, b_p = ps_mm[3 + (g % 2)], B_ps[3 + (g % 2)]
            m = g // 8
            mm_group(ps_p[:, 0:TB], b_p, [(bpq[:, g, 0, :], uT[:, m, :])], [B_s, B_u])
            mm_group(ps_p[:, TB:2 * TB], b_p, [(bpq[:, g, 1, :], uT[:, m, :])], [B_s, B_u])
            pq[g] = (ps_p, b_p)

        def bufs(g):
            si = g % NSB
            return [st[si][j][:] for j in range(NST)], B_st[si], si

        def stage1(g):
            (R1, R2, W1, W2, T1, T2, T3, T4, T5, T6), (bR1, bR2, bW1, bW2, bT1, bT2, bT3, bT4, bT5, bT6), si = bufs(g)
            ps_p, b_p = pq.pop(g)
            Pp, Qp = ps_p[:, 0:TB], ps_p[:, TB:2 * TB]
            C_g, S_g = ctab[:, g, 0:TB], stab[:, g, 0:TB]
            op("dve", lambda e: e.tensor_tensor(out=T1, in0=C_g, in1=Pp, op=ALU.mult), reads=[B_s, b_p], writes=[bT1])
            op("dve", lambda e: e.tensor_tensor(out=T2, in0=S_g, in1=Qp, op=ALU.mult), reads=[B_s, b_p], writes=[bT2])
            op("dve", lambda e: e.tensor_tensor(out=T3, in0=C_g, in1=Qp, op=ALU.mult), reads=[B_s, b_p], writes=[bT3])
            op("dve", lambda e: e.tensor_tensor(out=T4, in0=S_g, in1=Pp, op=ALU.mult), reads=[B_s, b_p], writes=[bT4])

        def stage2(g):
            (R1, R2, W1, W2, T1, T2, T3, T4, T5, T6), (bR1, bR2, bW1, bW2, bT1, bT2, bT3, bT4, bT5, bT6), si = bufs(g)
            op("dve", lambda e: e.tensor_tensor(out=R1, in0=T1, in1=T2, op=ALU.add), reads=[bT1, bT2], writes=[bR1])
            op("pool", lambda e: e.tensor_tensor(out=R2, in0=T3, in1=T4, op=ALU.subtract), reads=[bT3, bT4], writes=[bR2])

        def stage3(g):
            (R1, R2, W1, W2, T1, T2, T3, T4, T5, T6), (bR1, bR2, bW1, bW2, bT1, bT2, bT3, bT4, bT5, bT6), si = bufs(g)
            rb = r_p[:, g:g + 1].to_broadcast([128, TB])
            op("dve", lambda e: e.tensor_tensor_scan(out=W1, data0=rb, data1=R1, initial=init[:, g, 0:1], op0=ALU.mult, op1=ALU.add),
               reads=[B_s, bR1, B_init[g]], writes=[bW1])
            op("dve", lambda e: e.tensor_tensor_scan(out=W2, data0=rb, data1=R2, initial=init[:, g, 1:2], op0=ALU.mult, op1=ALU.add),
               reads=[B_s, bR2, B_init[g]], writes=[bW2])

        def stage4(g):
            (R1, R2, W1, W2, T1, T2, T3, T4, T5, T6), (bR1, bR2, bW1, bW2, bT1, bT2, bT3, bT4, bT5, bT6), si = bufs(g)
            C_g, S_g = ctab[:, g, 0:TB], stab[:, g, 0:TB]
            op("act", lambda e: e.activation(out=itmp[:, g, 0:1], in_=W1[:, TB - 1:TB], func=AF.Copy), reads=[bW1], writes=[B_wl])
            op("act", lambda e: e.activation(out=itmp[:, g, 1:2], in_=W2[:, TB - 1:TB], func=AF.Copy), reads=[bW2], writes=[B_wl])
            op("pool", lambda e: e.tensor_tensor(out=T5, in0=C_g, in1=W1, op=ALU.mult), reads=[B_s, bW1], writes=[bT5])
            op("pool", lambda e: e.tensor_tensor(out=T6, in0=S_g, in1=W2, op=ALU.mult), reads=[B_s, bW2], writes=[bT6])
            op("pool", lambda e: e.tensor_tensor(out=s_bf[:, si, :], in0=T5, in1=T6, op=ALU.subtract), reads=[bT5, bT6], writes=[B_sbf[si]])

        def stage5(g):
            si = g % NSB
            m = g // 8
            op("pe", lambda e: e.matmul(ps_acc[m][:, 0:TB], lhsT=ctb[:, g, :], rhs=s_bf[:, si, :], start=(g % 8 == 0), stop=(g % 8 == 7)),
               reads=[B_s, B_sbf[si]], writes=[B_yacc[m]])

        stages = [stage0, stage1, stage2, stage3, stage4, stage5]
        for step in range(G + len(stages) - 1):
            for k, fn in enumerate(stages):
                if 0 <= step - k < G:
                    fn(step - k)
            yield
        cT, sT = ctab[:, :, TB], stab[:, :, TB]
        op("dve", lambda e: e.tensor_tensor(out=ctmp[:, 0, :], in0=cT, in1=itmp[:, :, 0], op=ALU.mult), reads=[B_s, B_wl], writes=[B_ctmp])
        op("dve", lambda e: e.tensor_tensor(out=ctmp[:, 1, :], in0=sT, in1=itmp[:, :, 1], op=ALU.mult), reads=[B_s, B_wl], writes=[B_ctmp])
        op("dve", lambda e: e.tensor_tensor(out=init[:, :, 0], in0=ctmp[:, 0, :], in1=ctmp[:, 1, :], op=ALU.subtract), reads=[B_ctmp] + B_init, writes=B_init)
        op("dve", lambda e: e.tensor_tensor(out=ctmp[:, 2, :], in0=cT, in1=itmp[:, :, 1], op=ALU.mult), reads=[B_s, B_wl], writes=[B_ctmp])
        op("dve", lambda e: e.tensor_tensor(out=ctmp[:, 3, :], in0=sT, in1=itmp[:, :, 0], op=ALU.mult), reads=[B_s, B_wl], writes=[B_ctmp])
        op("dve", lambda e: e.tensor_tensor(out=init[:, :, 1], in0=ctmp[:, 2, :], in1=ctmp[:, 3, :], op=ALU.add), reads=[B_ctmp] + B_init, writes=B_init)
        yield

    def tail(bi):
        kind, blk = blocks[bi]
        if kind != "main":
            return
        par = bi % 2
        xt, b_xt = x_tok[bi % 3], B_x[bi % 3]
        outA, B_outA = outA2[par], B_outA2[par]
        gates, B_g = gates2[par], B_g2[par]
        u_f, B_u = u_f2[par], B_u2[par]
        for m in range(2):
            op("dve", lambda e, m=m: e.scalar_tensor_tensor(out=y_sb[:, m, :], in0=u_f[:, m, :], scalar=colp_sb[:, 42 + m:43 + m], in1=ps_acc[m][:, 0:TB],
                                                            op0=ALU.mult, op1=ALU.add), reads=[B_u, B_c, B_yacc[m]], writes=[B_y[m]])
            if debug:
                dma("sp", lambda e, m=m: e.dma_start(out=dbg_d[:, m * S_half + blk * TB:m * S_half + (blk + 1) * TB], in_=y_sb[:, m, :]), reads=[B_y[m]])
            op("act", lambda e, m=m: e.activation(out=z_f[:, m, :], in_=y_sb[:, m, :], func=AF.Gelu), reads=[B_y[m]], writes=[B_y[m]])
            op("pool", lambda e, m=m: e.tensor_copy(out=z_b[:, m, :], in_=z_f[:, m, :]), reads=[B_y[m]], writes=[B_y[m]])
            yield
        for mo in range(2):
            ps_g2, b_g2 = next_ps()
            mm_group(ps_g2[:, 0:TB], b_g2, [(w_glu_sb[:, mi, mo * 128:(mo + 1) * 128], z_b[:, mi, :]) for mi in range(2)], [B_w, B_y[0], B_y[1]])
            op("act", lambda e, mo=mo, ps_g2=ps_g2: e.activation(out=sg[:, mo, :], in_=ps_g2[:, 0:TB], func=AF.Sigmoid,
                                                                 bias=colp_sb[:, 40 + mo:41 + mo], scale=1.0), reads=[b_g2, B_c], writes=[B_y[mo]])
            op("pool", lambda e, mo=mo: e.tensor_tensor(out=outB[:, mo, :], in0=z_f[:, mo, :], in1=sg[:, mo, :], op=ALU.mult), reads=[B_y[mo]], writes=[B_outB])
            yield
        for dm in range(8):
            j = 0
            ps_a, b_a = next_ps()
            mm_group(ps_a[:, 0:TB], b_a, [(w_a_sb[:, c, dm * 128:(dm + 1) * 128], outA[:, c, :]) for c in range(6)], [B_w, B_outA])
            mm_group(ps_a[:, TB:2 * TB], b_a, [(w_b_sb[:, mi, dm * 128:(dm + 1) * 128], outB[:, mi, :]) for mi in range(2)], [B_w, B_outB])
            op("dve", lambda e, dm=dm, j=j, ps_a=ps_a: e.tensor_tensor(out=m1[:, j, :], in0=gates[:, dm, :], in1=ps_a[:, 0:TB], op=ALU.mult), reads=[B_g, b_a], writes=[B_m[j]])
            op("dve", lambda e, dm=dm, j=j, ps_a=ps_a: e.tensor_tensor(out=m2[:, j, :], in0=gates[:, 8 + dm, :], in1=ps_a[:, TB:2 * TB], op=ALU.mult), reads=[B_g, b_a], writes=[B_m[j]])
            op("pool", lambda e, dm=dm, j=j: e.tensor_tensor(out=merged[:, dm, :], in0=m1[:, j, :], in1=m2[:, j, :], op=ALU.add), reads=[B_m[j]], writes=[B_mg])
            yield
        for hf in range(2):
            ps_o, b_o = next_ps()
            mm_group(ps_o[:, :], b_o, [(merged[:, k, :], w_out_sb[:, k, hf * 512:(hf + 1) * 512]) for k in range(8)], [B_w, B_mg])
            op("dve", lambda e, hf=hf, ps_o=ps_o: e.tensor_tensor(out=xt[:, hf * 512:(hf + 1) * 512], in0=xt[:, hf * 512:(hf + 1) * 512],
                                                                  in1=ps_o[:, :], op=ALU.add), reads=[b_xt, b_o], writes=[b_xt])
            yield
        dma("sp", lambda e: e.dma_start(out=x1_d[blk * TB:(blk + 1) * TB, :], in_=xt[:]), reads=[b_xt], writes=[B_x1d])

    def interleave(gens):
        gens = [g for g in gens if g is not None]
        while gens:
            alive = []
            for g in gens:
                try:
                    next(g)
                    alive.append(g)
                except StopIteration:
                    pass
            gens = alive

    Apre = Alloc(nc)
    u_tok2 = [Apre.sb("u_tok%d" % i, [128, 256], BF16) for i in range(2)]
    B_ut = [Buf("ut0"), Buf("ut1")]
    pt_t = [Apre.sb("pt_t%d" % i, [128, 4, 64]) for i in range(4)]
    B_ptt = [Buf("ptt%d" % i) for i in range(4)]
    Uc = Apre.sb("Uc", [128, 8, 128])
    Us = Apre.sb("Us", [128, 8, 128])
    ta = Apre.sb("ta", [128, 8, 64])
    tb_ = Apre.sb("tb_", [128, 8, 64])
    ones2 = Apre.sb("ones2", [128, 2])
    S_sb = Apre.sb("S_sb", [128, 2, G])
    tq = Apre.sb("tq", [128, 2, G])
    B_fin = Buf("fin")
    B_M = Buf("Mst")
    zbanks = [ps_acc[0], ps_acc[1], ps_mm[3], ps_mm[4]]
    B_zb = [Buf("zb%d" % i) for i in range(4)]
    ps_pool[:] = [0, 1, 2]
    op("pool", lambda e: e.memset(Mst[:], 0.0), writes=[B_M])
    op("pool", lambda e: e.memset(ones2[:], 1.0), writes=[B_fin])
    def pre_B(j):
        par = j % 2
        ps_u, b_u = next_ps()
        mm_group(ps_u[:, 0:256], b_u, [(hT2[par][:, k, :], w_in_sb[:, k, 2304:2560]) for k in range(8)], [B_w, B_hT2[par]])
        op("act", lambda e, par=par, ps_u=ps_u: e.activation(out=u_tok2[par][:], in_=ps_u[:, 0:256], func=AF.Copy), reads=[b_u], writes=[B_ut[par]])
        if j == NB - 1:
            for _ in convgate(j):
                pass

    def pre_C(j):
        par = j % 2
        l = (LP - 1) - (j % LP)
        for zi in range(4):
            m = zi // 2
            op("pe", lambda e, zi=zi, m=m, l=l, par=par, j=j: e.matmul(zbanks[zi][:, :], lhsT=u_tok2[par][:, m * 128:(m + 1) * 128],
                                                                   rhs=PT[:, l, 4 * zi:4 * zi + 4, :].rearrange("p g c -> p (g c)"),
                                                                   start=(j % LP == 0), stop=(j % LP == LP - 1)),
               reads=[B_ut[par], B_s], writes=[B_zb[zi]])
        if j % LP == LP - 1:
            for zi in range(4):
                gsl = slice(4 * zi, 4 * zi + 4)
                Mre, Mim = Mst[:, gsl, 0:64], Mst[:, gsl, 64:128]
                cr = CRI[:, 0, 256 * zi:256 * (zi + 1)].rearrange("p (g s) -> p g s", g=4)
                ci = CRI[:, 1, 256 * zi:256 * (zi + 1)].rearrange("p (g s) -> p g s", g=4)
                Z3 = zbanks[zi][:, :].rearrange("p (g c) -> p g c", g=4)
                Zre, Zim = Z3[:, :, 0:64], Z3[:, :, 64:128]
                t1, t2, t3, t4 = [t[:] for t in pt_t]
                b1, b2, b3, b4 = B_ptt
                op("pool", lambda e, cr=cr, Mre=Mre, t1=t1: e.tensor_tensor(out=t1, in0=cr, in1=Mre, op=ALU.mult), reads=[B_s, B_M], writes=[b1])
                op("pool", lambda e, ci=ci, Mim=Mim, t2=t2: e.tensor_tensor(out=t2, in0=ci, in1=Mim, op=ALU.mult), reads=[B_s, B_M], writes=[b2])
                op("pool", lambda e, cr=cr, Mim=Mim, t3=t3: e.tensor_tensor(out=t3, in0=cr, in1=Mim, op=ALU.mult), reads=[B_s, B_M], writes=[b3])
                op("pool", lambda e, ci=ci, Mre=Mre, t4=t4: e.tensor_tensor(out=t4, in0=ci, in1=Mre, op=ALU.mult), reads=[B_s, B_M], writes=[b4])
                op("pool", lambda e, t1=t1, t2=t2: e.tensor_tensor(out=t1, in0=t1, in1=t2, op=ALU.subtract), reads=[b1, b2], writes=[b1])
                op("pool", lambda e, t3=t3, t4=t4: e.tensor_tensor(out=t3, in0=t3, in1=t4, op=ALU.add), reads=[b3, b4], writes=[b3])
                op("dve", lambda e, Mre=Mre, t1=t1, Zre=Zre: e.tensor_tensor(out=Mre, in0=t1, in1=Zre, op=ALU.add), reads=[b1, B_zb[zi]], writes=[B_M])
                op("dve", lambda e, Mim=Mim, t3=t3, Zim=Zim: e.tensor_tensor(out=Mim, in0=t3, in1=Zim, op=ALU.add), reads=[b3, B_zb[zi]], writes=[B_M])

    for k in range(NB + 2):
        if k < NB:
            load_norm_T(k)
        if 0 <= k - 1 < NB:
            pre_B(k - 1)
        if 0 <= k - 2 < NB:
            pre_C(k - 2)
    ps_S, b_S = next_ps()
    for m in range(2):
        gsl = slice(8 * m, 8 * m + 8)
        Bre, Bim = bpq[:, gsl, 0, 0:64], bpq[:, gsl, 0, 64:128]
        Mre, Mim = Mst[:, gsl, 0:64], Mst[:, gsl, 64:128]
        op("dve", lambda e, Bre=Bre, Mre=Mre: e.tensor_tensor(out=ta[:], in0=Bre, in1=Mre, op=ALU.mult), reads=[B_s, B_M], writes=[B_fin])
        op("dve", lambda e, Bim=Bim, Mim=Mim: e.tensor_tensor(out=tb_[:], in0=Bim, in1=Mim, op=ALU.mult), reads=[B_s, B_M], writes=[B_fin])
        op("dve", lambda e: e.tensor_tensor(out=Uc[:, :, 0:64], in0=ta[:], in1=tb_[:], op=ALU.subtract), reads=[B_fin], writes=[B_fin])
        op("dve", lambda e, Bre=Bre, Mim=Mim: e.tensor_tensor(out=ta[:], in0=Bre, in1=Mim, op=ALU.mult), reads=[B_s, B_M, B_fin], writes=[B_fin])
        op("dve", lambda e, Bim=Bim, Mre=Mre: e.tensor_tensor(out=tb_[:], in0=Bim, in1=Mre, op=ALU.mult), reads=[B_s, B_M, B_fin], writes=[B_fin])
        op("dve", lambda e: e.tensor_tensor(out=Uc[:, :, 64:128], in0=ta[:], in1=tb_[:], op=ALU.add), reads=[B_fin], writes=[B_fin])
        op("dve", lambda e: e.tensor_copy(out=Us[:, :, 0:64], in_=Uc[:, :, 64:128]), reads=[B_fin], writes=[B_fin])
        op("dve", lambda e: e.tensor_scalar(out=Us[:, :, 64:128], in0=Uc[:, :, 0:64], scalar1=-1.0, scalar2=None, op0=ALU.mult), reads=[B_fin], writes=[B_fin])
        for gl_ in range(8):
            g = 8 * m + gl_
            op("pe", lambda e, gl_=gl_, g=g: e.matmul(ps_S[:, 2 * g:2 * g + 2], lhsT=Uc[:, gl_, :], rhs=ones2[:, :], start=True, stop=True), reads=[B_fin], writes=[b_S])
            op("pe", lambda e, gl_=gl_, g=g: e.matmul(ps_S[:, 64 + 2 * g:64 + 2 * g + 2], lhsT=Us[:, gl_, :], rhs=ones2[:, :], start=True, stop=True), reads=[B_fin], writes=[b_S])
    for a in range(2):
        op("act", lambda e, a=a: e.activation(out=S_sb[:, a, :], in_=ps_S[:, 64 * a:64 * a + 32].rearrange("p (g t) -> p g t", t=2)[:, :, 0], func=AF.Copy),
           reads=[b_S, B_fin], writes=[B_fin])
    c1, s1 = ctab[:, :, 1], stab[:, :, 1]
    op("dve", lambda e: e.tensor_tensor(out=tq[:, 0, :], in0=c1, in1=S_sb[:, 0, :], op=ALU.mult), reads=[B_s, B_fin], writes=[B_fin])
    op("dve", lambda e: e.tensor_tensor(out=tq[:, 1, :], in0=s1, in1=S_sb[:, 1, :], op=ALU.mult), reads=[B_s, B_fin], writes=[B_fin])
    op("dve", lambda e: e.tensor_tensor(out=init[:, :, 0], in0=tq[:, 0, :], in1=tq[:, 1, :], op=ALU.subtract), reads=[B_fin] + B_init, writes=B_init)
    op("dve", lambda e: e.tensor_tensor(out=tq[:, 0, :], in0=c1, in1=S_sb[:, 1, :], op=ALU.mult), reads=[B_s, B_fin] + B_init, writes=[B_fin])
    op("dve", lambda e: e.tensor_tensor(out=tq[:, 1, :], in0=s1, in1=S_sb[:, 0, :], op=ALU.mult), reads=[B_s, B_fin], writes=[B_fin])
    op("dve", lambda e: e.tensor_tensor(out=init[:, :, 1], in0=tq[:, 0, :], in1=tq[:, 1, :], op=ALU.add), reads=[B_fin] + B_init, writes=B_init)
    S.barrier()
    Apre.close()
    AXr.close()
    ps_pool[:] = [0, 1, 2]

    MA.update(alloc_main())
    ct1, ct2, outA2, uT2, u_f2, gates2, st, s_bf, itmp, ctmp = [MA[k] for k in ("ct1", "ct2", "outA2", "uT2", "u_f2", "gates2", "st", "s_bf", "itmp", "ctmp")]
    y_sb, z_f, z_b, sg, outB, m1, m2, merged = [MA[k] for k in ("y_sb", "z_f", "z_b", "sg", "outB", "m1", "m2", "merged")]
    nblk = len(blocks)
    for _ in front(NB):
        pass
    for bi in range(NB, nblk + 1):
        gl = []
        if bi < nblk:
            gl.append(ssm(bi))
            gl.append(convgate(bi))
        if bi - 1 >= NB:
            gl.append(tail(bi - 1))
        if bi + 1 < nblk:
            gl.append(delayed(front(bi + 1), 12))
        if bi < nblk:
            gl.append(precast_gen(n_pre_per_blk))
        interleave(gl)

    S.barrier()
    A.close()
    ps_pool[:] = list(range(NPS))
    if do_b:
        Bq = Alloc(nc)
        sb = Bq.sb
        NT = S_half // 128
        w_q_sb = sb("w_q_sb", [128, 8, NQ], BF16)
        keys_sb = sb("keys_sb", [128, 2, 128], BF16)
        g2_sb = sb("g2_sb", [128, 2, D])
        B_wq = Buf("wq")
        for kc in range(8):
            dma("pool", lambda e, kc=kc: e.dma_start(out=w_q_sb[:, kc, :], in_=w_q[kc * 128:(kc + 1) * 128, :]), writes=[B_wq])
        for c in range(2):
            dma("pool", lambda e, c=c: e.dma_start(out=keys_sb[:, c, :], in_=keysT[c]), writes=[B_wq])
            dma("sp", lambda e, c=c: e.dma_start(out=g2_sb[:, c, :], in_=g_rep[1 + c]), writes=[B_wq])
        iota4 = sb("iota4", [128, 8, 16, 16])
        op("pool", lambda e: e.iota(iota4[:].rearrange("p h k a -> p (h k) a"), [[0, 128], [1, 16]], base=0, channel_multiplier=0,
                                    allow_small_or_imprecise_dtypes=True), writes=[B_wq])
        x1t = [sb("x1t%d" % i, [128, D]) for i in range(2)]
        B_x1 = [Buf("x1t0"), Buf("x1t1")]
        xn_f2 = [sb("xn_f%d" % i, [128, D]) for i in range(2)]
        B_xn2 = [Buf("xn0"), Buf("xn1")]
        xn_b = sb("xn_b", [128, D], BF16)
        B_xnb = Buf("xn_b")
        xnT = sb("xnT", [128, 8, 128], BF16)
        B_xnT = Buf("xnT")
        qT = sb("qT", [128, 16, 128], BF16)
        B_qT = Buf("qT")
        s_sb = sb("s_sb", [128, 16, 128])
        B_ssb = Buf("s_sb")
        srep = sb("srep", [128, 128])
        v12 = sb("v12", [128, 8, 2, 16])
        i12u = sb("i12u", [128, 8, 2, 16], U32)
        i12f = sb("i12f", [128, 8, 2, 16])
        cand = sb("cand", [128, 8, 16, 16])
        crep = sb("crep", [128, 256])
        best = sb("best", [128, 8, 16])
        posu = sb("posu", [128, 8, 16], U32)
        pau = sb("pau", [128, 8, 16], U32)
        pbu = sb("pbu", [128, 8, 16], U32)
        paf = sb("paf", [128, 8, 16])
        pbf = sb("pbf", [128, 8, 16])
        eq = sb("eq", [128, 8, 16, 16])
        If = sb("If", [128, 8, 16])
        Jf = sb("Jf", [128, 8, 16])
        eg = sb("eg", [128, 8, 16])
        zs = sb("zs", [128, 8])
        eidx2 = [sb("eidx%d" % i, [128, 128], I32) for i in range(2)]
        gate2 = [sb("gate%d" % i, [128, 128]) for i in range(2)]
        B_eg2 = [Buf("eg0"), Buf("eg1")]
        hpre = sb("hpre", [128, 128])
        B_tk = Buf("topk")
        junk_b = sb("junk_b", [128, D], BF16)
        B_jf = Buf("junk_b")
        NGB = 20
        gbuf = [sb("gbuf%d" % i, [128, 2 * D], BF16) for i in range(NGB)]
        B_gb = [Buf("gb%d" % i) for i in range(NGB)]
        gb_rr = [0]
        NHC = 4
        B_hc = [Buf("hc%d" % i) for i in range(NHC)]
        wcol = sb("wcol", [128, 2, 128])
        NDG = 6
        diag = [sb("diag%d" % i, [128, 128], BF16) for i in range(NDG)]
        B_dg = [Buf("dg%d" % i) for i in range(NDG)]
        o_t = sb("o_t", [128, D])
        B_ot = Buf("o_t")

        def prep_gen(t):
            par = t % 2
            xt, b_xt = x1t[par], B_x1[par]
            xn_f, B_xn = xn_f2[par], B_xn2[par]
            eidx, gate, B_eg = eidx2[par], gate2[par], B_eg2[par]
            dma("sp", lambda e: e.dma_start(out=xt[:], in_=x1_d[t * 128:(t + 1) * 128, :]), reads=[B_x1d], writes=[b_xt])
            norm_tile(xt[:], b_xt, g2_sb[:, 0, :], B_wq, xn_f[:], B_xn, 0)
            op("act", lambda e: e.activation(out=xn_b[:], in_=xn_f[:], func=AF.Copy), reads=[B_xn], writes=[B_xnb])
            yield
            transposes(xn_b[:], B_xnb, xnT, B_xnT)
            yield
            pend = None
            for qt in range(16):
                ps_q, b_q = next_ps()
                mm_group(ps_q[:, 0:128], b_q, [(w_q_sb[:, k, qt * 128:(qt + 1) * 128], xnT[:, k, :]) for k in range(8)], [B_wq, B_xnT])
                if pend is not None:
                    pend()
                pend = (lambda qt=qt, ps_q=ps_q, b_q=b_q: op("act", lambda e: e.activation(out=qT[:, qt, :], in_=ps_q[:, 0:128], func=AF.Copy), reads=[b_q], writes=[B_qT]))
                yield
            pend()
            yield
            pend = None
            for bnk in range(4):
                ps_s, b_s = next_ps()
                for j in range(4):
                    idx = bnk * 4 + j
                    mm_group(ps_s[:, j * 128:(j + 1) * 128], b_s, [(qT[:, idx, :], keys_sb[:, idx % 2, :])], [B_qT, B_wq])
                if pend is not None:
                    pend()
                pend = (lambda bnk=bnk, ps_s=ps_s, b_s=b_s: op("act", lambda e: e.activation(out=s_sb[:, bnk * 4:(bnk + 1) * 4, :].rearrange("p a n -> p (a n)"), in_=ps_s[:, :], func=AF.Copy),
                                                                 reads=[b_s], writes=[B_ssb]))
                yield
            pend()
            yield
            for h in range(8):
                for c in range(2):
                    sv = s_sb[:, 2 * h + c, :]
                    op("dve", lambda e, h=h, c=c, sv=sv: e.max(out=v12[:, h, c, 0:8], in_=sv), reads=[B_ssb], writes=[B_tk])
                    op("dve", lambda e, h=h, c=c, sv=sv: e.match_replace(out=srep[:], in_to_replace=v12[:, h, c, 0:8], in_values=sv, imm_value=-1e30), reads=[B_ssb, B_tk], writes=[B_tk])
                    yield
                    op("dve", lambda e, h=h, c=c: e.max(out=v12[:, h, c, 8:16], in_=srep[:]), reads=[B_tk], writes=[B_tk])
                    op("dve", lambda e, h=h, c=c, sv=sv: e.max_index(out=i12u[:, h, c, 0:8], in_max=v12[:, h, c, 0:8], in_values=sv), reads=[B_ssb, B_tk], writes=[B_tk])
                    yield
                    op("dve", lambda e, h=h, c=c: e.max_index(out=i12u[:, h, c, 8:16], in_max=v12[:, h, c, 8:16], in_values=srep[:]), reads=[B_tk], writes=[B_tk])
                    yield
            op("dve", lambda e: e.tensor_copy(out=i12f[:], in_=i12u[:]), reads=[B_tk], writes=[B_tk])
            v1b = v12[:, :, 0, :].rearrange("p h (a o) -> p h a o", o=1).to_broadcast([128, 8, 16, 16])
            v2b = v12[:, :, 1, :].rearrange("p h (o b) -> p h o b", o=1).to_broadcast([128, 8, 16, 16])
            op("dve", lambda e: e.tensor_tensor(out=cand[:], in0=v1b, in1=v2b, op=ALU.add), reads=[B_tk], writes=[B_tk])
            yield
            for h in range(8):
                ch = cand[:, h, :, :].rearrange("p a b -> p (a b)")
                op("dve", lambda e, h=h, ch=ch: e.max(out=best[:, h, 0:8], in_=ch), reads=[B_tk], writes=[B_tk])
                op("dve", lambda e, h=h, ch=ch: e.match_replace(out=crep[:], in_to_replace=best[:, h, 0:8], in_values=ch, imm_value=-1e30), reads=[B_tk], writes=[B_tk])
                yield
                op("dve", lambda e, h=h: e.max(out=best[:, h, 8:16], in_=crep[:]), reads=[B_tk], writes=[B_tk])
                op("dve", lambda e, h=h, ch=ch: e.max_index(out=posu[:, h, 0:8], in_max=best[:, h, 0:8], in_values=ch), reads=[B_tk], writes=[B_tk])
                yield
                op("dve", lambda e, h=h: e.max_index(out=posu[:, h, 8:16], in_max=best[:, h, 8:16], in_values=crep[:]), reads=[B_tk], writes=[B_tk])
                yield
            op("dve", lambda e: e.tensor_single_scalar(out=pau[:], in_=posu[:], scalar=4, op=ALU.logical_shift_right), reads=[B_tk], writes=[B_tk])
            op("dve", lambda e: e.tensor_single_scalar(out=pbu[:], in_=posu[:], scalar=15, op=ALU.bitwise_and), reads=[B_tk], writes=[B_tk])
            yield
            op("dve", lambda e: e.tensor_copy(out=paf[:], in_=pau[:]), reads=[B_tk], writes=[B_tk])
            op("dve", lambda e: e.tensor_copy(out=pbf[:], in_=pbu[:]), reads=[B_tk], writes=[B_tk])
            yield
            for (pf, c, dst) in ((paf, 0, If), (pbf, 1, Jf)):
                pfb = pf[:].rearrange("p h (k o) -> p h k o", o=1).to_broadcast([128, 8, 16, 16])
                ifb = i12f[:, :, c, :].rearrange("p h (o a) -> p h o a", o=1).to_broadcast([128, 8, 16, 16])
                op("dve", lambda e, pfb=pfb: e.tensor_tensor(out=eq[:], in0=pfb, in1=iota4[:], op=ALU.is_equal), reads=[B_tk, B_wq], writes=[B_tk])
                yield
                op("dve", lambda e, ifb=ifb: e.tensor_tensor(out=eq[:], in0=eq[:], in1=ifb, op=ALU.mult), reads=[B_tk], writes=[B_tk])
                yield
                op("dve", lambda e, dst=dst: e.tensor_reduce(out=dst[:].rearrange("p h k -> p (h k)"), in_=eq[:].rearrange("p h k a -> p (h k) a"), axis=AX.X, op=ALU.add),
                   reads=[B_tk], writes=[B_tk])
                yield
            op("dve", lambda e: e.scalar_tensor_tensor(out=paf[:], in0=If[:], scalar=128.0, in1=Jf[:], op0=ALU.mult, op1=ALU.add), reads=[B_tk], writes=[B_tk])
            op("dve", lambda e: e.tensor_copy(out=eidx[:], in_=paf[:].rearrange("p h k -> p (h k)")), reads=[B_tk, B_eg], writes=[B_eg])
            yield
            bmx = best[:, :, 0:1].to_broadcast([128, 8, 16])
            op("dve", lambda e: e.tensor_tensor(out=eg[:], in0=best[:], in1=bmx, op=ALU.subtract), reads=[B_tk], writes=[B_tk])
            op("act", lambda e: e.activation(out=eg[:], in_=eg[:], func=AF.Exp), reads=[B_tk], writes=[B_tk])
            op("dve", lambda e: e.tensor_reduce(out=zs[:], in_=eg[:], axis=AX.X, op=ALU.add), reads=[B_tk], writes=[B_tk])
            op("dve", lambda e: e.reciprocal(out=zs[:], in_=zs[:]), reads=[B_tk], writes=[B_tk])
            zb = zs[:].rearrange("p (h o) -> p h o", o=1).to_broadcast([128, 8, 16])
            op("dve", lambda e: e.tensor_tensor(out=gate[:].rearrange("p (h k) -> p h k", h=8), in0=eg[:], in1=zb, op=ALU.mult), reads=[B_tk, B_eg], writes=[B_eg])
            yield

        def run_gen(g, n=None):
            if g is None:
                return
            k = 0
            for _ in g:
                k += 1
                if n is not None and k >= n:
                    return

        gcur = prep_gen(0)
        run_gen(gcur)
        for t in range(NT):
            par = t % 2
            xt, b_xt = x1t[par], B_x1[par]
            xn_f, B_xn = xn_f2[par], B_xn2[par]
            eidx, gate, B_eg = eidx2[par], gate2[par], B_eg2[par]
            gnext = prep_gen(t + 1) if t + 1 < NT else None
            GS = 8
            slot_gb = {}
            for k in range(128 // GS + 1):
                if k < 128 // GS:
                    for sl in range(k * GS, (k + 1) * GS):
                        gi = gb_rr[0] % NGB
                        gb_rr[0] += 1
                        gb, b_gb = gbuf[gi], B_gb[gi]
                        slot_gb[sl] = (gb, b_gb)
                        b_hc = B_hc[k % NHC]
                        dma("pool", lambda e, gb=gb, sl=sl, eidx=eidx: e.indirect_dma_start(out=gb[:], out_offset=None, in_=uv_d[:, :],
                            in_offset=bass.IndirectOffsetOnAxis(ap=eidx[:, sl:sl + 1], axis=0)), reads=[B_eg, B_uv], writes=[b_gb])
                        op("dve", lambda e, gb=gb, sl=sl, xn_f=xn_f: e.scalar_tensor_tensor(out=junk_b[:], in0=gb[:, 0:D], scalar=1.0, in1=xn_f[:], op0=ALU.mult, op1=ALU.mult,
                                                                                            accum_out=hpre[:, sl:sl + 1]), reads=[b_gb, B_xn, b_hc], writes=[B_jf, b_hc])
                        run_gen(gnext, 1)
                if k >= 1:
                    kk = k - 1
                    b_hc = B_hc[kk % NHC]
                    cs = slice(kk * GS, (kk + 1) * GS)
                    op("act", lambda e, cs=cs: e.activation(out=wcol[:, 0, cs], in_=hpre[:, cs], func=AF.Gelu), reads=[b_hc], writes=[b_hc])
                    op("dve", lambda e, cs=cs, gate=gate: e.tensor_tensor(out=wcol[:, 1, cs], in0=wcol[:, 0, cs], in1=gate[:, cs], op=ALU.mult), reads=[b_hc, B_eg], writes=[b_hc])
                    for sl in range(kk * GS, (kk + 1) * GS):
                        gb, b_gb = slot_gb.pop(sl)
                        dg, b_dg = diag[sl % NDG], B_dg[sl % NDG]
                        op("act", lambda e, sl=sl, dg=dg: e.activation(out=dg[:], in_=ident[:], func=AF.Copy, scale=wcol[:, 1, sl:sl + 1]),
                           reads=[b_hc, B_id], writes=[b_dg])
                        for hf in range(2):
                            op("pe", lambda e, sl=sl, dg=dg, gb=gb, hf=hf: e.matmul(ps_acc[hf][:, :], lhsT=dg[:], rhs=gb[:, D + hf * 512:D + (hf + 1) * 512],
                                                                                 start=(sl == 0), stop=(sl == 127)), reads=[b_dg, b_gb], writes=[B_pacc])
            run_gen(gnext)
            for hf in range(2):
                op("dve", lambda e, hf=hf, xt=xt: e.tensor_tensor(out=xt[:, hf * 512:(hf + 1) * 512], in0=xt[:, hf * 512:(hf + 1) * 512], in1=ps_acc[hf][:, :], op=ALU.add),
                   reads=[b_xt, B_pacc], writes=[b_xt])
            norm_tile(xt[:], b_xt, g2_sb[:, 1, :], B_wq, o_t[:], B_ot, 4)
            dma("sp", lambda e, t=t: e.dma_start(out=out_d[t * 128:(t + 1) * 128, :], in_=o_t[:]), reads=[B_ot])
        S.barrier()
        Bq.close()
    P0.close()
    S.final_wait("sp")
    S.emit()
    S.close()
    return nc


def prep_shared(inp):
    f = np.float32
    sh = {}
    sh["w_in"] = np.ascontiguousarray(inp["w_in"][0], f)
    sh["w_a"] = np.ascontiguousarray(inp["w_branch_a"][0], f)
    sh["w_b"] = np.ascontiguousarray(inp["w_branch_b"][0], f)
    sh["w_out"] = np.ascontiguousarray(inp["w_out"][0], f)
    sh["w_glu"] = np.ascontiguousarray(inp["w_glu"][0], f)
    sh["w_q"] = np.ascontiguousarray(inp["w_q"][0], f)
    sh["keysT"] = np.ascontiguousarray(np.stack([inp["sub_keys_1"][0].T, inp["sub_keys_2"][0].T]), f)
    sh["g_rep"] = np.ascontiguousarray(np.stack([np.broadcast_to(inp["g_mix"][0], (128, D)),
                                                 np.broadcast_to(inp["g_ffn"][0], (128, D)),
                                                 np.broadcast_to(inp["g_final"], (128, D))]), f)
    colp = np.zeros((128, 48), f)
    colp[:, 0:16] = inp["b_gate"][0].reshape(16, 128).T
    cw = inp["conv_w"][0].reshape(3, 6, 128)
    colp[:, 16:34] = cw.transpose(2, 1, 0).reshape(128, 18)
    colp[:, 34:40] = inp["conv_b"][0].reshape(6, 128).T
    colp[:, 40:42] = inp["b_glu"][0].reshape(2, 128).T
    colp[:, 42:44] = inp["ssm_d"][0].reshape(2, 128).T
    sh["colp"] = colp
    lr, li, ld = inp["lam_re"][0], inp["lam_im"][0], inp["log_dt"][0]
    pt = np.zeros((3, 128, G), f)
    pt[0] = np.concatenate([lr.T, lr.T], 0)
    pt[1] = np.concatenate([li.T, li.T], 0)
    pt[2] = np.broadcast_to(ld[None, :], (128, G))
    sh["ssm_pt"] = pt
    row = np.zeros((3, 128, G * P), f)
    row[0] = np.broadcast_to(lr.reshape(1, -1), (128, G * P))
    row[1] = np.broadcast_to(li.reshape(1, -1), (128, G * P))
    row[2] = np.broadcast_to(np.repeat(ld, P).reshape(1, -1), (128, G * P))
    sh["ssm_row"] = row
    btm = np.zeros((2, 128, G, P), f)
    ctm = np.zeros((128, G, 128), f)
    for g in range(G):
        r0 = 16 * (g % 8)
        btm[0, r0:r0 + 16, g, :] = inp["ssm_b_re"][0][g].T
        btm[1, r0:r0 + 16, g, :] = inp["ssm_b_im"][0][g].T
        ctm[0:64, g, r0:r0 + 16] = inp["ssm_c_re"][0][g].T
        ctm[64:128, g, r0:r0 + 16] = inp["ssm_c_im"][0][g].T
    sh["bt_in"] = btm.reshape(2, 128, G * P)
    sh["ct_in"] = ctm.reshape(128, G * 128)
    sh["exp_u"] = np.ascontiguousarray(inp["expert_u"][0], f)
    sh["exp_v"] = np.ascontiguousarray(inp["expert_v"][0], f)
    return sh


def make_in_maps(inp, S_half, n_cores):
    sh = prep_shared(inp)
    x = np.asarray(inp["x"], np.float32)
    maps = []
    for c in range(n_cores):
        b, hf = c // 2, c % 2
        m = dict(sh)
        m["x_main"] = np.ascontiguousarray(x[b, hf * S_half:(hf + 1) * S_half])
        m["x_prev"] = np.ascontiguousarray(x[b, 0:S_half]) if hf == 1 else np.zeros((S_half, D), np.float32)
        maps.append(m)
    return maps


def kernel(**inputs):
    x = np.asarray(inputs["x"])
    bsz, seq, _ = x.shape
    S_half = seq // 2
    n_cores = 2 * bsz
    nc = build_nc(S_half)
    maps = make_in_maps(inputs, S_half, n_cores)
    res = run_bass_kernel_spmd(nc, maps, core_ids=list(range(n_cores)))
    out = np.zeros((bsz, seq, D), np.float32)
    for c in range(n_cores):
        b, hf = c // 2, c % 2
        out[b, hf * S_half:(hf + 1) * S_half] = res.results[c]["out"]
    return out
```

```python
import math
import numpy as np
import concourse.bass as bass
import concourse.mybir as mybir
from concourse.bass_utils import run_bass_kernel_spmd

F32 = mybir.dt.float32
BF16 = mybir.dt.bfloat16
U32 = mybir.dt.uint32
I32 = mybir.dt.int32
ALU = mybir.AluOpType
AF = mybir.ActivationFunctionType
AX = mybir.AxisListType

D = 1024
CONV_CH = 768
SSM_CH = 256
G = 16
P = 64
IN_COLS = 4608
NQ = 2048
NE = 16384
TB = 128
EPS = 1e-6
TWO_PI = 2.0 * math.pi
SKIP_PRECAST = False


class Buf:
    __slots__ = ("name", "w", "r")

    def __init__(self, name=""):
        self.name = name
        self.w = None
        self.r = []


class Sched:
    ENG = ("pe", "dve", "act", "pool", "sp")

    def __init__(self, nc, n_dma_sems=20):
        self.nc = nc
        self.stack = []
        self.streams = {e: [] for e in self.ENG}
        self.cnt = {e: 0 for e in self.ENG}
        self.sem = {}
        for e in self.ENG:
            g = nc.semaphore("s_" + e)
            self.sem[e] = g.__enter__()
            self.stack.append(g)
        self.known = {e: {} for e in self.ENG}
        self.dma_sems = {}
        for q in ("sp", "pool", "act"):
            self.dma_sems[q] = []
            for i in range({"sp": n_dma_sems, "pool": 48, "act": 8}[q]):
                g = nc.semaphore("d%s_%d" % (q, i))
                self.sem["d%s%d" % (q, i)] = g.__enter__()
                self.stack.append(g)
                self.dma_sems[q].append("d%s%d" % (q, i))
        self.dma_cnt = {k: 0 for q in self.dma_sems for k in self.dma_sems[q]}
        self.dma_rr = {"sp": 0, "pool": 0, "act": 0}
        self.n_inst = 0

    def _need(self, eng, deps):
        out = []
        for key, val in deps:
            if val <= 0:
                continue
            if self.known[eng].get(key, 0) >= val:
                continue
            self.known[eng][key] = val
            out.append((key, val))
        return out

    def _deps(self, reads, writes):
        deps = {}
        for b in reads:
            if b.w is not None:
                k, v = b.w
                deps[k] = max(deps.get(k, 0), v)
        for b in writes:
            if b.w is not None:
                k, v = b.w
                deps[k] = max(deps.get(k, 0), v)
            for k, v in b.r:
                deps[k] = max(deps.get(k, 0), v)
        return list(deps.items())

    def op(self, eng, fn, reads=(), writes=()):
        deps = self._deps(reads, writes)
        if eng == "pe":
            deps = [(k, v) for k, v in deps if k != "pe"]
        waits = self._need(eng, deps)
        self.cnt[eng] += 1
        n = self.cnt[eng]
        self.streams[eng].append((waits, fn, (eng, 1)))
        for b in reads:
            b.r.append((eng, n))
            if len(b.r) > 64:
                b.r = b.r[-64:] if False else self._compact(b.r)
        for b in writes:
            b.w = (eng, n)
            b.r = []
        self.n_inst += 1

    @staticmethod
    def _compact(r):
        m = {}
        for k, v in r:
            m[k] = max(m.get(k, 0), v)
        return list(m.items())

    def dma(self, queue, fn, reads=(), writes=()):
        deps = self._deps(reads, writes)
        pool_ = self.dma_sems[queue]
        key = pool_[self.dma_rr[queue] % len(pool_)]
        self.dma_rr[queue] += 1
        prev = self.dma_cnt[key]
        if prev > 0:
            deps.append((key, prev))
        waits = self._need(queue, deps)
        val = prev + 16
        self.dma_cnt[key] = val
        self.streams[queue].append((waits, fn, (key, 16)))
        for b in reads:
            b.r.append((key, val))
            if len(b.r) > 64:
                b.r = self._compact(b.r)
        for b in writes:
            b.w = (key, val)
            b.r = []
        self.n_inst += 1

    def barrier(self, dma=True):
        deps = [(e, self.cnt[e]) for e in self.ENG] + (list(self.dma_cnt.items()) if dma else [])
        for e in self.ENG:
            waits = self._need(e, [d for d in deps if d[0] != e])
            if waits:
                self.streams[e].append((waits, None, None))

    def final_wait(self, eng="sp"):
        deps = [(e, self.cnt[e]) for e in self.ENG if e != eng] + list(self.dma_cnt.items())
        waits = self._need(eng, deps)
        self.streams[eng].append((waits, None, None))

    def emit(self):
        nc = self.nc
        sems = self.sem
        streams = self.streams

        def run(engine, items):
            for waits, fn, inc in items:
                for key, val in waits:
                    engine.wait_ge(sems[key], val)
                if fn is not None:
                    fn(engine).then_inc(sems[inc[0]], inc[1])

        with nc.Block() as block:
            @block.tensor
            def _(e):
                run(e, streams["pe"])

            @block.vector
            def _(e):
                run(e, streams["dve"])

            @block.scalar
            def _(e):
                run(e, streams["act"])

            @block.gpsimd
            def _(e):
                run(e, streams["pool"])

            @block.sync
            def _(e):
                run(e, streams["sp"])

    def close(self):
        for g in reversed(self.stack):
            g.__exit__(None, None, None)


class Alloc:
    def __init__(self, nc):
        self.nc = nc
        self.stack = []

    def sb(self, name, shape, dt=F32, side=None):
        g = self.nc.sbuf_tensor(name, list(shape), dt, side=side) if side else self.nc.sbuf_tensor(name, list(shape), dt)
        t = g.__enter__()
        self.stack.append(g)
        return t

    def ps(self, name, shape, dt=F32):
        g = self.nc.psum_tensor(name, list(shape), dt)
        t = g.__enter__()
        self.stack.append(g)
        return t

    def close(self):
        for g in reversed(self.stack):
            g.__exit__(None, None, None)
        self.stack = []


def build_nc(S_half, debug=False, do_b=True):
    NB = S_half // TB
    nc = bass.Bass("TRN2", target_bir_lowering=False)

    def din(name, shape, dt=F32):
        return nc.dram_tensor(name, list(shape), dt, kind="ExternalInput").ap()

    x_main = din("x_main", [S_half, D])
    x_prev = din("x_prev", [S_half, D])
    w_in = din("w_in", [D, IN_COLS])
    w_a = din("w_a", [CONV_CH, D])
    w_b = din("w_b", [SSM_CH, D])
    w_out = din("w_out", [D, D])
    w_glu = din("w_glu", [SSM_CH, SSM_CH])
    w_q = din("w_q", [D, NQ])
    keysT = din("keysT", [2, 128, 128])
    g_rep = din("g_rep", [3, 128, D])
    colp = din("colp", [128, 48])
    ssm_pt = din("ssm_pt", [3, 128, G])
    ssm_row = din("ssm_row", [3, 128, G * P])
    bt_in = din("bt_in", [2, 128, G * P])
    ct_in = din("ct_in", [128, G * 128])
    exp_u = din("exp_u", [NE, D])
    exp_v = din("exp_v", [NE, D])
    out_d = nc.dram_tensor("out", [S_half, D], F32, kind="ExternalOutput").ap()
    if debug:
        x1_d = nc.dram_tensor("x1_d", [S_half, D], F32, kind="ExternalOutput").ap()
        dbg_d = nc.dram_tensor("dbg", [128, 2 * S_half], F32, kind="ExternalOutput").ap()
    else:
        x1_d = nc.dram_tensor("x1_d", [S_half, D], F32, kind="Internal").ap()
        dbg_d = None

    uv_d = nc.dram_tensor("uv_d", [NE, 2 * D], BF16, kind="Internal").ap()
    B_uv = Buf("uv_d")
    B_x1d = Buf("x1_d")

    S = Sched(nc)
    op, dma = S.op, S.dma

    P0 = Alloc(nc)
    identf = P0.sb("identf", [128, 128])
    ident = P0.sb("ident", [128, 128], BF16)
    junk = P0.sb("junk", [128, D], BF16)
    stat = P0.sb("stat", [128, 8])
    B_junk = Buf("junk")
    B_stat = Buf("stat")
    ps_tr = P0.ps("ps_tr", [128, 8, 128], BF16)
    B_ptr = Buf("ps_tr")
    NPS = 5
    ps_acc = [P0.ps("ps_acc%d" % i, [128, 512]) for i in range(2)]
    B_pacc = Buf("ps_acc")
    ps_mm = [P0.ps("ps_mm%d" % i, [128, 512]) for i in range(NPS)]
    B_ps = [Buf("ps%d" % i) for i in range(NPS)]
    ps_rr = [0]
    ps_pool = list(range(NPS))

    def next_ps():
        i = ps_pool[ps_rr[0] % len(ps_pool)]
        ps_rr[0] += 1
        return ps_mm[i], B_ps[i]

    def mm_group(out_ap, b_out, pairs, reads):
        n = len(pairs)
        for i, (l, r) in enumerate(pairs):
            op("pe", lambda e, l=l, r=r, i=i: e.matmul(out_ap, lhsT=l, rhs=r, start=(i == 0), stop=(i == n - 1)),
               reads=reads, writes=[b_out])

    def rstd_of(src, b_src, col):
        op("act", lambda e: e.activation(out=junk[:], in_=src, func=AF.Square, accum_out=stat[:, col:col + 1]),
           reads=[b_src, B_stat], writes=[B_junk, B_stat])
        op("dve", lambda e: e.tensor_scalar(out=stat[:, col + 1:col + 2], in0=stat[:, col:col + 1], scalar1=1.0 / D, scalar2=EPS,
                                            op0=ALU.mult, op1=ALU.add), reads=[B_stat], writes=[B_stat])
        op("act", lambda e: e.activation(out=stat[:, col + 2:col + 3], in_=stat[:, col + 1:col + 2], func=AF.Sqrt),
           reads=[B_stat], writes=[B_stat])
        op("dve", lambda e: e.reciprocal(out=stat[:, col + 3:col + 4], in_=stat[:, col + 2:col + 3]), reads=[B_stat], writes=[B_stat])

    def norm_tile(src, b_src, g_ap, b_gconst, dst, b_dst, col):
        rstd_of(src, b_src, col)
        op("dve", lambda e: e.scalar_tensor_tensor(out=dst, in0=src, scalar=stat[:, col + 3:col + 4], in1=g_ap,
                                                   op0=ALU.mult, op1=ALU.mult),
           reads=[b_src, B_stat, b_gconst], writes=[b_dst])

    def transposes(src_bf, b_src, dstT, b_dstT):
        for k in range(8):
            op("pe", lambda e, k=k: e.transpose(out=ps_tr[:, k, :], in_=src_bf[:, k * 128:(k + 1) * 128], identity=ident[:]),
               reads=[b_src, B_id], writes=[B_ptr])
        op("act", lambda e: e.activation(out=dstT[:, :, :], in_=ps_tr[:], func=AF.Copy), reads=[B_ptr], writes=[b_dstT])

    A = Alloc(nc)
    sb = A.sb
    w_in_sb = sb("w_in_sb", [128, 8, IN_COLS], BF16)
    w_a_sb = sb("w_a_sb", [128, 6, D], BF16)
    w_b_sb = sb("w_b_sb", [128, 2, D], BF16)
    w_out_sb = sb("w_out_sb", [128, 8, D], BF16)
    w_glu_sb = sb("w_glu_sb", [128, 2, SSM_CH], BF16)
    B_w = Buf("weights")

    def load_weights():
        for kc in range(8):
            for c in range(3):
                dma("pool", lambda e, kc=kc, c=c: e.dma_start(
                    out=w_in_sb[:, kc, c * 1536:(c + 1) * 1536],
                    in_=w_in[kc * 128:(kc + 1) * 128, c * 1536:(c + 1) * 1536]), writes=[B_w])
        for kc in range(6):
            dma("pool", lambda e, kc=kc: e.dma_start(out=w_a_sb[:, kc, :], in_=w_a[kc * 128:(kc + 1) * 128, :]), writes=[B_w])
        for kc in range(2):
            dma("pool", lambda e, kc=kc: e.dma_start(out=w_b_sb[:, kc, :], in_=w_b[kc * 128:(kc + 1) * 128, :]), writes=[B_w])
            dma("pool", lambda e, kc=kc: e.dma_start(out=w_glu_sb[:, kc, :], in_=w_glu[kc * 128:(kc + 1) * 128, :]), writes=[B_w])
        for kc in range(8):
            dma("pool", lambda e, kc=kc: e.dma_start(out=w_out_sb[:, kc, :], in_=w_out[kc * 128:(kc + 1) * 128, :]), writes=[B_w])


    g_sb = sb("g_sb", [128, D])
    colp_sb = sb("colp_sb", [128, 48])
    B_c = Buf("consts")
    dma("sp", lambda e: e.dma_start(out=g_sb[:], in_=g_rep[0]), writes=[B_c])
    dma("sp", lambda e: e.dma_start(out=colp_sb[:], in_=colp[:, :]), writes=[B_c])
    CW = lambda c, k: colp_sb[:, 16 + c * 3 + k:17 + c * 3 + k]
    CBIAS = lambda c: colp_sb[:, 34 + c:35 + c]

    B_id = Buf("ident")
    op("pool", lambda e: e.iota(identf[:], [[1, 128]], base=0, channel_multiplier=-1,
                                allow_small_or_imprecise_dtypes=True), writes=[B_id])
    op("dve", lambda e: e.tensor_single_scalar(out=ident[:], in_=identf[:], scalar=0.0, op=ALU.is_equal),
       reads=[B_id], writes=[B_id])

    r_p = sb("r_p", [128, G])
    ctab = sb("ctab", [128, G, TB + 1])
    stab = sb("stab", [128, G, TB + 1])
    nsT = sb("nsT", [128, G])
    bpq = sb("bpq", [128, G, 2, 128], BF16)
    ctb = sb("ctb", [128, G, 128], BF16)
    B_s = Buf("ssmc")
    LP = 2 if NB % 2 == 0 else 1
    AXr = Alloc(nc)
    PT = AXr.sb("PT", [128, LP, G, 128], BF16, side="right")
    CRI = AXr.sb("CRI", [128, 2, G * P], F32, side="right")
    Mst = AXr.sb("Mst", [128, G, 128], F32, side="right")
    A00 = Alloc(nc)
    ctf = A00.sb("ctf", [128, G * 128])
    dma("sp", lambda e: e.dma_start(out=ctf[:], in_=ct_in[:, :]), writes=[B_s])
    op("dve", lambda e: e.tensor_scalar(out=ctf[64:128, :], in0=ctf[64:128, :], scalar1=-1.0, scalar2=None, op0=ALU.mult), reads=[B_s], writes=[B_s])
    op("dve", lambda e: e.tensor_copy(out=ctb[:].rearrange("p g m -> p (g m)"), in_=ctf[:]), reads=[B_s], writes=[B_s])
    S.barrier(dma=False)
    A00.close()
    A0 = Alloc(nc)
    sb0 = A0.sb
    spt = sb0("spt", [128, 3, G])
    HW = 8 * P
    srow = sb0("srow", [128, 3, HW])
    bt = sb0("bt", [128, 2, HW])
    ecol = sb0("ecol", [128, 4])
    dma("sp", lambda e: e.dma_start(out=spt[:], in_=ssm_pt.rearrange("a p g -> p a g")), writes=[B_s])
    for l in range(LP):
        op("pool", lambda e, l=l: e.iota(ecol[:, l:l + 1], [[0, 1]], base=128 * l + 127, channel_multiplier=-1,
                                         allow_small_or_imprecise_dtypes=True), writes=[B_s])

    thp_p = sb0("thp_p", [128, G])
    tmp_p = sb0("tmp_p", [128, 4, G])
    iota_t = sb0("iota_t", [128, TB + 1])
    ytmp = sb0("ytmp", [128, 3, TB + 1])
    yti = sb0("yti", [128, TB + 1], I32)
    rti = sb0("rti", [128, HW], I32)
    rt2 = sb0("rt2", [128, HW])
    R = [sb0("rw%d" % i, [128, HW]) for i in range(8)]

    def expm1_poly(eng, out, z, t):
        op(eng, lambda e: e.tensor_scalar(out=out, in0=z, scalar1=1.0 / 720, scalar2=1.0 / 120, op0=ALU.mult, op1=ALU.add), reads=[B_s], writes=[B_s])
        for c in (1.0 / 24, 1.0 / 6, 0.5, 1.0):
            op(eng, lambda e: e.tensor_tensor(out=t, in0=out, in1=z, op=ALU.mult), reads=[B_s], writes=[B_s])
            op(eng, lambda e, c=c: e.tensor_scalar(out=out, in0=t, scalar1=c, scalar2=None, op0=ALU.add), reads=[B_s], writes=[B_s])
        op(eng, lambda e: e.tensor_tensor(out=t, in0=out, in1=z, op=ALU.mult), reads=[B_s], writes=[B_s])
        op(eng, lambda e: e.tensor_copy(out=out, in_=t), reads=[B_s], writes=[B_s])

    def sin_turns(out, y, off, t, ti, t2):
        op("dve", lambda e: e.tensor_scalar(out=t, in0=y, scalar1=8.0 + off, scalar2=None, op0=ALU.add), reads=[B_s], writes=[B_s])
        op("dve", lambda e: e.tensor_copy(out=ti, in_=t), reads=[B_s], writes=[B_s])
        op("dve", lambda e: e.tensor_copy(out=t2, in_=ti), reads=[B_s], writes=[B_s])
        op("dve", lambda e: e.tensor_tensor(out=t, in0=t, in1=t2, op=ALU.subtract), reads=[B_s], writes=[B_s])
        op("dve", lambda e: e.tensor_single_scalar(out=t2, in_=t, scalar=0.5, op=ALU.is_gt), reads=[B_s], writes=[B_s])
        op("dve", lambda e: e.tensor_tensor(out=t, in0=t, in1=t2, op=ALU.subtract), reads=[B_s], writes=[B_s])
        op("act", lambda e: e.activation(out=out, in_=t, func=AF.Sin, scale=TWO_PI), reads=[B_s], writes=[B_s])

    dt_p = tmp_p[:, 0, :]
    z_p = tmp_p[:, 1, :]
    e_p = tmp_p[:, 2, :]
    t_p = tmp_p[:, 3, :]
    op("act", lambda e: e.activation(out=dt_p, in_=spt[:, 2, :], func=AF.Exp), reads=[B_s], writes=[B_s])
    op("dve", lambda e: e.tensor_tensor(out=z_p, in0=spt[:, 0, :], in1=dt_p, op=ALU.mult), reads=[B_s], writes=[B_s])
    expm1_poly("dve", e_p, z_p, t_p)
    op("dve", lambda e: e.tensor_scalar(out=r_p[:], in0=e_p, scalar1=1.0, scalar2=None, op0=ALU.add), reads=[B_s], writes=[B_s])
    op("dve", lambda e: e.tensor_tensor(out=t_p, in0=spt[:, 1, :], in1=dt_p, op=ALU.mult), reads=[B_s], writes=[B_s])
    op("dve", lambda e: e.tensor_scalar(out=thp_p[:], in0=t_p, scalar1=1.0 / TWO_PI, scalar2=None, op0=ALU.mult), reads=[B_s], writes=[B_s])
    op("pool", lambda e: e.iota(iota_t[:], [[1, TB + 1]], base=0, channel_multiplier=0,
                                allow_small_or_imprecise_dtypes=True), writes=[B_s])
    load_weights()
    for g in range(G):
        op("dve", lambda e, g=g: e.tensor_scalar(out=ytmp[:, 0, :], in0=iota_t[:], scalar1=thp_p[:, g:g + 1], scalar2=None, op0=ALU.mult), reads=[B_s], writes=[B_s])
        sin_turns(stab[:, g, :], ytmp[:, 0, :], 0.0, ytmp[:, 1, :], yti[:], ytmp[:, 2, :])
        sin_turns(ctab[:, g, :], ytmp[:, 0, :], 0.25, ytmp[:, 1, :], yti[:], ytmp[:, 2, :])
    op("dve", lambda e: e.tensor_scalar(out=nsT[:], in0=stab[:, :, TB], scalar1=-1.0, scalar2=None, op0=ALU.mult), reads=[B_s], writes=[B_s])

    for hh in range(2):
        gs = slice(8 * hh, 8 * hh + 8)
        for a in range(3):
            dma("sp", lambda e, a=a, hh=hh: e.dma_start(out=srow[:, a, :], in_=ssm_row[a][:, hh * HW:(hh + 1) * HW]), reads=[B_s], writes=[B_s])
        for a in range(2):
            dma("sp", lambda e, a=a, hh=hh: e.dma_start(out=bt[:, a, :], in_=bt_in[a][:, hh * HW:(hh + 1) * HW]), reads=[B_s], writes=[B_s])
        lr_r, li_r = srow[:, 0, :], srow[:, 1, :]
        dt_r, z_r, em1_r, t_r, thp_r, sn_r, cs_r, sh_r = [R[i][:] for i in range(8)]
        op("act", lambda e: e.activation(out=dt_r, in_=srow[:, 2, :], func=AF.Exp), reads=[B_s], writes=[B_s])
        op("dve", lambda e: e.tensor_tensor(out=z_r, in0=lr_r, in1=dt_r, op=ALU.mult), reads=[B_s], writes=[B_s])
        expm1_poly("dve", em1_r, z_r, t_r)
        op("dve", lambda e: e.tensor_tensor(out=t_r, in0=li_r, in1=dt_r, op=ALU.mult), reads=[B_s], writes=[B_s])
        op("dve", lambda e: e.tensor_scalar(out=thp_r, in0=t_r, scalar1=1.0 / TWO_PI, scalar2=None, op0=ALU.mult), reads=[B_s], writes=[B_s])
        y_r, mg_r = R[5][:], R[0][:]
        for l in range(LP):
            op("dve", lambda e, l=l: e.tensor_scalar(out=y_r, in0=thp_r, scalar1=ecol[:, l:l + 1], scalar2=None, op0=ALU.mult), reads=[B_s], writes=[B_s])
            sin_turns(R[7][:], y_r, 0.0, t_r, rti[:], rt2[:])
            sin_turns(R[6][:], y_r, 0.25, t_r, rti[:], rt2[:])
            op("act", lambda e, l=l: e.activation(out=mg_r, in_=z_r, func=AF.Exp, scale=ecol[:, l:l + 1]), reads=[B_s], writes=[B_s])
            op("dve", lambda e, l=l, gs=gs: e.tensor_tensor(out=PT[:, l, gs, 0:64], in0=mg_r.rearrange("p (g s) -> p g s", g=8), in1=R[6][:].rearrange("p (g s) -> p g s", g=8), op=ALU.mult), reads=[B_s], writes=[B_s])
            op("dve", lambda e, l=l, gs=gs: e.tensor_tensor(out=PT[:, l, gs, 64:128], in0=mg_r.rearrange("p (g s) -> p g s", g=8), in1=R[7][:].rearrange("p (g s) -> p g s", g=8), op=ALU.mult), reads=[B_s], writes=[B_s])
        op("dve", lambda e: e.tensor_scalar(out=y_r, in0=thp_r, scalar1=float(128 * LP), scalar2=None, op0=ALU.mult), reads=[B_s], writes=[B_s])
        sin_turns(R[7][:], y_r, 0.0, t_r, rti[:], rt2[:])
        sin_turns(R[6][:], y_r, 0.25, t_r, rti[:], rt2[:])
        op("act", lambda e: e.activation(out=mg_r, in_=z_r, func=AF.Exp, scale=float(128 * LP)), reads=[B_s], writes=[B_s])
        op("dve", lambda e, hh=hh: e.tensor_tensor(out=CRI[:, 0, hh * HW:(hh + 1) * HW], in0=mg_r, in1=R[6][:], op=ALU.mult), reads=[B_s], writes=[B_s])
        op("dve", lambda e, hh=hh: e.tensor_tensor(out=CRI[:, 1, hh * HW:(hh + 1) * HW], in0=mg_r, in1=R[7][:], op=ALU.mult), reads=[B_s], writes=[B_s])
        op("act", lambda e: e.activation(out=dt_r, in_=srow[:, 2, :], func=AF.Exp), reads=[B_s], writes=[B_s])
        sin_turns(sn_r, thp_r, 0.0, t_r, rti[:], rt2[:])
        sin_turns(cs_r, thp_r, 0.25, t_r, rti[:], rt2[:])
        op("dve", lambda e: e.tensor_scalar(out=dt_r, in0=thp_r, scalar1=0.5, scalar2=None, op0=ALU.mult), reads=[B_s], writes=[B_s])
        sin_turns(sh_r, dt_r, 0.0, t_r, rti[:], rt2[:])
        op("dve", lambda e: e.tensor_tensor(out=z_r, in0=em1_r, in1=cs_r, op=ALU.mult), reads=[B_s], writes=[B_s])
        op("dve", lambda e: e.tensor_tensor(out=t_r, in0=sh_r, in1=sh_r, op=ALU.mult), reads=[B_s], writes=[B_s])
        op("dve", lambda e: e.scalar_tensor_tensor(out=z_r, in0=t_r, scalar=-2.0, in1=z_r, op0=ALU.mult, op1=ALU.add), reads=[B_s], writes=[B_s])
        op("dve", lambda e: e.scalar_tensor_tensor(out=sn_r, in0=em1_r, scalar=1.0, in1=sn_r, op0=ALU.add, op1=ALU.mult), reads=[B_s], writes=[B_s])
        nr_r, ni_r = z_r, sn_r
        op("dve", lambda e: e.tensor_tensor(out=cs_r, in0=lr_r, in1=lr_r, op=ALU.mult), reads=[B_s], writes=[B_s])
        op("dve", lambda e: e.tensor_tensor(out=t_r, in0=li_r, in1=li_r, op=ALU.mult), reads=[B_s], writes=[B_s])
        op("dve", lambda e: e.tensor_tensor(out=cs_r, in0=cs_r, in1=t_r, op=ALU.add), reads=[B_s], writes=[B_s])
        op("dve", lambda e: e.reciprocal(out=cs_r, in_=cs_r), reads=[B_s], writes=[B_s])
        rden = cs_r
        op("dve", lambda e: e.tensor_tensor(out=em1_r, in0=nr_r, in1=lr_r, op=ALU.mult), reads=[B_s], writes=[B_s])
        op("dve", lambda e: e.tensor_tensor(out=t_r, in0=ni_r, in1=li_r, op=ALU.mult), reads=[B_s], writes=[B_s])
        op("dve", lambda e: e.tensor_tensor(out=em1_r, in0=em1_r, in1=t_r, op=ALU.add), reads=[B_s], writes=[B_s])
        op("dve", lambda e: e.tensor_tensor(out=em1_r, in0=em1_r, in1=rden, op=ALU.mult), reads=[B_s], writes=[B_s])
        op("dve", lambda e: e.tensor_tensor(out=sh_r, in0=ni_r, in1=lr_r, op=ALU.mult), reads=[B_s], writes=[B_s])
        op("dve", lambda e: e.tensor_tensor(out=t_r, in0=nr_r, in1=li_r, op=ALU.mult), reads=[B_s], writes=[B_s])
        op("dve", lambda e: e.tensor_tensor(out=sh_r, in0=sh_r, in1=t_r, op=ALU.subtract), reads=[B_s], writes=[B_s])
        op("dve", lambda e: e.tensor_tensor(out=sh_r, in0=sh_r, in1=rden, op=ALU.mult), reads=[B_s], writes=[B_s])
        k_re, k_im = em1_r, sh_r
        op("dve", lambda e: e.tensor_tensor(out=z_r, in0=k_re, in1=bt[:, 0, :], op=ALU.mult), reads=[B_s], writes=[B_s])
        op("dve", lambda e: e.tensor_tensor(out=t_r, in0=k_im, in1=bt[:, 1, :], op=ALU.mult), reads=[B_s], writes=[B_s])
        op("dve", lambda e: e.tensor_tensor(out=z_r, in0=z_r, in1=t_r, op=ALU.subtract), reads=[B_s], writes=[B_s])
        op("dve", lambda e: e.tensor_tensor(out=sn_r, in0=k_re, in1=bt[:, 1, :], op=ALU.mult), reads=[B_s], writes=[B_s])
        op("dve", lambda e: e.tensor_tensor(out=t_r, in0=k_im, in1=bt[:, 0, :], op=ALU.mult), reads=[B_s], writes=[B_s])
        op("dve", lambda e: e.tensor_tensor(out=sn_r, in0=sn_r, in1=t_r, op=ALU.add), reads=[B_s], writes=[B_s])
        bre3 = R[1][:].rearrange("p (g s) -> p g s", g=8)
        bim3 = R[5][:].rearrange("p (g s) -> p g s", g=8)
        op("dve", lambda e, gs=gs: e.tensor_copy(out=bpq[:, gs, 0, 0:64], in_=bre3), reads=[B_s], writes=[B_s])
        op("dve", lambda e, gs=gs: e.tensor_copy(out=bpq[:, gs, 0, 64:128], in_=bim3), reads=[B_s], writes=[B_s])
        op("dve", lambda e, gs=gs: e.tensor_copy(out=bpq[:, gs, 1, 0:64], in_=bim3), reads=[B_s], writes=[B_s])
        op("dve", lambda e, gs=gs: e.tensor_scalar(out=bpq[:, gs, 1, 64:128], in0=bre3, scalar1=-1.0, scalar2=None, op0=ALU.mult), reads=[B_s], writes=[B_s])

    S.barrier()
    A0.close()
    x_tok = [sb("x_tok%d" % i, [128, D]) for i in range(3)]
    B_x = [Buf("x_tok%d" % i) for i in range(3)]
    h_bf = sb("h_bf", [128, 1, D], BF16)
    B_h = [Buf("h0")]
    hT2 = [sb("hT%d" % i, [128, 8, TB], BF16) for i in range(2)]
    B_hT2 = [Buf("hT0"), Buf("hT1")]
    v_sb = sb("v_sb", [128, 6, TB + 2])
    B_v = [Buf("v%d" % c) for c in range(6)]
    cc_sb = sb("cc_sb", [128, 2, TB])
    B_cc = [Buf("cc0"), Buf("cc1")]
    init = sb("init", [128, G, 2])
    B_init = [Buf("init%d" % g) for g in range(G)]
    op("pool", lambda e: e.memset(v_sb[:], 0.0), writes=B_v)
    op("pool", lambda e: e.memset(init[:], 0.0), writes=B_init)

    blocks = [("pre", i) for i in range(NB)] + [("main", i) for i in range(NB)]

    def proj(par, mt):
        ps, b_ps = next_ps()
        mm_group(ps[:, 0:TB], b_ps,
                 [(w_in_sb[:, k, mt * 128:(mt + 1) * 128], hT2[par][:, k, :]) for k in range(8)], [B_w, B_hT2[par]])
        return ps, b_ps

    def alloc_main():
        g = {}
        g["ct1"] = sb("ct1", [128, 2, TB]); g["ct2"] = sb("ct2", [128, 2, TB])
        g["outA2"] = [sb("outA%d" % i, [128, 6, TB], BF16) for i in range(2)]
        g["uT2"] = [sb("uT%d" % i, [128, 2, TB], BF16) for i in range(2)]
        g["u_f2"] = [sb("u_f%d" % i, [128, 2, TB]) for i in range(2)]
        g["gates2"] = [sb("gates%d" % i, [128, 16, TB], BF16) for i in range(2)]
        g["st"] = [[sb("st%d_%d" % (i, j), [128, TB]) for j in range(NST)] for i in range(NSB)]
        g["s_bf"] = sb("s_bf", [128, NSB, TB], BF16)
        g["itmp"] = sb("itmp", [128, G, 2])
        g["ctmp"] = sb("ctmp", [128, 4, G])
        g["y_sb"] = sb("y_sb", [128, 2, TB]); g["z_f"] = sb("z_f", [128, 2, TB]); g["z_b"] = sb("z_b", [128, 2, TB], BF16)
        g["sg"] = sb("sg", [128, 2, TB]); g["outB"] = sb("outB", [128, 2, TB], BF16)
        g["m1"] = sb("m1", [128, 1, TB]); g["m2"] = sb("m2", [128, 1, TB]); g["merged"] = sb("merged", [128, 8, TB], BF16)
        g["stg32"] = [sb("stg32_%d" % i, [128, 512]) for i in range(2)]
        g["stgb"] = [sb("stgb_%d" % i, [128, 512], BF16) for i in range(2)]
        return g

    NSB = 3
    NST = 10
    B_ct = [Buf("ct0"), Buf("ct1")]
    B_outA2 = [Buf("outA0"), Buf("outA1")]
    B_u2 = [Buf("u0"), Buf("u1")]
    B_g2 = [Buf("gates0"), Buf("gates1")]
    B_st = [[Buf("st%d_%d" % (i, j)) for j in range(NST)] for i in range(NSB)]
    B_sbf = [Buf("sbf%d" % i) for i in range(NSB)]
    B_y = [Buf("y0"), Buf("y1")]
    B_outB = Buf("outB")
    B_m = [Buf("m0"), Buf("m1")]
    B_mg = Buf("merged")
    B_wl = Buf("wl")
    B_ctmp = Buf("ctmp")
    B_yacc = [Buf("yacc0"), Buf("yacc1")]
    B_stg = [Buf("stg0"), Buf("stg1")]
    pre_chunks = [(tb, ch, hf) for tb in range(2) for ch in range(NE // 128) for hf in range(2)]
    pre_i = [0]
    n_pre_per_blk = -(-len(pre_chunks) // NB)
    MA = {}
    B_stg32 = [Buf("stg32_0"), Buf("stg32_1")]

    def precast_gen(n):
        if SKIP_PRECAST:
            return
        stg32, stgb = MA["stg32"], MA["stgb"]

        def load(i):
            tb, ch, hf = pre_chunks[i]
            src = (exp_u if tb == 0 else exp_v)[ch * 128:(ch + 1) * 128, hf * 512:(hf + 1) * 512]
            dma("act", lambda e, src=src, j=i % 2: e.dma_start(out=stg32[j][:], in_=src), writes=[B_stg32[i % 2]])

        for _ in range(n):
            i = pre_i[0]
            if i >= len(pre_chunks):
                return
            pre_i[0] += 1
            if i == 0:
                load(0)
            if i + 1 < len(pre_chunks):
                pass
            tb, ch, hf = pre_chunks[i]
            j = i % 2
            dst = uv_d[ch * 128:(ch + 1) * 128, tb * D + hf * 512:tb * D + (hf + 1) * 512]
            op("act", lambda e, j=j: e.activation(out=stgb[j][:], in_=stg32[j][:], func=AF.Copy), reads=[B_stg32[j]], writes=[B_stg[j]])
            if i + 1 < len(pre_chunks):
                load(i + 1)
            dma("act", lambda e, dst=dst, j=j: e.dma_start(out=dst, in_=stgb[j][:]), reads=[B_stg[j]], writes=[B_uv])
            yield

    def load_norm_T(bi):
        kind, blk = blocks[bi]
        par = bi % 2
        xsrc = x_main if kind == "main" else x_prev
        xt, b_xt = x_tok[bi % 3], B_x[bi % 3]
        dma("sp", lambda e: e.dma_start(out=xt[:], in_=xsrc[blk * TB:(blk + 1) * TB, :]), writes=[b_xt])
        norm_tile(xt[:], b_xt, g_sb[:], B_c, h_bf[:, 0, :], B_h[0], 4 * par)
        transposes(h_bf[:, 0, :], B_h[0], hT2[par], B_hT2[par])

    def front(bi):
        kind, blk = blocks[bi]
        par = bi % 2
        load_norm_T(bi)
        yield
        for m in range(2):
            ps_u, b_u = proj(par, 18 + m)
            op("act", lambda e, m=m, ps_u=ps_u: e.activation(out=u_f2[par][:, m, :], in_=ps_u[:, 0:TB], func=AF.Copy), reads=[b_u], writes=[B_u2[par]])
            op("act", lambda e, m=m, ps_u=ps_u: e.activation(out=uT2[par][:, m, :], in_=ps_u[:, 0:TB], func=AF.Copy), reads=[b_u], writes=[B_u2[par]])
            yield

    def delayed(gen, n):
        for _ in range(n):
            yield
        for _ in gen:
            yield

    def convgate(bi):
        kind, blk = blocks[bi]
        par = bi % 2
        is_main = kind == "main"
        last_pre = (kind == "pre" and blk == NB - 1)
        if is_main:
            outA, B_outA = outA2[par], B_outA2[par]
            gates, B_g = gates2[par], B_g2[par]
        if is_main or last_pre:
            for c in range(6):
                j = c % 2
                ps_c, b_c = proj(par, 6 + c)
                ps_x, b_xx = proj(par, 12 + c)
                op("act", lambda e, ps_c=ps_c, j=j: e.activation(out=cc_sb[:, j, :], in_=ps_c[:, 0:TB], func=AF.Copy),
                   reads=[b_c], writes=[B_cc[j]])
                op("dve", lambda e, ps_x=ps_x, j=j, c=c: e.tensor_tensor(out=v_sb[:, c, 2:TB + 2], in0=cc_sb[:, j, :], in1=ps_x[:, 0:TB], op=ALU.mult),
                   reads=[B_cc[j], b_xx], writes=[B_v[c]])
                yield
                if is_main:
                    ps_b, b_b = proj(par, c)
                    op("pool", lambda e, c=c, j=j: e.tensor_scalar(out=ct1[:, j, :], in0=v_sb[:, c, 2:TB + 2], scalar1=CW(c, 2), scalar2=CBIAS(c),
                                                                   op0=ALU.mult, op1=ALU.add), reads=[B_v[c], B_c], writes=[B_ct[j]])
                    op("dve", lambda e, c=c, j=j: e.scalar_tensor_tensor(out=ct2[:, j, :], in0=v_sb[:, c, 1:TB + 1], scalar=CW(c, 1), in1=ct1[:, j, :],
                                                                         op0=ALU.mult, op1=ALU.add), reads=[B_v[c], B_c, B_ct[j]], writes=[B_ct[j]])
                    op("dve", lambda e, c=c, j=j: e.scalar_tensor_tensor(out=ct1[:, j, :], in0=v_sb[:, c, 0:TB], scalar=CW(c, 0), in1=ct2[:, j, :],
                                                                         op0=ALU.mult, op1=ALU.add), reads=[B_v[c], B_c, B_ct[j]], writes=[B_ct[j]])
                    op("dve", lambda e, c=c, j=j, ps_b=ps_b: e.tensor_tensor(out=outA[:, c, :], in0=ct1[:, j, :], in1=ps_b[:, 0:TB], op=ALU.mult),
                       reads=[B_ct[j], b_b], writes=[B_outA])
                op("pool", lambda e, c=c: e.tensor_copy(out=v_sb[:, c, 0:2], in_=v_sb[:, c, TB:TB + 2]), reads=[B_v[c]], writes=[B_v[c]])
                yield
        if is_main:
            for t in range(16):
                ps_g, b_gg = proj(par, 20 + t)
                op("act", lambda e, t=t, ps_g=ps_g: e.activation(out=gates[:, t, :], in_=ps_g[:, 0:TB], func=AF.Sigmoid,
                                                                 bias=colp_sb[:, t:t + 1], scale=1.0), reads=[b_gg, B_c], writes=[B_g])
                if t % 2 == 1:
                    yield

    def ssm(bi):
        kind, blk = blocks[bi]
        par = bi % 2
        is_main = kind == "main"
        uT, B_u = uT2[par], B_u2[par]
        pq = {}

        def stage0(g):
            ps_p, b_p = ps_mm[3 + (g % 2)], B_ps[3 + (g % 2)]
            m = g // 8
            mm_group(ps_p[:, 0:TB], b_p, [(bpq[:, g, 0, :], uT[:, m, :])], [B_s, B_u])
            mm_group(ps_p[:, TB:2 * TB], b_p, [(bpq[:, g, 1, :], uT[:, m, :])], [B_s, B_u])
            pq[g] = (ps_p, b_p)

        def bufs(g):
            si = g % NSB
            return [st[si][j][:] for j in range(NST)], B_st[si], si

        def stage1(g):
            (R1, R2, W1, W2, T1, T2, T3, T4, T5, T6), (bR1, bR2, bW1, bW2, bT1, bT2, bT3, bT4, bT5, bT6), si = bufs(g)
            ps_p, b_p = pq.pop(g)
            Pp, Qp = ps_p[:, 0:TB], ps_p[:, TB:2 * TB]
            C_g, S_g = ctab[:, g, 0:TB], stab[:, g, 0:TB]
            op("dve", lambda e: e.tensor_tensor(out=T1, in0=C_g, in1=Pp, op=ALU.mult), reads=[B_s, b_p], writes=[bT1])
            op("dve", lambda e: e.tensor_tensor(out=T2, in0=S_g, in1=Qp, op=ALU.mult), reads=[B_s, b_p], writes=[bT2])
            op("dve", lambda e: e.tensor_tensor(out=T3, in0=C_g, in1=Qp, op=ALU.mult), reads=[B_s, b_p], writes=[bT3])
            op("dve", lambda e: e.tensor_tensor(out=T4, in0=S_g, in1=Pp, op=ALU.mult), reads=[B_s, b_p], writes=[bT4])

        def stage2(g):
            (R1, R2, W1, W2, T1, T2, T3, T4, T5, T6), (bR1, bR2, bW1, bW2, bT1, bT2, bT3, bT4, bT5, bT6), si = bufs(g)
            op("dve", lambda e: e.tensor_tensor(out=R1, in0=T1, in1=T2, op=ALU.add), reads=[bT1, bT2], writes=[bR1])
            op("pool", lambda e: e.tensor_tensor(out=R2, in0=T3, in1=T4, op=ALU.subtract), reads=[bT3, bT4], writes=[bR2])

        def stage3(g):
            (R1, R2, W1, W2, T1, T2, T3, T4, T5, T6), (bR1, bR2, bW1, bW2, bT1, bT2, bT3, bT4, bT5, bT6), si = bufs(g)
            rb = r_p[:, g:g + 1].to_broadcast([128, TB])
            op("dve", lambda e: e.tensor_tensor_scan(out=W1, data0=rb, data1=R1, initial=init[:, g, 0:1], op0=ALU.mult, op1=ALU.add),
               reads=[B_s, bR1, B_init[g]], writes=[bW1])
            op("dve", lambda e: e.tensor_tensor_scan(out=W2, data0=rb, data1=R2, initial=init[:, g, 1:2], op0=ALU.mult, op1=ALU.add),
               reads=[B_s, bR2, B_init[g]], writes=[bW2])

        def stage4(g):
            (R1, R2, W1, W2, T1, T2, T3, T4, T5, T6), (bR1, bR2, bW1, bW2, bT1, bT2, bT3, bT4, bT5, bT6), si = bufs(g)
            C_g, S_g = ctab[:, g, 0:TB], stab[:, g, 0:TB]
            op("act", lambda e: e.activation(out=itmp[:, g, 0:1], in_=W1[:, TB - 1:TB], func=AF.Copy), reads=[bW1], writes=[B_wl])
            op("act", lambda e: e.activation(out=itmp[:, g, 1:2], in_=W2[:, TB - 1:TB], func=AF.Copy), reads=[bW2], writes=[B_wl])
            op("pool", lambda e: e.tensor_tensor(out=T5, in0=C_g, in1=W1, op=ALU.mult), reads=[B_s, bW1], writes=[bT5])
            op("pool", lambda e: e.tensor_tensor(out=T6, in0=S_g, in1=W2, op=ALU.mult), reads=[B_s, bW2], writes=[bT6])
            op("pool", lambda e: e.tensor_tensor(out=s_bf[:, si, :], in0=T5, in1=T6, op=ALU.subtract), reads=[bT5, bT6], writes=[B_sbf[si]])

        def stage5(g):
            si = g % NSB
            m = g // 8
            op("pe", lambda e: e.matmul(ps_acc[m][:, 0:TB], lhsT=ctb[:, g, :], rhs=s_bf[:, si, :], start=(g % 8 == 0), stop=(g % 8 == 7)),
               reads=[B_s, B_sbf[si]], writes=[B_yacc[m]])

        stages = [stage0, stage1, stage2, stage3, stage4, stage5]
        for step in range(G + len(stages) - 1):
            for k, fn in enumerate(stages):
                if 0 <= step - k < G:
                    fn(step - k)
            yield
        cT, sT = ctab[:, :, TB], stab[:, :, TB]
        op("dve", lambda e: e.tensor_tensor(out=ctmp[:, 0, :], in0=cT, in1=itmp[:, :, 0], op=ALU.mult), reads=[B_s, B_wl], writes=[B_ctmp])
        op("dve", lambda e: e.tensor_tensor(out=ctmp[:, 1, :], in0=sT, in1=itmp[:, :, 1], op=ALU.mult), reads=[B_s, B_wl], writes=[B_ctmp])
        op("dve", lambda e: e.tensor_tensor(out=init[:, :, 0], in0=ctmp[:, 0, :], in1=ctmp[:, 1, :], op=ALU.subtract), reads=[B_ctmp] + B_init, writes=B_init)
        op("dve", lambda e: e.tensor_tensor(out=ctmp[:, 2, :], in0=cT, in1=itmp[:, :, 1], op=ALU.mult), reads=[B_s, B_wl], writes=[B_ctmp])
        op("dve", lambda e: e.tensor_tensor(out=ctmp[:, 3, :], in0=sT, in1=itmp[:, :, 0], op=ALU.mult), reads=[B_s, B_wl], writes=[B_ctmp])
        op("dve", lambda e: e.tensor_tensor(out=init[:, :, 1], in0=ctmp[:, 2, :], in1=ctmp[:, 3, :], op=ALU.add), reads=[B_ctmp] + B_init, writes=B_init)
        yield

    def tail(bi):
        kind, blk = blocks[bi]
        if kind != "main":
            return
        par = bi % 2
        xt, b_xt = x_tok[bi % 3], B_x[bi % 3]
        outA, B_outA = outA2[par], B_outA2[par]
        gates, B_g = gates2[par], B_g2[par]
        u_f, B_u = u_f2[par], B_u2[par]
        for m in range(2):
            op("dve", lambda e, m=m: e.scalar_tensor_tensor(out=y_sb[:, m, :], in0=u_f[:, m, :], scalar=colp_sb[:, 42 + m:43 + m], in1=ps_acc[m][:, 0:TB],
                                                            op0=ALU.mult, op1=ALU.add), reads=[B_u, B_c, B_yacc[m]], writes=[B_y[m]])
            if debug:
                dma("sp", lambda e, m=m: e.dma_start(out=dbg_d[:, m * S_half + blk * TB:m * S_half + (blk + 1) * TB], in_=y_sb[:, m, :]), reads=[B_y[m]])
            op("act", lambda e, m=m: e.activation(out=z_f[:, m, :], in_=y_sb[:, m, :], func=AF.Gelu), reads=[B_y[m]], writes=[B_y[m]])
            op("pool", lambda e, m=m: e.tensor_copy(out=z_b[:, m, :], in_=z_f[:, m, :]), reads=[B_y[m]], writes=[B_y[m]])
            yield
        for mo in range(2):
            ps_g2, b_g2 = next_ps()
            mm_group(ps_g2[:, 0:TB], b_g2, [(w_glu_sb[:, mi, mo * 128:(mo + 1) * 128], z_b[:, mi, :]) for mi in range(2)], [B_w, B_y[0], B_y[1]])
            op("act", lambda e, mo=mo, ps_g2=ps_g2: e.activation(out=sg[:, mo, :], in_=ps_g2[:, 0:TB], func=AF.Sigmoid,
                                                                 bias=colp_sb[:, 40 + mo:41 + mo], scale=1.0), reads=[b_g2, B_c], writes=[B_y[mo]])
            op("pool", lambda e, mo=mo: e.tensor_tensor(out=outB[:, mo, :], in0=z_f[:, mo, :], in1=sg[:, mo, :], op=ALU.mult), reads=[B_y[mo]], writes=[B_outB])
            yield
        for dm in range(8):
            j = 0
            ps_a, b_a = next_ps()
            mm_group(ps_a[:, 0:TB], b_a, [(w_a_sb[:, c, dm * 128:(dm + 1) * 128], outA[:, c, :]) for c in range(6)], [B_w, B_outA])
            mm_group(ps_a[:, TB:2 * TB], b_a, [(w_b_sb[:, mi, dm * 128:(dm + 1) * 128], outB[:, mi, :]) for mi in range(2)], [B_w, B_outB])
            op("dve", lambda e, dm=dm, j=j, ps_a=ps_a: e.tensor_tensor(out=m1[:, j, :], in0=gates[:, dm, :], in1=ps_a[:, 0:TB], op=ALU.mult), reads=[B_g, b_a], writes=[B_m[j]])
            op("dve", lambda e, dm=dm, j=j, ps_a=ps_a: e.tensor_tensor(out=m2[:, j, :], in0=gates[:, 8 + dm, :], in1=ps_a[:, TB:2 * TB], op=ALU.mult), reads=[B_g, b_a], writes=[B_m[j]])
            op("pool", lambda e, dm=dm, j=j: e.tensor_tensor(out=merged[:, dm, :], in0=m1[:, j, :], in1=m2[:, j, :], op=ALU.add), reads=[B_m[j]], writes=[B_mg])
            yield
        for hf in range(2):
            ps_o, b_o = next_ps()
            mm_group(ps_o[:, :], b_o, [(merged[:, k, :], w_out_sb[:, k, hf * 512:(hf + 1) * 512]) for k in range(8)], [B_w, B_mg])
            op("dve", lambda e, hf=hf, ps_o=ps_o: e.tensor_tensor(out=xt[:, hf * 512:(hf + 1) * 512], in0=xt[:, hf * 512:(hf + 1) * 512],
                                                                  in1=ps_o[:, :], op=ALU.add), reads=[b_xt, b_o], writes=[b_xt])
            yield
        dma("sp", lambda e: e.dma_start(out=x1_d[blk * TB:(blk + 1) * TB, :], in_=xt[:]), reads=[b_xt], writes=[B_x1d])

    def interleave(gens):
        gens = [g for g in gens if g is not None]
        while gens:
            alive = []
            for g in gens:
                try:
                    next(g)
                    alive.append(g)
                except StopIteration:
                    pass
            gens = alive

    Apre = Alloc(nc)
    u_tok2 = [Apre.sb("u_tok%d" % i, [128, 256], BF16) for i in range(2)]
    B_ut = [Buf("ut0"), Buf("ut1")]
    pt_t = [Apre.sb("pt_t%d" % i, [128, 4, 64]) for i in range(4)]
    B_ptt = [Buf("ptt%d" % i) for i in range(4)]
    Uc = Apre.sb("Uc", [128, 8, 128])
    Us = Apre.sb("Us", [128, 8, 128])
    ta = Apre.sb("ta", [128, 8, 64])
    tb_ = Apre.sb("tb_", [128, 8, 64])
    ones2 = Apre.sb("ones2", [128, 2])
    S_sb = Apre.sb("S_sb", [128, 2, G])
    tq = Apre.sb("tq", [128, 2, G])
    B_fin = Buf("fin")
    B_M = Buf("Mst")
    zbanks = [ps_acc[0], ps_acc[1], ps_mm[3], ps_mm[4]]
    B_zb = [Buf("zb%d" % i) for i in range(4)]
    ps_pool[:] = [0, 1, 2]
    op("pool", lambda e: e.memset(Mst[:], 0.0), writes=[B_M])
    op("pool", lambda e: e.memset(ones2[:], 1.0), writes=[B_fin])
    def pre_B(j):
        par = j % 2
        ps_u, b_u = next_ps()
        mm_group(ps_u[:, 0:256], b_u, [(hT2[par][:, k, :], w_in_sb[:, k, 2304:2560]) for k in range(8)], [B_w, B_hT2[par]])
        op("act", lambda e, par=par, ps_u=ps_u: e.activation(out=u_tok2[par][:], in_=ps_u[:, 0:256], func=AF.Copy), reads=[b_u], writes=[B_ut[par]])
        if j == NB - 1:
            for _ in convgate(j):
                pass

    def pre_C(j):
        par = j % 2
        l = (LP - 1) - (j % LP)
        for zi in range(4):
            m = zi // 2
            op("pe", lambda e, zi=zi, m=m, l=l, par=par, j=j: e.matmul(zbanks[zi][:, :], lhsT=u_tok2[par][:, m * 128:(m + 1) * 128],
                                                                   rhs=PT[:, l, 4 * zi:4 * zi + 4, :].rearrange("p g c -> p (g c)"),
                                                                   start=(j % LP == 0), stop=(j % LP == LP - 1)),
               reads=[B_ut[par], B_s], writes=[B_zb[zi]])
        if j % LP == LP - 1:
            for zi in range(4):
                gsl = slice(4 * zi, 4 * zi + 4)
                Mre, Mim = Mst[:, gsl, 0:64], Mst[:, gsl, 64:128]
                cr = CRI[:, 0, 256 * zi:256 * (zi + 1)].rearrange("p (g s) -> p g s", g=4)
                ci = CRI[:, 1, 256 * zi:256 * (zi + 1)].rearrange("p (g s) -> p g s", g=4)
                Z3 = zbanks[zi][:, :].rearrange("p (g c) -> p g c", g=4)
                Zre, Zim = Z3[:, :, 0:64], Z3[:, :, 64:128]
                t1, t2, t3, t4 = [t[:] for t in pt_t]
                b1, b2, b3, b4 = B_ptt
                op("pool", lambda e, cr=cr, Mre=Mre, t1=t1: e.tensor_tensor(out=t1, in0=cr, in1=Mre, op=ALU.mult), reads=[B_s, B_M], writes=[b1])
                op("pool", lambda e, ci=ci, Mim=Mim, t2=t2: e.tensor_tensor(out=t2, in0=ci, in1=Mim, op=ALU.mult), reads=[B_s, B_M], writes=[b2])
                op("pool", lambda e, cr=cr, Mim=Mim, t3=t3: e.tensor_tensor(out=t3, in0=cr, in1=Mim, op=ALU.mult), reads=[B_s, B_M], writes=[b3])
                op("pool", lambda e, ci=ci, Mre=Mre, t4=t4: e.tensor_tensor(out=t4, in0=ci, in1=Mre, op=ALU.mult), reads=[B_s, B_M], writes=[b4])
                op("pool", lambda e, t1=t1, t2=t2: e.tensor_tensor(out=t1, in0=t1, in1=t2, op=ALU.subtract), reads=[b1, b2], writes=[b1])
                op("pool", lambda e, t3=t3, t4=t4: e.tensor_tensor(out=t3, in0=t3, in1=t4, op=ALU.add), reads=[b3, b4], writes=[b3])
                op("dve", lambda e, Mre=Mre, t1=t1, Zre=Zre: e.tensor_tensor(out=Mre, in0=t1, in1=Zre, op=ALU.add), reads=[b1, B_zb[zi]], writes=[B_M])
                op("dve", lambda e, Mim=Mim, t3=t3, Zim=Zim: e.tensor_tensor(out=Mim, in0=t3, in1=Zim, op=ALU.add), reads=[b3, B_zb[zi]], writes=[B_M])

    for k in range(NB + 2):
        if k < NB:
            load_norm_T(k)
        if 0 <= k - 1 < NB:
            pre_B(k - 1)
        if 0 <= k - 2 < NB:
            pre_C(k - 2)
    ps_S, b_S = next_ps()
    for m in range(2):
        gsl = slice(8 * m, 8 * m + 8)
        Bre, Bim = bpq[:, gsl, 0, 0:64], bpq[:, gsl, 0, 64:128]
        Mre, Mim = Mst[:, gsl, 0:64], Mst[:, gsl, 64:128]
        op("dve", lambda e, Bre=Bre, Mre=Mre: e.tensor_tensor(out=ta[:], in0=Bre, in1=Mre, op=ALU.mult), reads=[B_s, B_M], writes=[B_fin])
        op("dve", lambda e, Bim=Bim, Mim=Mim: e.tensor_tensor(out=tb_[:], in0=Bim, in1=Mim, op=ALU.mult), reads=[B_s, B_M], writes=[B_fin])
        op("dve", lambda e: e.tensor_tensor(out=Uc[:, :, 0:64], in0=ta[:], in1=tb_[:], op=ALU.subtract), reads=[B_fin], writes=[B_fin])
        op("dve", lambda e, Bre=Bre, Mim=Mim: e.tensor_tensor(out=ta[:], in0=Bre, in1=Mim, op=ALU.mult), reads=[B_s, B_M, B_fin], writes=[B_fin])
        op("dve", lambda e, Bim=Bim, Mre=Mre: e.tensor_tensor(out=tb_[:], in0=Bim, in1=Mre, op=ALU.mult), reads=[B_s, B_M, B_fin], writes=[B_fin])
        op("dve", lambda e: e.tensor_tensor(out=Uc[:, :, 64:128], in0=ta[:], in1=tb_[:], op=ALU.add), reads=[B_fin], writes=[B_fin])
        op("dve", lambda e: e.tensor_copy(out=Us[:, :, 0:64], in_=Uc[:, :, 64:128]), reads=[B_fin], writes=[B_fin])
        op("dve", lambda e: e.tensor_scalar(out=Us[:, :, 64:128], in0=Uc[:, :, 0:64], scalar1=-1.0, scalar2=None, op0=ALU.mult), reads=[B_fin], writes=[B_fin])
        for gl_ in range(8):
            g = 8 * m + gl_
            op("pe", lambda e, gl_=gl_, g=g: e.matmul(ps_S[:, 2 * g:2 * g + 2], lhsT=Uc[:, gl_, :], rhs=ones2[:, :], start=True, stop=True), reads=[B_fin], writes=[b_S])
            op("pe", lambda e, gl_=gl_, g=g: e.matmul(ps_S[:, 64 + 2 * g:64 + 2 * g + 2], lhsT=Us[:, gl_, :], rhs=ones2[:, :], start=True, stop=True), reads=[B_fin], writes=[b_S])
    for a in range(2):
        op("act", lambda e, a=a: e.activation(out=S_sb[:, a, :], in_=ps_S[:, 64 * a:64 * a + 32].rearrange("p (g t) -> p g t", t=2)[:, :, 0], func=AF.Copy),
           reads=[b_S, B_fin], writes=[B_fin])
    c1, s1 = ctab[:, :, 1], stab[:, :, 1]
    op("dve", lambda e: e.tensor_tensor(out=tq[:, 0, :], in0=c1, in1=S_sb[:, 0, :], op=ALU.mult), reads=[B_s, B_fin], writes=[B_fin])
    op("dve", lambda e: e.tensor_tensor(out=tq[:, 1, :], in0=s1, in1=S_sb[:, 1, :], op=ALU.mult), reads=[B_s, B_fin], writes=[B_fin])
    op("dve", lambda e: e.tensor_tensor(out=init[:, :, 0], in0=tq[:, 0, :], in1=tq[:, 1, :], op=ALU.subtract), reads=[B_fin] + B_init, writes=B_init)
    op("dve", lambda e: e.tensor_tensor(out=tq[:, 0, :], in0=c1, in1=S_sb[:, 1, :], op=ALU.mult), reads=[B_s, B_fin] + B_init, writes=[B_fin])
    op("dve", lambda e: e.tensor_tensor(out=tq[:, 1, :], in0=s1, in1=S_sb[:, 0, :], op=ALU.mult), reads=[B_s, B_fin], writes=[B_fin])
    op("dve", lambda e: e.tensor_tensor(out=init[:, :, 1], in0=tq[:, 0, :], in1=tq[:, 1, :], op=ALU.add), reads=[B_fin] + B_init, writes=B_init)
    S.barrier()
    Apre.close()
    AXr.close()
    ps_pool[:] = [0, 1, 2]

    MA.update(alloc_main())
    ct1, ct2, outA2, uT2, u_f2, gates2, st, s_bf, itmp, ctmp = [MA[k] for k in ("ct1", "ct2", "outA2", "uT2", "u_f2", "gates2", "st", "s_bf", "itmp", "ctmp")]
    y_sb, z_f, z_b, sg, outB, m1, m2, merged = [MA[k] for k in ("y_sb", "z_f", "z_b", "sg", "outB", "m1", "m2", "merged")]
    nblk = len(blocks)
    for _ in front(NB):
        pass
    for bi in range(NB, nblk + 1):
        gl = []
        if bi < nblk:
            gl.append(ssm(bi))
            gl.append(convgate(bi))
        if bi - 1 >= NB:
            gl.append(tail(bi - 1))
        if bi + 1 < nblk:
            gl.append(delayed(front(bi + 1), 12))
        if bi < nblk:
            gl.append(precast_gen(n_pre_per_blk))
        interleave(gl)

    S.barrier()
    A.close()
    ps_pool[:] = list(range(NPS))
    if do_b:
        Bq = Alloc(nc)
        sb = Bq.sb
        NT = S_half // 128
        w_q_sb = sb("w_q_sb", [128, 8, NQ], BF16)
        keys_sb = sb("keys_sb", [128, 2, 128], BF16)
        g2_sb = sb("g2_sb", [128, 2, D])
        B_wq = Buf("wq")
        for kc in range(8):
            dma("pool", lambda e, kc=kc: e.dma_start(out=w_q_sb[:, kc, :], in_=w_q[kc * 128:(kc + 1) * 128, :]), writes=[B_wq])
        for c in range(2):
            dma("pool", lambda e, c=c: e.dma_start(out=keys_sb[:, c, :], in_=keysT[c]), writes=[B_wq])
            dma("sp", lambda e, c=c: e.dma_start(out=g2_sb[:, c, :], in_=g_rep[1 + c]), writes=[B_wq])
        iota4 = sb("iota4", [128, 8, 16, 16])
        op("pool", lambda e: e.iota(iota4[:].rearrange("p h k a -> p (h k) a"), [[0, 128], [1, 16]], base=0, channel_multiplier=0,
                                    allow_small_or_imprecise_dtypes=True), writes=[B_wq])
        x1t = [sb("x1t%d" % i, [128, D]) for i in range(2)]
        B_x1 = [Buf("x1t0"), Buf("x1t1")]
        xn_f2 = [sb("xn_f%d" % i, [128, D]) for i in range(2)]
        B_xn2 = [Buf("xn0"), Buf("xn1")]
        xn_b = sb("xn_b", [128, D], BF16)
        B_xnb = Buf("xn_b")
        xnT = sb("xnT", [128, 8, 128], BF16)
        B_xnT = Buf("xnT")
        qT = sb("qT", [128, 16, 128], BF16)
        B_qT = Buf("qT")
        s_sb = sb("s_sb", [128, 16, 128])
        B_ssb = Buf("s_sb")
        srep = sb("srep", [128, 128])
        v12 = sb("v12", [128, 8, 2, 16])
        i12u = sb("i12u", [128, 8, 2, 16], U32)
        i12f = sb("i12f", [128, 8, 2, 16])
        cand = sb("cand", [128, 8, 16, 16])
        crep = sb("crep", [128, 256])
        best = sb("best", [128, 8, 16])
        posu = sb("posu", [128, 8, 16], U32)
        pau = sb("pau", [128, 8, 16], U32)
        pbu = sb("pbu", [128, 8, 16], U32)
        paf = sb("paf", [128, 8, 16])
        pbf = sb("pbf", [128, 8, 16])
        eq = sb("eq", [128, 8, 16, 16])
        If = sb("If", [128, 8, 16])
        Jf = sb("Jf", [128, 8, 16])
        eg = sb("eg", [128, 8, 16])
        zs = sb("zs", [128, 8])
        eidx2 = [sb("eidx%d" % i, [128, 128], I32) for i in range(2)]
        gate2 = [sb("gate%d" % i, [128, 128]) for i in range(2)]
        B_eg2 = [Buf("eg0"), Buf("eg1")]
        hpre = sb("hpre", [128, 128])
        B_tk = Buf("topk")
        junk_b = sb("junk_b", [128, D], BF16)
        B_jf = Buf("junk_b")
        NGB = 22
        gbuf = [sb("gbuf%d" % i, [128, 2 * D], BF16) for i in range(NGB)]
        B_gb = [Buf("gb%d" % i) for i in range(NGB)]
        gb_rr = [0]
        NHC = 4
        B_hc = [Buf("hc%d" % i) for i in range(NHC)]
        wcol = sb("wcol", [128, 2, 128])
        NDG = 6
        diag = [sb("diag%d" % i, [128, 128], BF16) for i in range(NDG)]
        B_dg = [Buf("dg%d" % i) for i in range(NDG)]
        o_t = sb("o_t", [128, D])
        B_ot = Buf("o_t")

        def prep_gen(t):
            par = t % 2
            xt, b_xt = x1t[par], B_x1[par]
            xn_f, B_xn = xn_f2[par], B_xn2[par]
            eidx, gate, B_eg = eidx2[par], gate2[par], B_eg2[par]
            dma("sp", lambda e: e.dma_start(out=xt[:], in_=x1_d[t * 128:(t + 1) * 128, :]), reads=[B_x1d], writes=[b_xt])
            norm_tile(xt[:], b_xt, g2_sb[:, 0, :], B_wq, xn_f[:], B_xn, 0)
            op("act", lambda e: e.activation(out=xn_b[:], in_=xn_f[:], func=AF.Copy), reads=[B_xn], writes=[B_xnb])
            yield
            transposes(xn_b[:], B_xnb, xnT, B_xnT)
            yield
            pend = None
            for qt in range(16):
                ps_q, b_q = next_ps()
                mm_group(ps_q[:, 0:128], b_q, [(w_q_sb[:, k, qt * 128:(qt + 1) * 128], xnT[:, k, :]) for k in range(8)], [B_wq, B_xnT])
                if pend is not None:
                    pend()
                pend = (lambda qt=qt, ps_q=ps_q, b_q=b_q: op("act", lambda e: e.activation(out=qT[:, qt, :], in_=ps_q[:, 0:128], func=AF.Copy), reads=[b_q], writes=[B_qT]))
                yield
            pend()
            yield
            pend = None
            for bnk in range(4):
                ps_s, b_s = next_ps()
                for j in range(4):
                    idx = bnk * 4 + j
                    mm_group(ps_s[:, j * 128:(j + 1) * 128], b_s, [(qT[:, idx, :], keys_sb[:, idx % 2, :])], [B_qT, B_wq])
                if pend is not None:
                    pend()
                pend = (lambda bnk=bnk, ps_s=ps_s, b_s=b_s: op("act", lambda e: e.activation(out=s_sb[:, bnk * 4:(bnk + 1) * 4, :].rearrange("p a n -> p (a n)"), in_=ps_s[:, :], func=AF.Copy),
                                                                 reads=[b_s], writes=[B_ssb]))
                yield
            pend()
            yield
            for h in range(8):
                for c in range(2):
                    sv = s_sb[:, 2 * h + c, :]
                    op("dve", lambda e, h=h, c=c, sv=sv: e.max(out=v12[:, h, c, 0:8], in_=sv), reads=[B_ssb], writes=[B_tk])
                    op("dve", lambda e, h=h, c=c, sv=sv: e.match_replace(out=srep[:], in_to_replace=v12[:, h, c, 0:8], in_values=sv, imm_value=-1e30), reads=[B_ssb, B_tk], writes=[B_tk])
                    yield
                    op("dve", lambda e, h=h, c=c: e.max(out=v12[:, h, c, 8:16], in_=srep[:]), reads=[B_tk], writes=[B_tk])
                    op("dve", lambda e, h=h, c=c, sv=sv: e.max_index(out=i12u[:, h, c, 0:8], in_max=v12[:, h, c, 0:8], in_values=sv), reads=[B_ssb, B_tk], writes=[B_tk])
                    yield
                    op("dve", lambda e, h=h, c=c: e.max_index(out=i12u[:, h, c, 8:16], in_max=v12[:, h, c, 8:16], in_values=srep[:]), reads=[B_tk], writes=[B_tk])
                    yield
            op("dve", lambda e: e.tensor_copy(out=i12f[:], in_=i12u[:]), reads=[B_tk], writes=[B_tk])
            v1b = v12[:, :, 0, :].rearrange("p h (a o) -> p h a o", o=1).to_broadcast([128, 8, 16, 16])
            v2b = v12[:, :, 1, :].rearrange("p h (o b) -> p h o b", o=1).to_broadcast([128, 8, 16, 16])
            op("dve", lambda e: e.tensor_tensor(out=cand[:], in0=v1b, in1=v2b, op=ALU.add), reads=[B_tk], writes=[B_tk])
            yield
            for h in range(8):
                ch = cand[:, h, :, :].rearrange("p a b -> p (a b)")
                op("dve", lambda e, h=h, ch=ch: e.max(out=best[:, h, 0:8], in_=ch), reads=[B_tk], writes=[B_tk])
                op("dve", lambda e, h=h, ch=ch: e.match_replace(out=crep[:], in_to_replace=best[:, h, 0:8], in_values=ch, imm_value=-1e30), reads=[B_tk], writes=[B_tk])
                yield
                op("dve", lambda e, h=h: e.max(out=best[:, h, 8:16], in_=crep[:]), reads=[B_tk], writes=[B_tk])
                op("dve", lambda e, h=h, ch=ch: e.max_index(out=posu[:, h, 0:8], in_max=best[:, h, 0:8], in_values=ch), reads=[B_tk], writes=[B_tk])
                yield
                op("dve", lambda e, h=h: e.max_index(out=posu[:, h, 8:16], in_max=best[:, h, 8:16], in_values=crep[:]), reads=[B_tk], writes=[B_tk])
                yield
            op("dve", lambda e: e.tensor_single_scalar(out=pau[:], in_=posu[:], scalar=4, op=ALU.logical_shift_right), reads=[B_tk], writes=[B_tk])
            op("dve", lambda e: e.tensor_single_scalar(out=pbu[:], in_=posu[:], scalar=15, op=ALU.bitwise_and), reads=[B_tk], writes=[B_tk])
            yield
            op("dve", lambda e: e.tensor_copy(out=paf[:], in_=pau[:]), reads=[B_tk], writes=[B_tk])
            op("dve", lambda e: e.tensor_copy(out=pbf[:], in_=pbu[:]), reads=[B_tk], writes=[B_tk])
            yield
            for (pf, c, dst) in ((paf, 0, If), (pbf, 1, Jf)):
                pfb = pf[:].rearrange("p h (k o) -> p h k o", o=1).to_broadcast([128, 8, 16, 16])
                ifb = i12f[:, :, c, :].rearrange("p h (o a) -> p h o a", o=1).to_broadcast([128, 8, 16, 16])
                op("dve", lambda e, pfb=pfb: e.tensor_tensor(out=eq[:], in0=pfb, in1=iota4[:], op=ALU.is_equal), reads=[B_tk, B_wq], writes=[B_tk])
                yield
                op("dve", lambda e, ifb=ifb: e.tensor_tensor(out=eq[:], in0=eq[:], in1=ifb, op=ALU.mult), reads=[B_tk], writes=[B_tk])
                yield
                op("dve", lambda e, dst=dst: e.tensor_reduce(out=dst[:].rearrange("p h k -> p (h k)"), in_=eq[:].rearrange("p h k a -> p (h k) a"), axis=AX.X, op=ALU.add),
                   reads=[B_tk], writes=[B_tk])
                yield
            op("dve", lambda e: e.scalar_tensor_tensor(out=paf[:], in0=If[:], scalar=128.0, in1=Jf[:], op0=ALU.mult, op1=ALU.add), reads=[B_tk], writes=[B_tk])
            op("dve", lambda e: e.tensor_copy(out=eidx[:], in_=paf[:].rearrange("p h k -> p (h k)")), reads=[B_tk, B_eg], writes=[B_eg])
            yield
            bmx = best[:, :, 0:1].to_broadcast([128, 8, 16])
            op("dve", lambda e: e.tensor_tensor(out=eg[:], in0=best[:], in1=bmx, op=ALU.subtract), reads=[B_tk], writes=[B_tk])
            op("act", lambda e: e.activation(out=eg[:], in_=eg[:], func=AF.Exp), reads=[B_tk], writes=[B_tk])
            op("dve", lambda e: e.tensor_reduce(out=zs[:], in_=eg[:], axis=AX.X, op=ALU.add), reads=[B_tk], writes=[B_tk])
            op("dve", lambda e: e.reciprocal(out=zs[:], in_=zs[:]), reads=[B_tk], writes=[B_tk])
            zb = zs[:].rearrange("p (h o) -> p h o", o=1).to_broadcast([128, 8, 16])
            op("dve", lambda e: e.tensor_tensor(out=gate[:].rearrange("p (h k) -> p h k", h=8), in0=eg[:], in1=zb, op=ALU.mult), reads=[B_tk, B_eg], writes=[B_eg])
            yield

        def run_gen(g, n=None):
            if g is None:
                return
            k = 0
            for _ in g:
                k += 1
                if n is not None and k >= n:
                    return

        gcur = prep_gen(0)
        run_gen(gcur)
        for t in range(NT):
            par = t % 2
            xt, b_xt = x1t[par], B_x1[par]
            xn_f, B_xn = xn_f2[par], B_xn2[par]
            eidx, gate, B_eg = eidx2[par], gate2[par], B_eg2[par]
            gnext = prep_gen(t + 1) if t + 1 < NT else None
            GS = 8
            slot_gb = {}
            for k in range(128 // GS + 1):
                if k >= 1:
                    kk = k - 1
                    b_hc = B_hc[kk % NHC]
                    cs = slice(kk * GS, (kk + 1) * GS)
                    op("act", lambda e, cs=cs: e.activation(out=wcol[:, 0, cs], in_=hpre[:, cs], func=AF.Gelu), reads=[b_hc], writes=[b_hc])
                    for sl in range(kk * GS, (kk + 1) * GS):
                        gb, b_gb = slot_gb.pop(sl)
                        dg, b_dg = diag[sl % NDG], B_dg[sl % NDG]
                        op("act", lambda e, sl=sl, gate=gate: e.activation(out=wcol[:, 1, sl:sl + 1], in_=wcol[:, 0, sl:sl + 1], func=AF.Copy, scale=gate[:, sl:sl + 1]),
                           reads=[b_hc, B_eg], writes=[b_hc])
                        op("act", lambda e, sl=sl, dg=dg: e.activation(out=dg[:], in_=ident[:], func=AF.Copy, scale=wcol[:, 1, sl:sl + 1]),
                           reads=[b_hc, B_id], writes=[b_dg])
                        for hf in range(2):
                            op("pe", lambda e, sl=sl, dg=dg, gb=gb, hf=hf: e.matmul(ps_acc[hf][:, :], lhsT=dg[:], rhs=gb[:, D + hf * 512:D + (hf + 1) * 512],
                                                                                 start=(sl == 0), stop=(sl == 127)), reads=[b_dg, b_gb], writes=[B_pacc])
                if k < 128 // GS:
                    for sl in range(k * GS, (k + 1) * GS):
                        gi = gb_rr[0] % NGB
                        gb_rr[0] += 1
                        gb, b_gb = gbuf[gi], B_gb[gi]
                        slot_gb[sl] = (gb, b_gb)
                        b_hc = B_hc[k % NHC]
                        dma("pool", lambda e, gb=gb, sl=sl, eidx=eidx: e.indirect_dma_start(out=gb[:], out_offset=None, in_=uv_d[:, :],
                            in_offset=bass.IndirectOffsetOnAxis(ap=eidx[:, sl:sl + 1], axis=0)), reads=[B_eg, B_uv], writes=[b_gb])
                        op("dve", lambda e, gb=gb, sl=sl, xn_f=xn_f: e.scalar_tensor_tensor(out=junk_b[:], in0=gb[:, 0:D], scalar=1.0, in1=xn_f[:], op0=ALU.mult, op1=ALU.mult,
                                                                                            accum_out=hpre[:, sl:sl + 1]), reads=[b_gb, B_xn, b_hc], writes=[B_jf, b_hc])
                        run_gen(gnext, 1)
            run_gen(gnext)
            for hf in range(2):
                op("dve", lambda e, hf=hf, xt=xt: e.tensor_tensor(out=xt[:, hf * 512:(hf + 1) * 512], in0=xt[:, hf * 512:(hf + 1) * 512], in1=ps_acc[hf][:, :], op=ALU.add),
                   reads=[b_xt, B_pacc], writes=[b_xt])
            norm_tile(xt[:], b_xt, g2_sb[:, 1, :], B_wq, o_t[:], B_ot, 4)
            dma("sp", lambda e, t=t: e.dma_start(out=out_d[t * 128:(t + 1) * 128, :], in_=o_t[:]), reads=[B_ot])
        S.barrier()
        Bq.close()
    P0.close()
    S.final_wait("sp")
    S.emit()
    S.close()
    return nc


def prep_shared(inp):
    f = np.float32
    sh = {}
    sh["w_in"] = np.ascontiguousarray(inp["w_in"][0], f)
    sh["w_a"] = np.ascontiguousarray(inp["w_branch_a"][0], f)
    sh["w_b"] = np.ascontiguousarray(inp["w_branch_b"][0], f)
    sh["w_out"] = np.ascontiguousarray(inp["w_out"][0], f)
    sh["w_glu"] = np.ascontiguousarray(inp["w_glu"][0], f)
    sh["w_q"] = np.ascontiguousarray(inp["w_q"][0], f)
    sh["keysT"] = np.ascontiguousarray(np.stack([inp["sub_keys_1"][0].T, inp["sub_keys_2"][0].T]), f)
    sh["g_rep"] = np.ascontiguousarray(np.stack([np.broadcast_to(inp["g_mix"][0], (128, D)),
                                                 np.broadcast_to(inp["g_ffn"][0], (128, D)),
                                                 np.broadcast_to(inp["g_final"], (128, D))]), f)
    colp = np.zeros((128, 48), f)
    colp[:, 0:16] = inp["b_gate"][0].reshape(16, 128).T
    cw = inp["conv_w"][0].reshape(3, 6, 128)
    colp[:, 16:34] = cw.transpose(2, 1, 0).reshape(128, 18)
    colp[:, 34:40] = inp["conv_b"][0].reshape(6, 128).T
    colp[:, 40:42] = inp["b_glu"][0].reshape(2, 128).T
    colp[:, 42:44] = inp["ssm_d"][0].reshape(2, 128).T
    sh["colp"] = colp
    lr, li, ld = inp["lam_re"][0], inp["lam_im"][0], inp["log_dt"][0]
    pt = np.zeros((3, 128, G), f)
    pt[0] = np.concatenate([lr.T, lr.T], 0)
    pt[1] = np.concatenate([li.T, li.T], 0)
    pt[2] = np.broadcast_to(ld[None, :], (128, G))
    sh["ssm_pt"] = pt
    row = np.zeros((3, 128, G * P), f)
    row[0] = np.broadcast_to(lr.reshape(1, -1), (128, G * P))
    row[1] = np.broadcast_to(li.reshape(1, -1), (128, G * P))
    row[2] = np.broadcast_to(np.repeat(ld, P).reshape(1, -1), (128, G * P))
    sh["ssm_row"] = row
    btm = np.zeros((2, 128, G, P), f)
    ctm = np.zeros((128, G, 128), f)
    for g in range(G):
        r0 = 16 * (g % 8)
        btm[0, r0:r0 + 16, g, :] = inp["ssm_b_re"][0][g].T
        btm[1, r0:r0 + 16, g, :] = inp["ssm_b_im"][0][g].T
        ctm[0:64, g, r0:r0 + 16] = inp["ssm_c_re"][0][g].T
        ctm[64:128, g, r0:r0 + 16] = inp["ssm_c_im"][0][g].T
    sh["bt_in"] = btm.reshape(2, 128, G * P)
    sh["ct_in"] = ctm.reshape(128, G * 128)
    sh["exp_u"] = np.ascontiguousarray(inp["expert_u"][0], f)
    sh["exp_v"] = np.ascontiguousarray(inp["expert_v"][0], f)
    return sh


def make_in_maps(inp, S_half, n_cores):
    sh = prep_shared(inp)
    x = np.asarray(inp["x"], np.float32)
    maps = []
    for c in range(n_cores):
        b, hf = c // 2, c % 2
        m = dict(sh)
        m["x_main"] = np.ascontiguousarray(x[b, hf * S_half:(hf + 1) * S_half])
        m["x_prev"] = np.ascontiguousarray(x[b, 0:S_half]) if hf == 1 else np.zeros((S_half, D), np.float32)
        maps.append(m)
    return maps


def kernel(**inputs):
    x = np.asarray(inputs["x"])
    bsz, seq, _ = x.shape
    S_half = seq // 2
    n_cores = 2 * bsz
    nc = build_nc(S_half)
    maps = make_in_maps(inputs, S_half, n_cores)
    res = run_bass_kernel_spmd(nc, maps, core_ids=list(range(n_cores)))
    out = np.zeros((bsz, seq, D), np.float32)
    for c in range(n_cores):
        b, hf = c // 2, c % 2
        out[b, hf * S_half:(hf + 1) * S_half] = res.results[c]["out"]
    return out
```

```python
import math
import numpy as np
import concourse.bass as bass
import concourse.mybir as mybir
from concourse.bass_utils import run_bass_kernel_spmd

F32 = mybir.dt.float32
BF16 = mybir.dt.bfloat16
U32 = mybir.dt.uint32
I32 = mybir.dt.int32
ALU = mybir.AluOpType
AF = mybir.ActivationFunctionType
AX = mybir.AxisListType

D = 1024
CONV_CH = 768
SSM_CH = 256
G = 16
P = 64
IN_COLS = 4608
NQ = 2048
NE = 16384
TB = 128
EPS = 1e-6
TWO_PI = 2.0 * math.pi
SKIP_PRECAST = False


class Buf:
    __slots__ = ("name", "w", "r")

    def __init__(self, name=""):
        self.name = name
        self.w = None
        self.r = []


class Sched:
    ENG = ("pe", "dve", "act", "pool", "sp")

    def __init__(self, nc, n_dma_sems=20):
        self.nc = nc
        self.stack = []
        self.streams = {e: [] for e in self.ENG}
        self.cnt = {e: 0 for e in self.ENG}
        self.sem = {}
        for e in self.ENG:
            g = nc.semaphore("s_" + e)
            self.sem[e] = g.__enter__()
            self.stack.append(g)
        self.known = {e: {} for e in self.ENG}
        self.dma_sems = {}
        for q in ("sp", "pool", "act"):
            self.dma_sems[q] = []
            for i in range({"sp": n_dma_sems, "pool": 48, "act": 8}[q]):
                g = nc.semaphore("d%s_%d" % (q, i))
                self.sem["d%s%d" % (q, i)] = g.__enter__()
                self.stack.append(g)
                self.dma_sems[q].append("d%s%d" % (q, i))
        self.dma_cnt = {k: 0 for q in self.dma_sems for k in self.dma_sems[q]}
        self.dma_rr = {"sp": 0, "pool": 0, "act": 0}
        self.n_inst = 0

    def _need(self, eng, deps):
        out = []
        for key, val in deps:
            if val <= 0:
                continue
            if self.known[eng].get(key, 0) >= val:
                continue
            self.known[eng][key] = val
            out.append((key, val))
        return out

    def _deps(self, reads, writes):
        deps = {}
        for b in reads:
            if b.w is not None:
                k, v = b.w
                deps[k] = max(deps.get(k, 0), v)
        for b in writes:
            if b.w is not None:
                k, v = b.w
                deps[k] = max(deps.get(k, 0), v)
            for k, v in b.r:
                deps[k] = max(deps.get(k, 0), v)
        return list(deps.items())

    def op(self, eng, fn, reads=(), writes=()):
        deps = self._deps(reads, writes)
        if eng == "pe":
            deps = [(k, v) for k, v in deps if k != "pe"]
        waits = self._need(eng, deps)
        self.cnt[eng] += 1
        n = self.cnt[eng]
        self.streams[eng].append((waits, fn, (eng, 1)))
        for b in reads:
            b.r.append((eng, n))
            if len(b.r) > 64:
                b.r = b.r[-64:] if False else self._compact(b.r)
        for b in writes:
            b.w = (eng, n)
            b.r = []
        self.n_inst += 1

    @staticmethod
    def _compact(r):
        m = {}
        for k, v in r:
            m[k] = max(m.get(k, 0), v)
        return list(m.items())

    def dma(self, queue, fn, reads=(), writes=()):
        deps = self._deps(reads, writes)
        pool_ = self.dma_sems[queue]
        key = pool_[self.dma_rr[queue] % len(pool_)]
        self.dma_rr[queue] += 1
        prev = self.dma_cnt[key]
        if prev > 0:
            deps.append((key, prev))
        waits = self._need(queue, deps)
        val = prev + 16
        self.dma_cnt[key] = val
        self.streams[queue].append((waits, fn, (key, 16)))
        for b in reads:
            b.r.append((key, val))
            if len(b.r) > 64:
                b.r = self._compact(b.r)
        for b in writes:
            b.w = (key, val)
            b.r = []
        self.n_inst += 1

    def barrier(self, dma=True):
        deps = [(e, self.cnt[e]) for e in self.ENG] + (list(self.dma_cnt.items()) if dma else [])
        for e in self.ENG:
            waits = self._need(e, [d for d in deps if d[0] != e])
            if waits:
                self.streams[e].append((waits, None, None))

    def final_wait(self, eng="sp"):
        deps = [(e, self.cnt[e]) for e in self.ENG if e != eng] + list(self.dma_cnt.items())
        waits = self._need(eng, deps)
        self.streams[eng].append((waits, None, None))

    def emit(self):
        nc = self.nc
        sems = self.sem
        streams = self.streams

        def run(engine, items):
            for waits, fn, inc in items:
                for key, val in waits:
                    engine.wait_ge(sems[key], val)
                if fn is not None:
                    fn(engine).then_inc(sems[inc[0]], inc[1])

        with nc.Block() as block:
            @block.tensor
            def _(e):
                run(e, streams["pe"])

            @block.vector
            def _(e):
                run(e, streams["dve"])

            @block.scalar
            def _(e):
                run(e, streams["act"])

            @block.gpsimd
            def _(e):
                run(e, streams["pool"])

            @block.sync
            def _(e):
                run(e, streams["sp"])

    def close(self):
        for g in reversed(self.stack):
            g.__exit__(None, None, None)


class Alloc:
    def __init__(self, nc):
        self.nc = nc
        self.stack = []

    def sb(self, name, shape, dt=F32, side=None):
        g = self.nc.sbuf_tensor(name, list(shape), dt, side=side) if side else self.nc.sbuf_tensor(name, list(shape), dt)
        t = g.__enter__()
        self.stack.append(g)
        return t

    def ps(self, name, shape, dt=F32):
        g = self.nc.psum_tensor(name, list(shape), dt)
        t = g.__enter__()
        self.stack.append(g)
        return t

    def close(self):
        for g in reversed(self.stack):
            g.__exit__(None, None, None)
        self.stack = []


def build_nc(S_half, debug=False, do_b=True):
    NB = S_half // TB
    nc = bass.Bass("TRN2", target_bir_lowering=False)

    def din(name, shape, dt=F32):
        return nc.dram_tensor(name, list(shape), dt, kind="ExternalInput").ap()

    x_main = din("x_main", [S_half, D])
    x_prev = din("x_prev", [S_half, D])
    w_in = din("w_in", [D, IN_COLS])
    w_a = din("w_a", [CONV_CH, D])
    w_b = din("w_b", [SSM_CH, D])
    w_out = din("w_out", [D, D])
    w_glu = din("w_glu", [SSM_CH, SSM_CH])
    w_q = din("w_q", [D, NQ])
    keysT = din("keysT", [2, 128, 128])
    g_rep = din("g_rep", [3, 128, D])
    colp = din("colp", [128, 48])
    ssm_pt = din("ssm_pt", [3, 128, G])
    ssm_row = din("ssm_row", [3, 128, G * P])
    bt_in = din("bt_in", [2, 128, G * P])
    ct_in = din("ct_in", [128, G * 128])
    exp_u = din("exp_u", [NE, D])
    exp_v = din("exp_v", [NE, D])
    out_d = nc.dram_tensor("out", [S_half, D], F32, kind="ExternalOutput").ap()
    if debug:
        x1_d = nc.dram_tensor("x1_d", [S_half, D], F32, kind="ExternalOutput").ap()
        dbg_d = nc.dram_tensor("dbg", [128, 2 * S_half], F32, kind="ExternalOutput").ap()
    else:
        x1_d = nc.dram_tensor("x1_d", [S_half, D], F32, kind="Internal").ap()
        dbg_d = None

    uv_d = nc.dram_tensor("uv_d", [NE, 2 * D], BF16, kind="Internal").ap()
    B_uv = Buf("uv_d")
    B_x1d = Buf("x1_d")

    S = Sched(nc)
    op, dma = S.op, S.dma

    P0 = Alloc(nc)
    identf = P0.sb("identf", [128, 128])
    ident = P0.sb("ident", [128, 128], BF16)
    junk = P0.sb("junk", [128, D], BF16)
    stat = P0.sb("stat", [128, 8])
    B_junk = Buf("junk")
    B_stat = Buf("stat")
    ps_tr = P0.ps("ps_tr", [128, 8, 128], BF16)
    B_ptr = Buf("ps_tr")
    NPS = 5
    ps_acc = [P0.ps("ps_acc%d" % i, [128, 512]) for i in range(2)]
    B_pacc = Buf("ps_acc")
    ps_mm = [P0.ps("ps_mm%d" % i, [128, 512]) for i in range(NPS)]
    B_ps = [Buf("ps%d" % i) for i in range(NPS)]
    ps_rr = [0]
    ps_pool = list(range(NPS))

    def next_ps():
        i = ps_pool[ps_rr[0] % len(ps_pool)]
        ps_rr[0] += 1
        return ps_mm[i], B_ps[i]

    def mm_group(out_ap, b_out, pairs, reads):
        n = len(pairs)
        for i, (l, r) in enumerate(pairs):
            op("pe", lambda e, l=l, r=r, i=i: e.matmul(out_ap, lhsT=l, rhs=r, start=(i == 0), stop=(i == n - 1)),
               reads=reads, writes=[b_out])

    def rstd_of(src, b_src, col):
        op("act", lambda e: e.activation(out=junk[:], in_=src, func=AF.Square, accum_out=stat[:, col:col + 1]),
           reads=[b_src, B_stat], writes=[B_junk, B_stat])
        op("dve", lambda e: e.tensor_scalar(out=stat[:, col + 1:col + 2], in0=stat[:, col:col + 1], scalar1=1.0 / D, scalar2=EPS,
                                            op0=ALU.mult, op1=ALU.add), reads=[B_stat], writes=[B_stat])
        op("act", lambda e: e.activation(out=stat[:, col + 2:col + 3], in_=stat[:, col + 1:col + 2], func=AF.Sqrt),
           reads=[B_stat], writes=[B_stat])
        op("dve", lambda e: e.reciprocal(out=stat[:, col + 3:col + 4], in_=stat[:, col + 2:col + 3]), reads=[B_stat], writes=[B_stat])

    def norm_tile(src, b_src, g_ap, b_gconst, dst, b_dst, col):
        rstd_of(src, b_src, col)
        op("dve", lambda e: e.scalar_tensor_tensor(out=dst, in0=src, scalar=stat[:, col + 3:col + 4], in1=g_ap,
                                                   op0=ALU.mult, op1=ALU.mult),
           reads=[b_src, B_stat, b_gconst], writes=[b_dst])

    def transposes(src_bf, b_src, dstT, b_dstT):
        for k in range(8):
            op("pe", lambda e, k=k: e.transpose(out=ps_tr[:, k, :], in_=src_bf[:, k * 128:(k + 1) * 128], identity=ident[:]),
               reads=[b_src, B_id], writes=[B_ptr])
        op("act", lambda e: e.activation(out=dstT[:, :, :], in_=ps_tr[:], func=AF.Copy), reads=[B_ptr], writes=[b_dstT])

    A = Alloc(nc)
    sb = A.sb
    w_in_sb = sb("w_in_sb", [128, 8, IN_COLS], BF16)
    w_a_sb = sb("w_a_sb", [128, 6, D], BF16)
    w_b_sb = sb("w_b_sb", [128, 2, D], BF16)
    w_out_sb = sb("w_out_sb", [128, 8, D], BF16)
    w_glu_sb = sb("w_glu_sb", [128, 2, SSM_CH], BF16)
    B_w = Buf("weights")

    def load_weights():
        for kc in range(8):
            for c in range(3):
                dma("pool", lambda e, kc=kc, c=c: e.dma_start(
                    out=w_in_sb[:, kc, c * 1536:(c + 1) * 1536],
                    in_=w_in[kc * 128:(kc + 1) * 128, c * 1536:(c + 1) * 1536]), writes=[B_w])
        for kc in range(6):
            dma("pool", lambda e, kc=kc: e.dma_start(out=w_a_sb[:, kc, :], in_=w_a[kc * 128:(kc + 1) * 128, :]), writes=[B_w])
        for kc in range(2):
            dma("pool", lambda e, kc=kc: e.dma_start(out=w_b_sb[:, kc, :], in_=w_b[kc * 128:(kc + 1) * 128, :]), writes=[B_w])
            dma("pool", lambda e, kc=kc: e.dma_start(out=w_glu_sb[:, kc, :], in_=w_glu[kc * 128:(kc + 1) * 128, :]), writes=[B_w])
        for kc in range(8):
            dma("pool", lambda e, kc=kc: e.dma_start(out=w_out_sb[:, kc, :], in_=w_out[kc * 128:(kc + 1) * 128, :]), writes=[B_w])


    g_sb = sb("g_sb", [128, D])
    colp_sb = sb("colp_sb", [128, 48])
    B_c = Buf("consts")
    dma("sp", lambda e: e.dma_start(out=g_sb[:], in_=g_rep[0]), writes=[B_c])
    dma("sp", lambda e: e.dma_start(out=colp_sb[:], in_=colp[:, :]), writes=[B_c])
    CW = lambda c, k: colp_sb[:, 16 + c * 3 + k:17 + c * 3 + k]
    CBIAS = lambda c: colp_sb[:, 34 + c:35 + c]

    B_id = Buf("ident")
    op("pool", lambda e: e.iota(identf[:], [[1, 128]], base=0, channel_multiplier=-1,
                                allow_small_or_imprecise_dtypes=True), writes=[B_id])
    op("dve", lambda e: e.tensor_single_scalar(out=ident[:], in_=identf[:], scalar=0.0, op=ALU.is_equal),
       reads=[B_id], writes=[B_id])

    r_p = sb("r_p", [128, G])
    ctab = sb("ctab", [128, G, TB + 1])
    stab = sb("stab", [128, G, TB + 1])
    nsT = sb("nsT", [128, G])
    bpq = sb("bpq", [128, G, 2, 128], BF16)
    ctb = sb("ctb", [128, G, 128], BF16)
    B_s = Buf("ssmc")
    LP = 2 if NB % 2 == 0 else 1
    AXr = Alloc(nc)
    PT = AXr.sb("PT", [128, LP, G, 128], BF16, side="right")
    CRI = AXr.sb("CRI", [128, 2, G * P], F32, side="right")
    Mst = AXr.sb("Mst", [128, G, 128], F32, side="right")
    A00 = Alloc(nc)
    ctf = A00.sb("ctf", [128, G * 128])
    dma("sp", lambda e: e.dma_start(out=ctf[:], in_=ct_in[:, :]), writes=[B_s])
    op("dve", lambda e: e.tensor_scalar(out=ctf[64:128, :], in0=ctf[64:128, :], scalar1=-1.0, scalar2=None, op0=ALU.mult), reads=[B_s], writes=[B_s])
    op("dve", lambda e: e.tensor_copy(out=ctb[:].rearrange("p g m -> p (g m)"), in_=ctf[:]), reads=[B_s], writes=[B_s])
    S.barrier(dma=False)
    A00.close()
    A0 = Alloc(nc)
    sb0 = A0.sb
    spt = sb0("spt", [128, 3, G])
    HW = 8 * P
    srow = sb0("srow", [128, 3, HW])
    bt = sb0("bt", [128, 2, HW])
    ecol = sb0("ecol", [128, 4])
    dma("sp", lambda e: e.dma_start(out=spt[:], in_=ssm_pt.rearrange("a p g -> p a g")), writes=[B_s])
    for l in range(LP):
        op("pool", lambda e, l=l: e.iota(ecol[:, l:l + 1], [[0, 1]], base=128 * l + 127, channel_multiplier=-1,
                                         allow_small_or_imprecise_dtypes=True), writes=[B_s])

    thp_p = sb0("thp_p", [128, G])
    tmp_p = sb0("tmp_p", [128, 4, G])
    iota_t = sb0("iota_t", [128, TB + 1])
    ytmp = sb0("ytmp", [128, 3, TB + 1])
    yti = sb0("yti", [128, TB + 1], I32)
    rti = sb0("rti", [128, HW], I32)
    rt2 = sb0("rt2", [128, HW])
    R = [sb0("rw%d" % i, [128, HW]) for i in range(8)]

    def expm1_poly(eng, out, z, t):
        op(eng, lambda e: e.tensor_scalar(out=out, in0=z, scalar1=1.0 / 720, scalar2=1.0 / 120, op0=ALU.mult, op1=ALU.add), reads=[B_s], writes=[B_s])
        for c in (1.0 / 24, 1.0 / 6, 0.5, 1.0):
            op(eng, lambda e: e.tensor_tensor(out=t, in0=out, in1=z, op=ALU.mult), reads=[B_s], writes=[B_s])
            op(eng, lambda e, c=c: e.tensor_scalar(out=out, in0=t, scalar1=c, scalar2=None, op0=ALU.add), reads=[B_s], writes=[B_s])
        op(eng, lambda e: e.tensor_tensor(out=t, in0=out, in1=z, op=ALU.mult), reads=[B_s], writes=[B_s])
        op(eng, lambda e: e.tensor_copy(out=out, in_=t), reads=[B_s], writes=[B_s])

    def sin_turns(out, y, off, t, ti, t2):
        op("dve", lambda e: e.tensor_scalar(out=t, in0=y, scalar1=8.0 + off, scalar2=None, op0=ALU.add), reads=[B_s], writes=[B_s])
        op("dve", lambda e: e.tensor_copy(out=ti, in_=t), reads=[B_s], writes=[B_s])
        op("dve", lambda e: e.tensor_copy(out=t2, in_=ti), reads=[B_s], writes=[B_s])
        op("dve", lambda e: e.tensor_tensor(out=t, in0=t, in1=t2, op=ALU.subtract), reads=[B_s], writes=[B_s])
        op("dve", lambda e: e.tensor_single_scalar(out=t2, in_=t, scalar=0.5, op=ALU.is_gt), reads=[B_s], writes=[B_s])
        op("dve", lambda e: e.tensor_tensor(out=t, in0=t, in1=t2, op=ALU.subtract), reads=[B_s], writes=[B_s])
        op("act", lambda e: e.activation(out=out, in_=t, func=AF.Sin, scale=TWO_PI), reads=[B_s], writes=[B_s])

    dt_p = tmp_p[:, 0, :]
    z_p = tmp_p[:, 1, :]
    e_p = tmp_p[:, 2, :]
    t_p = tmp_p[:, 3, :]
    op("act", lambda e: e.activation(out=dt_p, in_=spt[:, 2, :], func=AF.Exp), reads=[B_s], writes=[B_s])
    op("dve", lambda e: e.tensor_tensor(out=z_p, in0=spt[:, 0, :], in1=dt_p, op=ALU.mult), reads=[B_s], writes=[B_s])
    expm1_poly("dve", e_p, z_p, t_p)
    op("dve", lambda e: e.tensor_scalar(out=r_p[:], in0=e_p, scalar1=1.0, scalar2=None, op0=ALU.add), reads=[B_s], writes=[B_s])
    op("dve", lambda e: e.tensor_tensor(out=t_p, in0=spt[:, 1, :], in1=dt_p, op=ALU.mult), reads=[B_s], writes=[B_s])
    op("dve", lambda e: e.tensor_scalar(out=thp_p[:], in0=t_p, scalar1=1.0 / TWO_PI, scalar2=None, op0=ALU.mult), reads=[B_s], writes=[B_s])
    op("pool", lambda e: e.iota(iota_t[:], [[1, TB + 1]], base=0, channel_multiplier=0,
                                allow_small_or_imprecise_dtypes=True), writes=[B_s])
    load_weights()
    for g in range(G):
        op("dve", lambda e, g=g: e.tensor_scalar(out=ytmp[:, 0, :], in0=iota_t[:], scalar1=thp_p[:, g:g + 1], scalar2=None, op0=ALU.mult), reads=[B_s], writes=[B_s])
        sin_turns(stab[:, g, :], ytmp[:, 0, :], 0.0, ytmp[:, 1, :], yti[:], ytmp[:, 2, :])
        sin_turns(ctab[:, g, :], ytmp[:, 0, :], 0.25, ytmp[:, 1, :], yti[:], ytmp[:, 2, :])
    op("dve", lambda e: e.tensor_scalar(out=nsT[:], in0=stab[:, :, TB], scalar1=-1.0, scalar2=None, op0=ALU.mult), reads=[B_s], writes=[B_s])

    for hh in range(2):
        gs = slice(8 * hh, 8 * hh + 8)
        for a in range(3):
            dma("sp", lambda e, a=a, hh=hh: e.dma_start(out=srow[:, a, :], in_=ssm_row[a][:, hh * HW:(hh + 1) * HW]), reads=[B_s], writes=[B_s])
        for a in range(2):
            dma("sp", lambda e, a=a, hh=hh: e.dma_start(out=bt[:, a, :], in_=bt_in[a][:, hh * HW:(hh + 1) * HW]), reads=[B_s], writes=[B_s])
        lr_r, li_r = srow[:, 0, :], srow[:, 1, :]
        dt_r, z_r, em1_r, t_r, thp_r, sn_r, cs_r, sh_r = [R[i][:] for i in range(8)]
        op("act", lambda e: e.activation(out=dt_r, in_=srow[:, 2, :], func=AF.Exp), reads=[B_s], writes=[B_s])
        op("dve", lambda e: e.tensor_tensor(out=z_r, in0=lr_r, in1=dt_r, op=ALU.mult), reads=[B_s], writes=[B_s])
        expm1_poly("dve", em1_r, z_r, t_r)
        op("dve", lambda e: e.tensor_tensor(out=t_r, in0=li_r, in1=dt_r, op=ALU.mult), reads=[B_s], writes=[B_s])
        op("dve", lambda e: e.tensor_scalar(out=thp_r, in0=t_r, scalar1=1.0 / TWO_PI, scalar2=None, op0=ALU.mult), reads=[B_s], writes=[B_s])
        y_r, mg_r = R[5][:], R[0][:]
        for l in range(LP):
            op("dve", lambda e, l=l: e.tensor_scalar(out=y_r, in0=thp_r, scalar1=ecol[:, l:l + 1], scalar2=None, op0=ALU.mult), reads=[B_s], writes=[B_s])
            sin_turns(R[7][:], y_r, 0.0, t_r, rti[:], rt2[:])
            sin_turns(R[6][:], y_r, 0.25, t_r, rti[:], rt2[:])
            op("act", lambda e, l=l: e.activation(out=mg_r, in_=z_r, func=AF.Exp, scale=ecol[:, l:l + 1]), reads=[B_s], writes=[B_s])
            op("dve", lambda e, l=l, gs=gs: e.tensor_tensor(out=PT[:, l, gs, 0:64], in0=mg_r.rearrange("p (g s) -> p g s", g=8), in1=R[6][:].rearrange("p (g s) -> p g s", g=8), op=ALU.mult), reads=[B_s], writes=[B_s])
            op("dve", lambda e, l=l, gs=gs: e.tensor_tensor(out=PT[:, l, gs, 64:128], in0=mg_r.rearrange("p (g s) -> p g s", g=8), in1=R[7][:].rearrange("p (g s) -> p g s", g=8), op=ALU.mult), reads=[B_s], writes=[B_s])
        op("dve", lambda e: e.tensor_scalar(out=y_r, in0=thp_r, scalar1=float(128 * LP), scalar2=None, op0=ALU.mult), reads=[B_s], writes=[B_s])
        sin_turns(R[7][:], y_r, 0.0, t_r, rti[:], rt2[:])
        sin_turns(R[6][:], y_r, 0.25, t_r, rti[:], rt2[:])
        op("act", lambda e: e.activation(out=mg_r, in_=z_r, func=AF.Exp, scale=float(128 * LP)), reads=[B_s], writes=[B_s])
        op("dve", lambda e, hh=hh: e.tensor_tensor(out=CRI[:, 0, hh * HW:(hh + 1) * HW], in0=mg_r, in1=R[6][:], op=ALU.mult), reads=[B_s], writes=[B_s])
        op("dve", lambda e, hh=hh: e.tensor_tensor(out=CRI[:, 1, hh * HW:(hh + 1) * HW], in0=mg_r, in1=R[7][:], op=ALU.mult), reads=[B_s], writes=[B_s])
        op("act", lambda e: e.activation(out=dt_r, in_=srow[:, 2, :], func=AF.Exp), reads=[B_s], writes=[B_s])
        sin_turns(sn_r, thp_r, 0.0, t_r, rti[:], rt2[:])
        sin_turns(cs_r, thp_r, 0.25, t_r, rti[:], rt2[:])
        op("dve", lambda e: e.tensor_scalar(out=dt_r, in0=thp_r, scalar1=0.5, scalar2=None, op0=ALU.mult), reads=[B_s], writes=[B_s])
        sin_turns(sh_r, dt_r, 0.0, t_r, rti[:], rt2[:])
        op("dve", lambda e: e.tensor_tensor(out=z_r, in0=em1_r, in1=cs_r, op=ALU.mult), reads=[B_s], writes=[B_s])
        op("dve", lambda e: e.tensor_tensor(out=t_r, in0=sh_r, in1=sh_r, op=ALU.mult), reads=[B_s], writes=[B_s])
        op("dve", lambda e: e.scalar_tensor_tensor(out=z_r, in0=t_r, scalar=-2.0, in1=z_r, op0=ALU.mult, op1=ALU.add), reads=[B_s], writes=[B_s])
        op("dve", lambda e: e.scalar_tensor_tensor(out=sn_r, in0=em1_r, scalar=1.0, in1=sn_r, op0=ALU.add, op1=ALU.mult), reads=[B_s], writes=[B_s])
        nr_r, ni_r = z_r, sn_r
        op("dve", lambda e: e.tensor_tensor(out=cs_r, in0=lr_r, in1=lr_r, op=ALU.mult), reads=[B_s], writes=[B_s])
        op("dve", lambda e: e.tensor_tensor(out=t_r, in0=li_r, in1=li_r, op=ALU.mult), reads=[B_s], writes=[B_s])
        op("dve", lambda e: e.tensor_tensor(out=cs_r, in0=cs_r, in1=t_r, op=ALU.add), reads=[B_s], writes=[B_s])
        op("dve", lambda e: e.reciprocal(out=cs_r, in_=cs_r), reads=[B_s], writes=[B_s])
        rden = cs_r
        op("dve", lambda e: e.tensor_tensor(out=em1_r, in0=nr_r, in1=lr_r, op=ALU.mult), reads=[B_s], writes=[B_s])
        op("dve", lambda e: e.tensor_tensor(out=t_r, in0=ni_r, in1=li_r, op=ALU.mult), reads=[B_s], writes=[B_s])
        op("dve", lambda e: e.tensor_tensor(out=em1_r, in0=em1_r, in1=t_r, op=ALU.add), reads=[B_s], writes=[B_s])
        op("dve", lambda e: e.tensor_tensor(out=em1_r, in0=em1_r, in1=rden, op=ALU.mult), reads=[B_s], writes=[B_s])
        op("dve", lambda e: e.tensor_tensor(out=sh_r, in0=ni_r, in1=lr_r, op=ALU.mult), reads=[B_s], writes=[B_s])
        op("dve", lambda e: e.tensor_tensor(out=t_r, in0=nr_r, in1=li_r, op=ALU.mult), reads=[B_s], writes=[B_s])
        op("dve", lambda e: e.tensor_tensor(out=sh_r, in0=sh_r, in1=t_r, op=ALU.subtract), reads=[B_s], writes=[B_s])
        op("dve", lambda e: e.tensor_tensor(out=sh_r, in0=sh_r, in1=rden, op=ALU.mult), reads=[B_s], writes=[B_s])
        k_re, k_im = em1_r, sh_r
        op("dve", lambda e: e.tensor_tensor(out=z_r, in0=k_re, in1=bt[:, 0, :], op=ALU.mult), reads=[B_s], writes=[B_s])
        op("dve", lambda e: e.tensor_tensor(out=t_r, in0=k_im, in1=bt[:, 1, :], op=ALU.mult), reads=[B_s], writes=[B_s])
        op("dve", lambda e: e.tensor_tensor(out=z_r, in0=z_r, in1=t_r, op=ALU.subtract), reads=[B_s], writes=[B_s])
        op("dve", lambda e: e.tensor_tensor(out=sn_r, in0=k_re, in1=bt[:, 1, :], op=ALU.mult), reads=[B_s], writes=[B_s])
        op("dve", lambda e: e.tensor_tensor(out=t_r, in0=k_im, in1=bt[:, 0, :], op=ALU.mult), reads=[B_s], writes=[B_s])
        op("dve", lambda e: e.tensor_tensor(out=sn_r, in0=sn_r, in1=t_r, op=ALU.add), reads=[B_s], writes=[B_s])
        bre3 = R[1][:].rearrange("p (g s) -> p g s", g=8)
        bim3 = R[5][:].rearrange("p (g s) -> p g s", g=8)
        op("dve", lambda e, gs=gs: e.tensor_copy(out=bpq[:, gs, 0, 0:64], in_=bre3), reads=[B_s], writes=[B_s])
        op("dve", lambda e, gs=gs: e.tensor_copy(out=bpq[:, gs, 0, 64:128], in_=bim3), reads=[B_s], writes=[B_s])
        op("dve", lambda e, gs=gs: e.tensor_copy(out=bpq[:, gs, 1, 0:64], in_=bim3), reads=[B_s], writes=[B_s])
        op("dve", lambda e, gs=gs: e.tensor_scalar(out=bpq[:, gs, 1, 64:128], in0=bre3, scalar1=-1.0, scalar2=None, op0=ALU.mult), reads=[B_s], writes=[B_s])

    S.barrier()
    A0.close()
    x_tok = [sb("x_tok%d" % i, [128, D]) for i in range(3)]
    B_x = [Buf("x_tok%d" % i) for i in range(3)]
    h_bf = sb("h_bf", [128, 1, D], BF16)
    B_h = [Buf("h0")]
    hT2 = [sb("hT%d" % i, [128, 8, TB], BF16) for i in range(2)]
    B_hT2 = [Buf("hT0"), Buf("hT1")]
    v_sb = sb("v_sb", [128, 6, TB + 2])
    B_v = [Buf("v%d" % c) for c in range(6)]
    cc_sb = sb("cc_sb", [128, 2, TB])
    B_cc = [Buf("cc0"), Buf("cc1")]
    init = sb("init", [128, G, 2])
    B_init = [Buf("init%d" % g) for g in range(G)]
    op("pool", lambda e: e.memset(v_sb[:], 0.0), writes=B_v)
    op("pool", lambda e: e.memset(init[:], 0.0), writes=B_init)

    blocks = [("pre", i) for i in range(NB)] + [("main", i) for i in range(NB)]

    def proj(par, mt):
        ps, b_ps = next_ps()
        mm_group(ps[:, 0:TB], b_ps,
                 [(w_in_sb[:, k, mt * 128:(mt + 1) * 128], hT2[par][:, k, :]) for k in range(8)], [B_w, B_hT2[par]])
        return ps, b_ps

    def alloc_main():
        g = {}
        g["ct1"] = sb("ct1", [128, 2, TB]); g["ct2"] = sb("ct2", [128, 2, TB])
        g["outA2"] = [sb("outA%d" % i, [128, 6, TB], BF16) for i in range(2)]
        g["uT2"] = [sb("uT%d" % i, [128, 2, TB], BF16) for i in range(2)]
        g["u_f2"] = [sb("u_f%d" % i, [128, 2, TB]) for i in range(2)]
        g["gates2"] = [sb("gates%d" % i, [128, 16, TB], BF16) for i in range(2)]
        g["st"] = [[sb("st%d_%d" % (i, j), [128, TB]) for j in range(NST)] for i in range(NSB)]
        g["s_bf"] = sb("s_bf", [128, NSB, TB], BF16)
        g["itmp"] = sb("itmp", [128, G, 2])
        g["ctmp"] = sb("ctmp", [128, 4, G])
        g["y_sb"] = sb("y_sb", [128, 2, TB]); g["z_f"] = sb("z_f", [128, 2, TB]); g["z_b"] = sb("z_b", [128, 2, TB], BF16)
        g["sg"] = sb("sg", [128, 2, TB]); g["outB"] = sb("outB", [128, 2, TB], BF16)
        g["m1"] = sb("m1", [128, 1, TB]); g["m2"] = sb("m2", [128, 1, TB]); g["merged"] = sb("merged", [128, 8, TB], BF16)
        g["stg32"] = [sb("stg32_%d" % i, [128, 512]) for i in range(2)]
        g["stgb"] = [sb("stgb_%d" % i, [128, 512], BF16) for i in range(2)]
        return g

    NSB = 3
    NST = 10
    B_ct = [Buf("ct0"), Buf("ct1")]
    B_outA2 = [Buf("outA0"), Buf("outA1")]
    B_u2 = [Buf("u0"), Buf("u1")]
    B_g2 = [Buf("gates0"), Buf("gates1")]
    B_st = [[Buf("st%d_%d" % (i, j)) for j in range(NST)] for i in range(NSB)]
    B_sbf = [Buf("sbf%d" % i) for i in range(NSB)]
    B_y = [Buf("y0"), Buf("y1")]
    B_outB = Buf("outB")
    B_m = [Buf("m0"), Buf("m1")]
    B_mg = Buf("merged")
    B_wl = Buf("wl")
    B_ctmp = Buf("ctmp")
    B_yacc = [Buf("yacc0"), Buf("yacc1")]
    B_stg = [Buf("stg0"), Buf("stg1")]
    pre_chunks = [(tb, ch, hf) for tb in range(2) for ch in range(NE // 128) for hf in range(2)]
    pre_i = [0]
    n_pre_per_blk = -(-len(pre_chunks) // NB)
    MA = {}
    B_stg32 = [Buf("stg32_0"), Buf("stg32_1")]

    def precast_gen(n):
        if SKIP_PRECAST:
            return
        stg32, stgb = MA["stg32"], MA["stgb"]

        def load(i):
            tb, ch, hf = pre_chunks[i]
            src = (exp_u if tb == 0 else exp_v)[ch * 128:(ch + 1) * 128, hf * 512:(hf + 1) * 512]
            dma("act", lambda e, src=src, j=i % 2: e.dma_start(out=stg32[j][:], in_=src), writes=[B_stg32[i % 2]])

        for _ in range(n):
            i = pre_i[0]
            if i >= len(pre_chunks):
                return
            pre_i[0] += 1
            if i == 0:
                load(0)
            if i + 1 < len(pre_chunks):
                pass
            tb, ch, hf = pre_chunks[i]
            j = i % 2
            dst = uv_d[ch * 128:(ch + 1) * 128, tb * D + hf * 512:tb * D + (hf + 1) * 512]
            op("act", lambda e, j=j: e.activation(out=stgb[j][:], in_=stg32[j][:], func=AF.Copy), reads=[B_stg32[j]], writes=[B_stg[j]])
            if i + 1 < len(pre_chunks):
                load(i + 1)
            dma("act", lambda e, dst=dst, j=j: e.dma_start(out=dst, in_=stgb[j][:]), reads=[B_stg[j]], writes=[B_uv])
            yield

    def load_norm_T(bi):
        kind, blk = blocks[bi]
        par = bi % 2
        xsrc = x_main if kind == "main" else x_prev
        xt, b_xt = x_tok[bi % 3], B_x[bi % 3]
        dma("sp", lambda e: e.dma_start(out=xt[:], in_=xsrc[blk * TB:(blk + 1) * TB, :]), writes=[b_xt])
        norm_tile(xt[:], b_xt, g_sb[:], B_c, h_bf[:, 0, :], B_h[0], 4 * par)
        transposes(h_bf[:, 0, :], B_h[0], hT2[par], B_hT2[par])

    def front(bi):
        kind, blk = blocks[bi]
        par = bi % 2
        load_norm_T(bi)
        yield
        for m in range(2):
            ps_u, b_u = proj(par, 18 + m)
            op("act", lambda e, m=m, ps_u=ps_u: e.activation(out=u_f2[par][:, m, :], in_=ps_u[:, 0:TB], func=AF.Copy), reads=[b_u], writes=[B_u2[par]])
            op("act", lambda e, m=m, ps_u=ps_u: e.activation(out=uT2[par][:, m, :], in_=ps_u[:, 0:TB], func=AF.Copy), reads=[b_u], writes=[B_u2[par]])
            yield

    def delayed(gen, n):
        for _ in range(n):
            yield
        for _ in gen:
            yield

    def convgate(bi):
        kind, blk = blocks[bi]
        par = bi % 2
        is_main = kind == "main"
        last_pre = (kind == "pre" and blk == NB - 1)
        if is_main:
            outA, B_outA = outA2[par], B_outA2[par]
            gates, B_g = gates2[par], B_g2[par]
        if is_main or last_pre:
            for c in range(6):
                j = c % 2
                ps_c, b_c = proj(par, 6 + c)
                ps_x, b_xx = proj(par, 12 + c)
                op("act", lambda e, ps_c=ps_c, j=j: e.activation(out=cc_sb[:, j, :], in_=ps_c[:, 0:TB], func=AF.Copy),
                   reads=[b_c], writes=[B_cc[j]])
                op("dve", lambda e, ps_x=ps_x, j=j, c=c: e.tensor_tensor(out=v_sb[:, c, 2:TB + 2], in0=cc_sb[:, j, :], in1=ps_x[:, 0:TB], op=ALU.mult),
                   reads=[B_cc[j], b_xx], writes=[B_v[c]])
                yield
                if is_main:
                    ps_b, b_b = proj(par, c)
                    op("pool", lambda e, c=c, j=j: e.tensor_scalar(out=ct1[:, j, :], in0=v_sb[:, c, 2:TB + 2], scalar1=CW(c, 2), scalar2=CBIAS(c),
                                                                   op0=ALU.mult, op1=ALU.add), reads=[B_v[c], B_c], writes=[B_ct[j]])
                    op("dve", lambda e, c=c, j=j: e.scalar_tensor_tensor(out=ct2[:, j, :], in0=v_sb[:, c, 1:TB + 1], scalar=CW(c, 1), in1=ct1[:, j, :],
                                                                         op0=ALU.mult, op1=ALU.add), reads=[B_v[c], B_c, B_ct[j]], writes=[B_ct[j]])
                    op("dve", lambda e, c=c, j=j: e.scalar_tensor_tensor(out=ct1[:, j, :], in0=v_sb[:, c, 0:TB], scalar=CW(c, 0), in1=ct2[:, j, :],
                                                                         op0=ALU.mult, op1=ALU.add), reads=[B_v[c], B_c, B_ct[j]], writes=[B_ct[j]])
                    op("dve", lambda e, c=c, j=j, ps_b=ps_b: e.tensor_tensor(out=outA[:, c, :], in0=ct1[:, j, :], in1=ps_b[:, 0:TB], op=ALU.mult),
                       reads=[B_ct[j], b_b], writes=[B_outA])
                op("pool", lambda e, c=c: e.tensor_copy(out=v_sb[:, c, 0:2], in_=v_sb[:, c, TB:TB + 2]), reads=[B_v[c]], writes=[B_v[c]])
                yield
        if is_main:
            for t in range(16):
                ps_g, b_gg = proj(par, 20 + t)
                op("act", lambda e, t=t, ps_g=ps_g: e.activation(out=gates[:, t, :], in_=ps_g[:, 0:TB], func=AF.Sigmoid,
                                                                 bias=colp_sb[:, t:t + 1], scale=1.0), reads=[b_gg, B_c], writes=[B_g])
                if t % 2 == 1:
                    yield

    def ssm(bi):
        kind, blk = blocks[bi]
        par = bi % 2
        is_main = kind == "main"
        uT, B_u = uT2[par], B_u2[par]
        pq = {}

        def stage0(g):
            ps_p, b_p = ps_mm[3 + (g % 2)], B_ps[3 + (g % 2)]
            m = g // 8
            mm_group(ps_p[:, 0:TB], b_p, [(bpq[:, g, 0, :], uT[:, m, :])], [B_s, B_u])
            mm_group(ps_p[:, TB:2 * TB], b_p, [(bpq[:, g, 1, :], uT[:, m, :])], [B_s, B_u])
            pq[g] = (ps_p, b_p)

        def bufs(g):
            si = g % NSB
            return [st[si][j][:] for j in range(NST)], B_st[si], si

        def stage1(g):
            (R1, R2, W1, W2, T1, T2, T3, T4, T5, T6), (bR1, bR2, bW1, bW2, bT1, bT2, bT3, bT4, bT5, bT6), si = bufs(g)
            ps_p, b_p = pq.pop(g)
            Pp, Qp = ps_p[:, 0:TB], ps_p[:, TB:2 * TB]
            C_g, S_g = ctab[:, g, 0:TB], stab[:, g, 0:TB]
            op("dve", lambda e: e.tensor_tensor(out=T1, in0=C_g, in1=Pp, op=ALU.mult), reads=[B_s, b_p], writes=[bT1])
            op("dve", lambda e: e.tensor_tensor(out=T2, in0=S_g, in1=Qp, op=ALU.mult), reads=[B_s, b_p], writes=[bT2])
            op("dve", lambda e: e.tensor_tensor(out=T3, in0=C_g, in1=Qp, op=ALU.mult), reads=[B_s, b_p], writes=[bT3])
            op("dve", lambda e: e.tensor_tensor(out=T4, in0=S_g, in1=Pp, op=ALU.mult), reads=[B_s, b_p], writes=[bT4])

        def stage2(g):
            (R1, R2, W1, W2, T1, T2, T3, T4, T5, T6), (bR1, bR2, bW1, bW2, bT1, bT2, bT3, bT4, bT5, bT6), si = bufs(g)
            op("dve", lambda e: e.tensor_tensor(out=R1, in0=T1, in1=T2, op=ALU.add), reads=[bT1, bT2], writes=[bR1])
            op("pool", lambda e: e.tensor_tensor(out=R2, in0=T3, in1=T4, op=ALU.subtract), reads=[bT3, bT4], writes=[bR2])

        def stage3(g):
            (R1, R2, W1, W2, T1, T2, T3, T4, T5, T6), (bR1, bR2, bW1, bW2, bT1, bT2, bT3, bT4, bT5, bT6), si = bufs(g)
            rb = r_p[:, g:g + 1].to_broadcast([128, TB])
            op("dve", lambda e: e.tensor_tensor_scan(out=W1, data0=rb, data1=R1, initial=init[:, g, 0:1], op0=ALU.mult, op1=ALU.add),
               reads=[B_s, bR1, B_init[g]], writes=[bW1])
            op("dve", lambda e: e.tensor_tensor_scan(out=W2, data0=rb, data1=R2, initial=init[:, g, 1:2], op0=ALU.mult, op1=ALU.add),
               reads=[B_s, bR2, B_init[g]], writes=[bW2])

        def stage4(g):
            (R1, R2, W1, W2, T1, T2, T3, T4, T5, T6), (bR1, bR2, bW1, bW2, bT1, bT2, bT3, bT4, bT5, bT6), si = bufs(g)
            C_g, S_g = ctab[:, g, 0:TB], stab[:, g, 0:TB]
            op("act", lambda e: e.activation(out=itmp[:, g, 0:1], in_=W1[:, TB - 1:TB], func=AF.Copy), reads=[bW1], writes=[B_wl])
            op("act", lambda e: e.activation(out=itmp[:, g, 1:2], in_=W2[:, TB - 1:TB], func=AF.Copy), reads=[bW2], writes=[B_wl])
            op("pool", lambda e: e.tensor_tensor(out=T5, in0=C_g, in1=W1, op=ALU.mult), reads=[B_s, bW1], writes=[bT5])
            op("pool", lambda e: e.tensor_tensor(out=T6, in0=S_g, in1=W2, op=ALU.mult), reads=[B_s, bW2], writes=[bT6])
            op("pool", lambda e: e.tensor_tensor(out=s_bf[:, si, :], in0=T5, in1=T6, op=ALU.subtract), reads=[bT5, bT6], writes=[B_sbf[si]])

        def stage5(g):
            si = g % NSB
            m = g // 8
            op("pe", lambda e: e.matmul(ps_acc[m][:, 0:TB], lhsT=ctb[:, g, :], rhs=s_bf[:, si, :], start=(g % 8 == 0), stop=(g % 8 == 7)),
               reads=[B_s, B_sbf[si]], writes=[B_yacc[m]])

        stages = [stage0, stage1, stage2, stage3, stage4, stage5]
        for step in range(G + len(stages) - 1):
            for k, fn in enumerate(stages):
                if 0 <= step - k < G:
                    fn(step - k)
            yield
        cT, sT = ctab[:, :, TB], stab[:, :, TB]
        op("dve", lambda e: e.tensor_tensor(out=ctmp[:, 0, :], in0=cT, in1=itmp[:, :, 0], op=ALU.mult), reads=[B_s, B_wl], writes=[B_ctmp])
        op("dve", lambda e: e.tensor_tensor(out=ctmp[:, 1, :], in0=sT, in1=itmp[:, :, 1], op=ALU.mult), reads=[B_s, B_wl], writes=[B_ctmp])
        op("dve", lambda e: e.tensor_tensor(out=init[:, :, 0], in0=ctmp[:, 0, :], in1=ctmp[:, 1, :], op=ALU.subtract), reads=[B_ctmp] + B_init, writes=B_init)
        op("dve", lambda e: e.tensor_tensor(out=ctmp[:, 2, :], in0=cT, in1=itmp[:, :, 1], op=ALU.mult), reads=[B_s, B_wl], writes=[B_ctmp])
        op("dve", lambda e: e.tensor_tensor(out=ctmp[:, 3, :], in0=sT, in1=itmp[:, :, 0], op=ALU.mult), reads=[B_s, B_wl], writes=[B_ctmp])
        op("dve", lambda e: e.tensor_tensor(out=init[:, :, 1], in0=ctmp[:, 2, :], in1=ctmp[:, 3, :], op=ALU.add), reads=[B_ctmp] + B_init, writes=B_init)
        yield

    def tail(bi):
        kind, blk = blocks[bi]
        if kind != "main":
            return
        par = bi % 2
        xt, b_xt = x_tok[bi % 3], B_x[bi % 3]
        outA, B_outA = outA2[par], B_outA2[par]
        gates, B_g = gates2[par], B_g2[par]
        u_f, B_u = u_f2[par], B_u2[par]
        for m in range(2):
            op("dve", lambda e, m=m: e.scalar_tensor_tensor(out=y_sb[:, m, :], in0=u_f[:, m, :], scalar=colp_sb[:, 42 + m:43 + m], in1=ps_acc[m][:, 0:TB],
                                                            op0=ALU.mult, op1=ALU.add), reads=[B_u, B_c, B_yacc[m]], writes=[B_y[m]])
            if debug:
                dma("sp", lambda e, m=m: e.dma_start(out=dbg_d[:, m * S_half + blk * TB:m * S_half + (blk + 1) * TB], in_=y_sb[:, m, :]), reads=[B_y[m]])
            op("act", lambda e, m=m: e.activation(out=z_f[:, m, :], in_=y_sb[:, m, :], func=AF.Gelu), reads=[B_y[m]], writes=[B_y[m]])
            op("pool", lambda e, m=m: e.tensor_copy(out=z_b[:, m, :], in_=z_f[:, m, :]), reads=[B_y[m]], writes=[B_y[m]])
            yield
        for mo in range(2):
            ps_g2, b_g2 = next_ps()
            mm_group(ps_g2[:, 0:TB], b_g2, [(w_glu_sb[:, mi, mo * 128:(mo + 1) * 128], z_b[:, mi, :]) for mi in range(2)], [B_w, B_y[0], B_y[1]])
            op("act", lambda e, mo=mo, ps_g2=ps_g2: e.activation(out=sg[:, mo, :], in_=ps_g2[:, 0:TB], func=AF.Sigmoid,
                                                                 bias=colp_sb[:, 40 + mo:41 + mo], scale=1.0), reads=[b_g2, B_c], writes=[B_y[mo]])
            op("pool", lambda e, mo=mo: e.tensor_tensor(out=outB[:, mo, :], in0=z_f[:, mo, :], in1=sg[:, mo, :], op=ALU.mult), reads=[B_y[mo]], writes=[B_outB])
            yield
        for dm in range(8):
            j = 0
            ps_a, b_a = next_ps()
            mm_group(ps_a[:, 0:TB], b_a, [(w_a_sb[:, c, dm * 128:(dm + 1) * 128], outA[:, c, :]) for c in range(6)], [B_w, B_outA])
            mm_group(ps_a[:, TB:2 * TB], b_a, [(w_b_sb[:, mi, dm * 128:(dm + 1) * 128], outB[:, mi, :]) for mi in range(2)], [B_w, B_outB])
            op("dve", lambda e, dm=dm, j=j, ps_a=ps_a: e.tensor_tensor(out=m1[:, j, :], in0=gates[:, dm, :], in1=ps_a[:, 0:TB], op=ALU.mult), reads=[B_g, b_a], writes=[B_m[j]])
            op("dve", lambda e, dm=dm, j=j, ps_a=ps_a: e.tensor_tensor(out=m2[:, j, :], in0=gates[:, 8 + dm, :], in1=ps_a[:, TB:2 * TB], op=ALU.mult), reads=[B_g, b_a], writes=[B_m[j]])
            op("pool", lambda e, dm=dm, j=j: e.tensor_tensor(out=merged[:, dm, :], in0=m1[:, j, :], in1=m2[:, j, :], op=ALU.add), reads=[B_m[j]], writes=[B_mg])
            yield
        for hf in range(2):
            ps_o, b_o = next_ps()
            mm_group(ps_o[:, :], b_o, [(merged[:, k, :], w_out_sb[:, k, hf * 512:(hf + 1) * 512]) for k in range(8)], [B_w, B_mg])
            op("dve", lambda e, hf=hf, ps_o=ps_o: e.tensor_tensor(out=xt[:, hf * 512:(hf + 1) * 512], in0=xt[:, hf * 512:(hf + 1) * 512],
                                                                  in1=ps_o[:, :], op=ALU.add), reads=[b_xt, b_o], writes=[b_xt])
            yield
        dma("sp", lambda e: e.dma_start(out=x1_d[blk * TB:(blk + 1) * TB, :], in_=xt[:]), reads=[b_xt], writes=[B_x1d])

    def interleave(gens):
        gens = [g for g in gens if g is not None]
        while gens:
            alive = []
            for g in gens:
                try:
                    next(g)
                    alive.append(g)
                except StopIteration:
                    pass
            gens = alive

    Apre = Alloc(nc)
    u_tok2 = [Apre.sb("u_tok%d" % i, [128, 256], BF16) for i in range(2)]
    B_ut = [Buf("ut0"), Buf("ut1")]
    pt_t = [Apre.sb("pt_t%d" % i, [128, 4, 64]) for i in range(4)]
    B_ptt = [Buf("ptt%d" % i) for i in range(4)]
    Uc = Apre.sb("Uc", [128, 8, 128])
    Us = Apre.sb("Us", [128, 8, 128])
    ta = Apre.sb("ta", [128, 8, 64])
    tb_ = Apre.sb("tb_", [128, 8, 64])
    ones2 = Apre.sb("ones2", [128, 2])
    S_sb = Apre.sb("S_sb", [128, 2, G])
    tq = Apre.sb("tq", [128, 2, G])
    B_fin = Buf("fin")
    B_M = Buf("Mst")
    zbanks = [ps_acc[0], ps_acc[1], ps_mm[3], ps_mm[4]]
    B_zb = [Buf("zb%d" % i) for i in range(4)]
    ps_pool[:] = [0, 1, 2]
    op("pool", lambda e: e.memset(Mst[:], 0.0), writes=[B_M])
    op("pool", lambda e: e.memset(ones2[:], 1.0), writes=[B_fin])
    def pre_B(j):
        par = j % 2
        ps_u, b_u = next_ps()
        mm_group(ps_u[:, 0:256], b_u, [(hT2[par][:, k, :], w_in_sb[:, k, 2304:2560]) for k in range(8)], [B_w, B_hT2[par]])
        op("act", lambda e, par=par, ps_u=ps_u: e.activation(out=u_tok2[par][:], in_=ps_u[:, 0:256], func=AF.Copy), reads=[b_u], writes=[B_ut[par]])
        if j == NB - 1:
            for _ in convgate(j):
                pass

    def pre_C(j):
        par = j % 2
        l = (LP - 1) - (j % LP)
        for zi in range(4):
            m = zi // 2
            op("pe", lambda e, zi=zi, m=m, l=l, par=par, j=j: e.matmul(zbanks[zi][:, :], lhsT=u_tok2[par][:, m * 128:(m + 1) * 128],
                                                                   rhs=PT[:, l, 4 * zi:4 * zi + 4, :].rearrange("p g c -> p (g c)"),
                                                                   start=(j % LP == 0), stop=(j % LP == LP - 1)),
               reads=[B_ut[par], B_s], writes=[B_zb[zi]])
        if j % LP == LP - 1:
            for zi in range(4):
                gsl = slice(4 * zi, 4 * zi + 4)
                Mre, Mim = Mst[:, gsl, 0:64], Mst[:, gsl, 64:128]
                cr = CRI[:, 0, 256 * zi:256 * (zi + 1)].rearrange("p (g s) -> p g s", g=4)
                ci = CRI[:, 1, 256 * zi:256 * (zi + 1)].rearrange("p (g s) -> p g s", g=4)
                Z3 = zbanks[zi][:, :].rearrange("p (g c) -> p g c", g=4)
                Zre, Zim = Z3[:, :, 0:64], Z3[:, :, 64:128]
                t1, t2, t3, t4 = [t[:] for t in pt_t]
                b1, b2, b3, b4 = B_ptt
                op("pool", lambda e, cr=cr, Mre=Mre, t1=t1: e.tensor_tensor(out=t1, in0=cr, in1=Mre, op=ALU.mult), reads=[B_s, B_M], writes=[b1])
                op("pool", lambda e, ci=ci, Mim=Mim, t2=t2: e.tensor_tensor(out=t2, in0=ci, in1=Mim, op=ALU.mult), reads=[B_s, B_M], writes=[b2])
                op("pool", lambda e, cr=cr, Mim=Mim, t3=t3: e.tensor_tensor(out=t3, in0=cr, in1=Mim, op=ALU.mult), reads=[B_s, B_M], writes=[b3])
                op("pool", lambda e, ci=ci, Mre=Mre, t4=t4: e.tensor_tensor(out=t4, in0=ci, in1=Mre, op=ALU.mult), reads=[B_s, B_M], writes=[b4])
                op("pool", lambda e, t1=t1, t2=t2: e.tensor_tensor(out=t1, in0=t1, in1=t2, op=ALU.subtract), reads=[b1, b2], writes=[b1])
                op("pool", lambda e, t3=t3, t4=t4: e.tensor_tensor(out=t3, in0=t3, in1=t4, op=ALU.add), reads=[b3, b4], writes=[b3])
                op("dve", lambda e, Mre=Mre, t1=t1, Zre=Zre: e.tensor_tensor(out=Mre, in0=t1, in1=Zre, op=ALU.add), reads=[b1, B_zb[zi]], writes=[B_M])
                op("dve", lambda e, Mim=Mim, t3=t3, Zim=Zim: e.tensor_tensor(out=Mim, in0=t3, in1=Zim, op=ALU.add), reads=[b3, B_zb[zi]], writes=[B_M])

    for k in range(NB + 2):
        if k < NB:
            load_norm_T(k)
        if 0 <= k - 1 < NB:
            pre_B(k - 1)
        if 0 <= k - 2 < NB:
            pre_C(k - 2)
    ps_S, b_S = next_ps()
    for m in range(2):
        gsl = slice(8 * m, 8 * m + 8)
        Bre, Bim = bpq[:, gsl, 0, 0:64], bpq[:, gsl, 0, 64:128]
        Mre, Mim = Mst[:, gsl, 0:64], Mst[:, gsl, 64:128]
        op("dve", lambda e, Bre=Bre, Mre=Mre: e.tensor_tensor(out=ta[:], in0=Bre, in1=Mre, op=ALU.mult), reads=[B_s, B_M], writes=[B_fin])
        op("dve", lambda e, Bim=Bim, Mim=Mim: e.tensor_tensor(out=tb_[:], in0=Bim, in1=Mim, op=ALU.mult), reads=[B_s, B_M], writes=[B_fin])
        op("dve", lambda e: e.tensor_tensor(out=Uc[:, :, 0:64], in0=ta[:], in1=tb_[:], op=ALU.subtract), reads=[B_fin], writes=[B_fin])
        op("dve", lambda e, Bre=Bre, Mim=Mim: e.tensor_tensor(out=ta[:], in0=Bre, in1=Mim, op=ALU.mult), reads=[B_s, B_M, B_fin], writes=[B_fin])
        op("dve", lambda e, Bim=Bim, Mre=Mre: e.tensor_tensor(out=tb_[:], in0=Bim, in1=Mre, op=ALU.mult), reads=[B_s, B_M, B_fin], writes=[B_fin])
        op("dve", lambda e: e.tensor_tensor(out=Uc[:, :, 64:128], in0=ta[:], in1=tb_[:], op=ALU.add), reads=[B_fin], writes=[B_fin])
        op("dve", lambda e: e.tensor_copy(out=Us[:, :, 0:64], in_=Uc[:, :, 64:128]), reads=[B_fin], writes=[B_fin])
        op("dve", lambda e: e.tensor_scalar(out=Us[:, :, 64:128], in0=Uc[:, :, 0:64], scalar1=-1.0, scalar2=None, op0=ALU.mult), reads=[B_fin], writes=[B_fin])
        for gl_ in range(8):
            g = 8 * m + gl_
            op("pe", lambda e, gl_=gl_, g=g: e.matmul(ps_S[:, 2 * g:2 * g + 2], lhsT=Uc[:, gl_, :], rhs=ones2[:, :], start=True, stop=True), reads=[B_fin], writes=[b_S])
            op("pe", lambda e, gl_=gl_, g=g: e.matmul(ps_S[:, 64 + 2 * g:64 + 2 * g + 2], lhsT=Us[:, gl_, :], rhs=ones2[:, :], start=True, stop=True), reads=[B_fin], writes=[b_S])
    for a in range(2):
        op("act", lambda e, a=a: e.activation(out=S_sb[:, a, :], in_=ps_S[:, 64 * a:64 * a + 32].rearrange("p (g t) -> p g t", t=2)[:, :, 0], func=AF.Copy),
           reads=[b_S, B_fin], writes=[B_fin])
    c1, s1 = ctab[:, :, 1], stab[:, :, 1]
    op("dve", lambda e: e.tensor_tensor(out=tq[:, 0, :], in0=c1, in1=S_sb[:, 0, :], op=ALU.mult), reads=[B_s, B_fin], writes=[B_fin])
    op("dve", lambda e: e.tensor_tensor(out=tq[:, 1, :], in0=s1, in1=S_sb[:, 1, :], op=ALU.mult), reads=[B_s, B_fin], writes=[B_fin])
    op("dve", lambda e: e.tensor_tensor(out=init[:, :, 0], in0=tq[:, 0, :], in1=tq[:, 1, :], op=ALU.subtract), reads=[B_fin] + B_init, writes=B_init)
    op("dve", lambda e: e.tensor_tensor(out=tq[:, 0, :], in0=c1, in1=S_sb[:, 1, :], op=ALU.mult), reads=[B_s, B_fin] + B_init, writes=[B_fin])
    op("dve", lambda e: e.tensor_tensor(out=tq[:, 1, :], in0=s1, in1=S_sb[:, 0, :], op=ALU.mult), reads=[B_s, B_fin], writes=[B_fin])
    op("dve", lambda e: e.tensor_tensor(out=init[:, :, 1], in0=tq[:, 0, :], in1=tq[:, 1, :], op=ALU.add), reads=[B_fin] + B_init, writes=B_init)
    S.barrier()
    Apre.close()
    AXr.close()
    ps_pool[:] = [0, 1, 2]

    MA.update(alloc_main())
    ct1, ct2, outA2, uT2, u_f2, gates2, st, s_bf, itmp, ctmp = [MA[k] for k in ("ct1", "ct2", "outA2", "uT2", "u_f2", "gates2", "st", "s_bf", "itmp", "ctmp")]
    y_sb, z_f, z_b, sg, outB, m1, m2, merged = [MA[k] for k in ("y_sb", "z_f", "z_b", "sg", "outB", "m1", "m2", "merged")]
    nblk = len(blocks)
    for _ in front(NB):
        pass
    for bi in range(NB, nblk + 1):
        gl = []
        if bi < nblk:
            gl.append(ssm(bi))
            gl.append(convgate(bi))
        if bi - 1 >= NB:
            gl.append(tail(bi - 1))
        if bi + 1 < nblk:
            gl.append(delayed(front(bi + 1), 12))
        if bi < nblk:
            gl.append(precast_gen(n_pre_per_blk))
        interleave(gl)

    S.barrier()
    A.close()
    ps_pool[:] = list(range(NPS))
    if do_b:
        Bq = Alloc(nc)
        sb = Bq.sb
        NT = S_half // 128
        w_q_sb = sb("w_q_sb", [128, 8, NQ], BF16)
        keys_sb = sb("keys_sb", [128, 2, 128], BF16)
        g2_sb = sb("g2_sb", [128, 2, D])
        B_wq = Buf("wq")
        for kc in range(8):
            dma("pool", lambda e, kc=kc: e.dma_start(out=w_q_sb[:, kc, :], in_=w_q[kc * 128:(kc + 1) * 128, :]), writes=[B_wq])
        for c in range(2):
            dma("pool", lambda e, c=c: e.dma_start(out=keys_sb[:, c, :], in_=keysT[c]), writes=[B_wq])
            dma("sp", lambda e, c=c: e.dma_start(out=g2_sb[:, c, :], in_=g_rep[1 + c]), writes=[B_wq])
        iota4 = sb("iota4", [128, 8, 16, 16])
        op("pool", lambda e: e.iota(iota4[:].rearrange("p h k a -> p (h k) a"), [[0, 128], [1, 16]], base=0, channel_multiplier=0,
                                    allow_small_or_imprecise_dtypes=True), writes=[B_wq])
        x1t = [sb("x1t%d" % i, [128, D]) for i in range(3)]
        B_x1 = [Buf("x1t0"), Buf("x1t1"), Buf("x1t2")]
        xn_f2 = [sb("xn_f%d" % i, [128, D]) for i in range(2)]
        B_xn2 = [Buf("xn0"), Buf("xn1")]
        xn_b = sb("xn_b", [128, D], BF16)
        B_xnb = Buf("xn_b")
        xnT = sb("xnT", [128, 8, 128], BF16)
        B_xnT = Buf("xnT")
        qT = sb("qT", [128, 16, 128], BF16)
        B_qT = Buf("qT")
        s_sb = sb("s_sb", [128, 16, 128])
        B_ssb = Buf("s_sb")
        srep = sb("srep", [128, 128])
        v12 = sb("v12", [128, 8, 2, 16])
        i12u = sb("i12u", [128, 8, 2, 16], U32)
        i12f = sb("i12f", [128, 8, 2, 16])
        cand = sb("cand", [128, 8, 16, 16])
        crep = sb("crep", [128, 256])
        best = sb("best", [128, 8, 16])
        posu = sb("posu", [128, 8, 16], U32)
        pau = sb("pau", [128, 8, 16], U32)
        pbu = sb("pbu", [128, 8, 16], U32)
        paf = sb("paf", [128, 8, 16])
        pbf = sb("pbf", [128, 8, 16])
        eq = sb("eq", [128, 8, 16, 16])
        If = sb("If", [128, 8, 16])
        Jf = sb("Jf", [128, 8, 16])
        eg = sb("eg", [128, 8, 16])
        zs = sb("zs", [128, 8])
        eidx2 = [sb("eidx%d" % i, [128, 128], I32) for i in range(2)]
        gate2 = [sb("gate%d" % i, [128, 128]) for i in range(2)]
        B_eg2 = [Buf("eg0"), Buf("eg1")]
        hpre = sb("hpre", [128, 128])
        B_tk = Buf("topk")
        junk_b = sb("junk_b", [128, D], BF16)
        B_jf = Buf("junk_b")
        NGB = 20
        gbuf = [sb("gbuf%d" % i, [128, 2 * D], BF16) for i in range(NGB)]
        B_gb = [Buf("gb%d" % i) for i in range(NGB)]
        gb_rr = [0]
        NHC = 4
        B_hc = [Buf("hc%d" % i) for i in range(NHC)]
        wcol = sb("wcol", [128, 2, 128])
        NDG = 6
        diag = [sb("diag%d" % i, [128, 128], BF16) for i in range(NDG)]
        B_dg = [Buf("dg%d" % i) for i in range(NDG)]
        o_t = sb("o_t", [128, D])
        B_ot = Buf("o_t")

        def prep_gen(t):
            par = t % 2
            xt, b_xt = x1t[t % 3], B_x1[t % 3]
            xn_f, B_xn = xn_f2[par], B_xn2[par]
            eidx, gate, B_eg = eidx2[par], gate2[par], B_eg2[par]
            dma("sp", lambda e: e.dma_start(out=xt[:], in_=x1_d[t * 128:(t + 1) * 128, :]), reads=[B_x1d], writes=[b_xt])
            norm_tile(xt[:], b_xt, g2_sb[:, 0, :], B_wq, xn_f[:], B_xn, 0)
            op("act", lambda e: e.activation(out=xn_b[:], in_=xn_f[:], func=AF.Copy), reads=[B_xn], writes=[B_xnb])
            yield
            transposes(xn_b[:], B_xnb, xnT, B_xnT)
            yield
            pend = None
            for qt in range(16):
                ps_q, b_q = next_ps()
                mm_group(ps_q[:, 0:128], b_q, [(w_q_sb[:, k, qt * 128:(qt + 1) * 128], xnT[:, k, :]) for k in range(8)], [B_wq, B_xnT])
                if pend is not None:
                    pend()
                pend = (lambda qt=qt, ps_q=ps_q, b_q=b_q: op("act", lambda e: e.activation(out=qT[:, qt, :], in_=ps_q[:, 0:128], func=AF.Copy), reads=[b_q], writes=[B_qT]))
                yield
            pend()
            yield
            pend = None
            for bnk in range(4):
                ps_s, b_s = next_ps()
                for j in range(4):
                    idx = bnk * 4 + j
                    mm_group(ps_s[:, j * 128:(j + 1) * 128], b_s, [(qT[:, idx, :], keys_sb[:, idx % 2, :])], [B_qT, B_wq])
                if pend is not None:
                    pend()
                pend = (lambda bnk=bnk, ps_s=ps_s, b_s=b_s: op("act", lambda e: e.activation(out=s_sb[:, bnk * 4:(bnk + 1) * 4, :].rearrange("p a n -> p (a n)"), in_=ps_s[:, :], func=AF.Copy),
                                                                 reads=[b_s], writes=[B_ssb]))
                yield
            pend()
            yield
            for h in range(8):
                for c in range(2):
                    sv = s_sb[:, 2 * h + c, :]
                    op("dve", lambda e, h=h, c=c, sv=sv: e.max(out=v12[:, h, c, 0:8], in_=sv), reads=[B_ssb], writes=[B_tk])
                    op("dve", lambda e, h=h, c=c, sv=sv: e.match_replace(out=srep[:], in_to_replace=v12[:, h, c, 0:8], in_values=sv, imm_value=-1e30), reads=[B_ssb, B_tk], writes=[B_tk])
                    yield
                    op("dve", lambda e, h=h, c=c: e.max(out=v12[:, h, c, 8:16], in_=srep[:]), reads=[B_tk], writes=[B_tk])
                    op("dve", lambda e, h=h, c=c, sv=sv: e.max_index(out=i12u[:, h, c, 0:8], in_max=v12[:, h, c, 0:8], in_values=sv), reads=[B_ssb, B_tk], writes=[B_tk])
                    yield
                    op("dve", lambda e, h=h, c=c: e.max_index(out=i12u[:, h, c, 8:16], in_max=v12[:, h, c, 8:16], in_values=srep[:]), reads=[B_tk], writes=[B_tk])
                    yield
            op("dve", lambda e: e.tensor_copy(out=i12f[:], in_=i12u[:]), reads=[B_tk], writes=[B_tk])
            v1b = v12[:, :, 0, :].rearrange("p h (a o) -> p h a o", o=1).to_broadcast([128, 8, 16, 16])
            v2b = v12[:, :, 1, :].rearrange("p h (o b) -> p h o b", o=1).to_broadcast([128, 8, 16, 16])
            op("dve", lambda e: e.tensor_tensor(out=cand[:], in0=v1b, in1=v2b, op=ALU.add), reads=[B_tk], writes=[B_tk])
            yield
            for h in range(8):
                ch = cand[:, h, :, :].rearrange("p a b -> p (a b)")
                op("dve", lambda e, h=h, ch=ch: e.max(out=best[:, h, 0:8], in_=ch), reads=[B_tk], writes=[B_tk])
                op("dve", lambda e, h=h, ch=ch: e.match_replace(out=crep[:], in_to_replace=best[:, h, 0:8], in_values=ch, imm_value=-1e30), reads=[B_tk], writes=[B_tk])
                yield
                op("dve", lambda e, h=h: e.max(out=best[:, h, 8:16], in_=crep[:]), reads=[B_tk], writes=[B_tk])
                op("dve", lambda e, h=h, ch=ch: e.max_index(out=posu[:, h, 0:8], in_max=best[:, h, 0:8], in_values=ch), reads=[B_tk], writes=[B_tk])
                yield
                op("dve", lambda e, h=h: e.max_index(out=posu[:, h, 8:16], in_max=best[:, h, 8:16], in_values=crep[:]), reads=[B_tk], writes=[B_tk])
                yield
            op("dve", lambda e: e.tensor_single_scalar(out=pau[:], in_=posu[:], scalar=4, op=ALU.logical_shift_right), reads=[B_tk], writes=[B_tk])
            op("dve", lambda e: e.tensor_single_scalar(out=pbu[:], in_=posu[:], scalar=15, op=ALU.bitwise_and), reads=[B_tk], writes=[B_tk])
            yield
            op("dve", lambda e: e.tensor_copy(out=paf[:], in_=pau[:]), reads=[B_tk], writes=[B_tk])
            op("dve", lambda e: e.tensor_copy(out=pbf[:], in_=pbu[:]), reads=[B_tk], writes=[B_tk])
            yield
            for (pf, c, dst) in ((paf, 0, If), (pbf, 1, Jf)):
                pfb = pf[:].rearrange("p h (k o) -> p h k o", o=1).to_broadcast([128, 8, 16, 16])
                ifb = i12f[:, :, c, :].rearrange("p h (o a) -> p h o a", o=1).to_broadcast([128, 8, 16, 16])
                op("dve", lambda e, pfb=pfb: e.tensor_tensor(out=eq[:], in0=pfb, in1=iota4[:], op=ALU.is_equal), reads=[B_tk, B_wq], writes=[B_tk])
                yield
                op("dve", lambda e, ifb=ifb: e.tensor_tensor(out=eq[:], in0=eq[:], in1=ifb, op=ALU.mult), reads=[B_tk], writes=[B_tk])
                yield
                op("dve", lambda e, dst=dst: e.tensor_reduce(out=dst[:].rearrange("p h k -> p (h k)"), in_=eq[:].rearrange("p h k a -> p (h k) a"), axis=AX.X, op=ALU.add),
                   reads=[B_tk], writes=[B_tk])
                yield
            op("dve", lambda e: e.scalar_tensor_tensor(out=paf[:], in0=If[:], scalar=128.0, in1=Jf[:], op0=ALU.mult, op1=ALU.add), reads=[B_tk], writes=[B_tk])
            op("dve", lambda e: e.tensor_copy(out=eidx[:], in_=paf[:].rearrange("p h k -> p (h k)")), reads=[B_tk, B_eg], writes=[B_eg])
            yield
            bmx = best[:, :, 0:1].to_broadcast([128, 8, 16])
            op("dve", lambda e: e.tensor_tensor(out=eg[:], in0=best[:], in1=bmx, op=ALU.subtract), reads=[B_tk], writes=[B_tk])
            op("act", lambda e: e.activation(out=eg[:], in_=eg[:], func=AF.Exp), reads=[B_tk], writes=[B_tk])
            op("dve", lambda e: e.tensor_reduce(out=zs[:], in_=eg[:], axis=AX.X, op=ALU.add), reads=[B_tk], writes=[B_tk])
            op("dve", lambda e: e.reciprocal(out=zs[:], in_=zs[:]), reads=[B_tk], writes=[B_tk])
            zb = zs[:].rearrange("p (h o) -> p h o", o=1).to_broadcast([128, 8, 16])
            op("dve", lambda e: e.tensor_tensor(out=gate[:].rearrange("p (h k) -> p h k", h=8), in0=eg[:], in1=zb, op=ALU.mult), reads=[B_tk, B_eg], writes=[B_eg])
            yield

        def run_gen(g, n=None):
            if g is None:
                return
            k = 0
            for _ in g:
                k += 1
                if n is not None and k >= n:
                    return

        gcur = prep_gen(0)
        run_gen(gcur)
        def finalize(t):
            xt, b_xt = x1t[t % 3], B_x1[t % 3]
            for hf in range(2):
                op("dve", lambda e, hf=hf, xt=xt: e.tensor_tensor(out=xt[:, hf * 512:(hf + 1) * 512], in0=xt[:, hf * 512:(hf + 1) * 512], in1=ps_acc[hf][:, :], op=ALU.add),
                   reads=[b_xt, B_pacc], writes=[b_xt])
            norm_tile(xt[:], b_xt, g2_sb[:, 1, :], B_wq, o_t[:], B_ot, 4)
            dma("sp", lambda e, t=t: e.dma_start(out=out_d[t * 128:(t + 1) * 128, :], in_=o_t[:]), reads=[B_ot])

        for t in range(NT):
            par = t % 2
            xt, b_xt = x1t[t % 3], B_x1[t % 3]
            xn_f, B_xn = xn_f2[par], B_xn2[par]
            eidx, gate, B_eg = eidx2[par], gate2[par], B_eg2[par]
            gnext = prep_gen(t + 1) if t + 1 < NT else None
            GS = 8
            slot_gb = {}
            for k in range(128 // GS + 1):
                if k == 1 and t >= 1:
                    finalize(t - 1)
                if k >= 1:
                    kk = k - 1
                    b_hc = B_hc[kk % NHC]
                    cs = slice(kk * GS, (kk + 1) * GS)
                    op("act", lambda e, cs=cs: e.activation(out=wcol[:, 0, cs], in_=hpre[:, cs], func=AF.Gelu), reads=[b_hc], writes=[b_hc])
                    for sl in range(kk * GS, (kk + 1) * GS):
                        gb, b_gb = slot_gb.pop(sl)
                        dg, b_dg = diag[sl % NDG], B_dg[sl % NDG]
                        op("act", lambda e, sl=sl, gate=gate: e.activation(out=wcol[:, 1, sl:sl + 1], in_=wcol[:, 0, sl:sl + 1], func=AF.Copy, scale=gate[:, sl:sl + 1]),
                           reads=[b_hc, B_eg], writes=[b_hc])
                        op("act", lambda e, sl=sl, dg=dg: e.activation(out=dg[:], in_=ident[:], func=AF.Copy, scale=wcol[:, 1, sl:sl + 1]),
                           reads=[b_hc, B_id], writes=[b_dg])
                        for hf in range(2):
                            op("pe", lambda e, sl=sl, dg=dg, gb=gb, hf=hf: e.matmul(ps_acc[hf][:, :], lhsT=dg[:], rhs=gb[:, D + hf * 512:D + (hf + 1) * 512],
                                                                                 start=(sl == 0), stop=(sl == 127)), reads=[b_dg, b_gb], writes=[B_pacc])
                if k < 128 // GS:
                    for sl in range(k * GS, (k + 1) * GS):
                        gi = gb_rr[0] % NGB
                        gb_rr[0] += 1
                        gb, b_gb = gbuf[gi], B_gb[gi]
                        slot_gb[sl] = (gb, b_gb)
                        b_hc = B_hc[k % NHC]
                        dma("pool", lambda e, gb=gb, sl=sl, eidx=eidx: e.indirect_dma_start(out=gb[:], out_offset=None, in_=uv_d[:, :],
                            in_offset=bass.IndirectOffsetOnAxis(ap=eidx[:, sl:sl + 1], axis=0)), reads=[B_eg, B_uv], writes=[b_gb])
                        op("dve", lambda e, gb=gb, sl=sl, xn_f=xn_f: e.scalar_tensor_tensor(out=junk_b[:], in0=gb[:, 0:D], scalar=1.0, in1=xn_f[:], op0=ALU.mult, op1=ALU.mult,
                                                                                            accum_out=hpre[:, sl:sl + 1]), reads=[b_gb, B_xn, b_hc], writes=[B_jf, b_hc])
                        run_gen(gnext, 1)
            run_gen(gnext)
        finalize(NT - 1)
        S.barrier()
        Bq.close()
    P0.close()
    S.final_wait("sp")
    S.emit()
    S.close()
    return nc


def prep_shared(inp):
    f = np.float32
    sh = {}
    sh["w_in"] = np.ascontiguousarray(inp["w_in"][0], f)
    sh["w_a"] = np.ascontiguousarray(inp["w_branch_a"][0], f)
    sh["w_b"] = np.ascontiguousarray(inp["w_branch_b"][0], f)
    sh["w_out"] = np.ascontiguousarray(inp["w_out"][0], f)
    sh["w_glu"] = np.ascontiguousarray(inp["w_glu"][0], f)
    sh["w_q"] = np.ascontiguousarray(inp["w_q"][0], f)
    sh["keysT"] = np.ascontiguousarray(np.stack([inp["sub_keys_1"][0].T, inp["sub_keys_2"][0].T]), f)
    sh["g_rep"] = np.ascontiguousarray(np.stack([np.broadcast_to(inp["g_mix"][0], (128, D)),
                                                 np.broadcast_to(inp["g_ffn"][0], (128, D)),
                                                 np.broadcast_to(inp["g_final"], (128, D))]), f)
    colp = np.zeros((128, 48), f)
    colp[:, 0:16] = inp["b_gate"][0].reshape(16, 128).T
    cw = inp["conv_w"][0].reshape(3, 6, 128)
    colp[:, 16:34] = cw.transpose(2, 1, 0).reshape(128, 18)
    colp[:, 34:40] = inp["conv_b"][0].reshape(6, 128).T
    colp[:, 40:42] = inp["b_glu"][0].reshape(2, 128).T
    colp[:, 42:44] = inp["ssm_d"][0].reshape(2, 128).T
    sh["colp"] = colp
    lr, li, ld = inp["lam_re"][0], inp["lam_im"][0], inp["log_dt"][0]
    pt = np.zeros((3, 128, G), f)
    pt[0] = np.concatenate([lr.T, lr.T], 0)
    pt[1] = np.concatenate([li.T, li.T], 0)
    pt[2] = np.broadcast_to(ld[None, :], (128, G))
    sh["ssm_pt"] = pt
    row = np.zeros((3, 128, G * P), f)
    row[0] = np.broadcast_to(lr.reshape(1, -1), (128, G * P))
    row[1] = np.broadcast_to(li.reshape(1, -1), (128, G * P))
    row[2] = np.broadcast_to(np.repeat(ld, P).reshape(1, -1), (128, G * P))
    sh["ssm_row"] = row
    btm = np.zeros((2, 128, G, P), f)
    ctm = np.zeros((128, G, 128), f)
    for g in range(G):
        r0 = 16 * (g % 8)
        btm[0, r0:r0 + 16, g, :] = inp["ssm_b_re"][0][g].T
        btm[1, r0:r0 + 16, g, :] = inp["ssm_b_im"][0][g].T
        ctm[0:64, g, r0:r0 + 16] = inp["ssm_c_re"][0][g].T
        ctm[64:128, g, r0:r0 + 16] = inp["ssm_c_im"][0][g].T
    sh["bt_in"] = btm.reshape(2, 128, G * P)
    sh["ct_in"] = ctm.reshape(128, G * 128)
    sh["exp_u"] = np.ascontiguousarray(inp["expert_u"][0], f)
    sh["exp_v"] = np.ascontiguousarray(inp["expert_v"][0], f)
    return sh


def make_in_maps(inp, S_half, n_cores):
    sh = prep_shared(inp)
    x = np.asarray(inp["x"], np.float32)
    maps = []
    for c in range(n_cores):
        b, hf = c // 2, c % 2
        m = dict(sh)
        m["x_main"] = np.ascontiguousarray(x[b, hf * S_half:(hf + 1) * S_half])
        m["x_prev"] = np.ascontiguousarray(x[b, 0:S_half]) if hf == 1 else np.zeros((S_half, D), np.float32)
        maps.append(m)
    return maps


def kernel(**inputs):
    x = np.asarray(inputs["x"])
    bsz, seq, _ = x.shape
    S_half = seq // 2
    n_cores = 2 * bsz
    nc = build_nc(S_half)
    maps = make_in_maps(inputs, S_half, n_cores)
    res = run_bass_kernel_spmd(nc, maps, core_ids=list(range(n_cores)))
    out = np.zeros((bsz, seq, D), np.float32)
    for c in range(n_cores):
        b, hf = c // 2, c % 2
        out[b, hf * S_half:(hf + 1) * S_half] = res.results[c]["out"]
    return out
```

```python
import math
import numpy as np
import concourse.bass as bass
import concourse.mybir as mybir
from concourse.bass_utils import run_bass_kernel_spmd

F32 = mybir.dt.float32
BF16 = mybir.dt.bfloat16
U32 = mybir.dt.uint32
I32 = mybir.dt.int32
ALU = mybir.AluOpType
AF = mybir.ActivationFunctionType
AX = mybir.AxisListType

D = 1024
CONV_CH = 768
SSM_CH = 256
G = 16
P = 64
IN_COLS = 4608
NQ = 2048
NE = 16384
TB = 128
EPS = 1e-6
TWO_PI = 2.0 * math.pi
SKIP_PRECAST = False


class Buf:
    __slots__ = ("name", "w", "r")

    def __init__(self, name=""):
        self.name = name
        self.w = None
        self.r = []


class Sched:
    ENG = ("pe", "dve", "act", "pool", "sp")

    def __init__(self, nc, n_dma_sems=20):
        self.nc = nc
        self.stack = []
        self.streams = {e: [] for e in self.ENG}
        self.cnt = {e: 0 for e in self.ENG}
        self.sem = {}
        for e in self.ENG:
            g = nc.semaphore("s_" + e)
            self.sem[e] = g.__enter__()
            self.stack.append(g)
        self.known = {e: {} for e in self.ENG}
        self.dma_sems = {}
        for q in ("sp", "pool", "act"):
            self.dma_sems[q] = []
            for i in range({"sp": n_dma_sems, "pool": 48, "act": 8}[q]):
                g = nc.semaphore("d%s_%d" % (q, i))
                self.sem["d%s%d" % (q, i)] = g.__enter__()
                self.stack.append(g)
                self.dma_sems[q].append("d%s%d" % (q, i))
        self.dma_cnt = {k: 0 for q in self.dma_sems for k in self.dma_sems[q]}
        self.dma_rr = {"sp": 0, "pool": 0, "act": 0}
        self.n_inst = 0

    def _need(self, eng, deps):
        out = []
        for key, val in deps:
            if val <= 0:
                continue
            if self.known[eng].get(key, 0) >= val:
                continue
            self.known[eng][key] = val
            out.append((key, val))
        return out

    def _deps(self, reads, writes):
        deps = {}
        for b in reads:
            if b.w is not None:
                k, v = b.w
                deps[k] = max(deps.get(k, 0), v)
        for b in writes:
            if b.w is not None:
                k, v = b.w
                deps[k] = max(deps.get(k, 0), v)
            for k, v in b.r:
                deps[k] = max(deps.get(k, 0), v)
        return list(deps.items())

    def op(self, eng, fn, reads=(), writes=()):
        deps = self._deps(reads, writes)
        if eng == "pe":
            deps = [(k, v) for k, v in deps if k != "pe"]
        waits = self._need(eng, deps)
        self.cnt[eng] += 1
        n = self.cnt[eng]
        self.streams[eng].append((waits, fn, (eng, 1)))
        for b in reads:
            b.r.append((eng, n))
            if len(b.r) > 64:
                b.r = b.r[-64:] if False else self._compact(b.r)
        for b in writes:
            b.w = (eng, n)
            b.r = []
        self.n_inst += 1

    @staticmethod
    def _compact(r):
        m = {}
        for k, v in r:
            m[k] = max(m.get(k, 0), v)
        return list(m.items())

    def dma(self, queue, fn, reads=(), writes=()):
        deps = self._deps(reads, writes)
        pool_ = self.dma_sems[queue]
        key = pool_[self.dma_rr[queue] % len(pool_)]
        self.dma_rr[queue] += 1
        prev = self.dma_cnt[key]
        if prev > 0:
            deps.append((key, prev))
        waits = self._need(queue, deps)
        val = prev + 16
        self.dma_cnt[key] = val
        self.streams[queue].append((waits, fn, (key, 16)))
        for b in reads:
            b.r.append((key, val))
            if len(b.r) > 64:
                b.r = self._compact(b.r)
        for b in writes:
            b.w = (key, val)
            b.r = []
        self.n_inst += 1

    def barrier(self, dma=True):
        deps = [(e, self.cnt[e]) for e in self.ENG] + (list(self.dma_cnt.items()) if dma else [])
        for e in self.ENG:
            waits = self._need(e, [d for d in deps if d[0] != e])
            if waits:
                self.streams[e].append((waits, None, None))

    def final_wait(self, eng="sp"):
        deps = [(e, self.cnt[e]) for e in self.ENG if e != eng] + list(self.dma_cnt.items())
        waits = self._need(eng, deps)
        self.streams[eng].append((waits, None, None))

    def emit(self):
        nc = self.nc
        sems = self.sem
        streams = self.streams

        def run(engine, items):
            for waits, fn, inc in items:
                for key, val in waits:
                    engine.wait_ge(sems[key], val)
                if fn is not None:
                    fn(engine).then_inc(sems[inc[0]], inc[1])

        with nc.Block() as block:
            @block.tensor
            def _(e):
                run(e, streams["pe"])

            @block.vector
            def _(e):
                run(e, streams["dve"])

            @block.scalar
            def _(e):
                run(e, streams["act"])

            @block.gpsimd
            def _(e):
                run(e, streams["pool"])

            @block.sync
            def _(e):
                run(e, streams["sp"])

    def close(self):
        for g in reversed(self.stack):
            g.__exit__(None, None, None)


class Alloc:
    def __init__(self, nc):
        self.nc = nc
        self.stack = []

    def sb(self, name, shape, dt=F32, side=None):
        g = self.nc.sbuf_tensor(name, list(shape), dt, side=side) if side else self.nc.sbuf_tensor(name, list(shape), dt)
        t = g.__enter__()
        self.stack.append(g)
        return t

    def ps(self, name, shape, dt=F32):
        g = self.nc.psum_tensor(name, list(shape), dt)
        t = g.__enter__()
        self.stack.append(g)
        return t

    def close(self):
        for g in reversed(self.stack):
            g.__exit__(None, None, None)
        self.stack = []


def build_nc(S_half, debug=False, do_b=True):
    NB = S_half // TB
    nc = bass.Bass("TRN2", target_bir_lowering=False)

    def din(name, shape, dt=F32):
        return nc.dram_tensor(name, list(shape), dt, kind="ExternalInput").ap()

    x_main = din("x_main", [S_half, D])
    x_prev = din("x_prev", [S_half, D])
    w_in = din("w_in", [D, IN_COLS])
    w_a = din("w_a", [CONV_CH, D])
    w_b = din("w_b", [SSM_CH, D])
    w_out = din("w_out", [D, D])
    w_glu = din("w_glu", [SSM_CH, SSM_CH])
    w_q = din("w_q", [D, NQ])
    keysT = din("keysT", [2, 128, 128])
    g_rep = din("g_rep", [3, 128, D])
    colp = din("colp", [128, 48])
    ssm_pt = din("ssm_pt", [3, 128, G])
    ssm_row = din("ssm_row", [3, 128, G * P])
    bt_in = din("bt_in", [2, 128, G * P])
    ct_in = din("ct_in", [128, G * 128])
    exp_u = din("exp_u", [NE, D])
    exp_v = din("exp_v", [NE, D])
    out_d = nc.dram_tensor("out", [S_half, D], F32, kind="ExternalOutput").ap()
    if debug:
        x1_d = nc.dram_tensor("x1_d", [S_half, D], F32, kind="ExternalOutput").ap()
        dbg_d = nc.dram_tensor("dbg", [128, 2 * S_half], F32, kind="ExternalOutput").ap()
    else:
        x1_d = nc.dram_tensor("x1_d", [S_half, D], F32, kind="Internal").ap()
        dbg_d = None

    uv_d = nc.dram_tensor("uv_d", [NE, 2 * D], BF16, kind="Internal").ap()
    B_uv = Buf("uv_d")
    B_x1d = Buf("x1_d")

    S = Sched(nc)
    op, dma = S.op, S.dma

    P0 = Alloc(nc)
    identf = P0.sb("identf", [128, 128])
    ident = P0.sb("ident", [128, 128], BF16)
    junk = P0.sb("junk", [128, D], BF16)
    stat = P0.sb("stat", [128, 8])
    B_junk = Buf("junk")
    B_stat = Buf("stat")
    ps_tr = P0.ps("ps_tr", [128, 8, 128], BF16)
    B_ptr = Buf("ps_tr")
    NPS = 5
    ps_acc = [P0.ps("ps_acc%d" % i, [128, 512]) for i in range(2)]
    B_pacc = Buf("ps_acc")
    ps_mm = [P0.ps("ps_mm%d" % i, [128, 512]) for i in range(NPS)]
    B_ps = [Buf("ps%d" % i) for i in range(NPS)]
    ps_rr = [0]
    ps_pool = list(range(NPS))

    def next_ps():
        i = ps_pool[ps_rr[0] % len(ps_pool)]
        ps_rr[0] += 1
        return ps_mm[i], B_ps[i]

    def mm_group(out_ap, b_out, pairs, reads):
        n = len(pairs)
        for i, (l, r) in enumerate(pairs):
            op("pe", lambda e, l=l, r=r, i=i: e.matmul(out_ap, lhsT=l, rhs=r, start=(i == 0), stop=(i == n - 1)),
               reads=reads, writes=[b_out])

    def rstd_of(src, b_src, col):
        op("act", lambda e: e.activation(out=junk[:], in_=src, func=AF.Square, accum_out=stat[:, col:col + 1]),
           reads=[b_src, B_stat], writes=[B_junk, B_stat])
        op("dve", lambda e: e.tensor_scalar(out=stat[:, col + 1:col + 2], in0=stat[:, col:col + 1], scalar1=1.0 / D, scalar2=EPS,
                                            op0=ALU.mult, op1=ALU.add), reads=[B_stat], writes=[B_stat])
        op("act", lambda e: e.activation(out=stat[:, col + 2:col + 3], in_=stat[:, col + 1:col + 2], func=AF.Sqrt),
           reads=[B_stat], writes=[B_stat])
        op("dve", lambda e: e.reciprocal(out=stat[:, col + 3:col + 4], in_=stat[:, col + 2:col + 3]), reads=[B_stat], writes=[B_stat])

    def norm_tile(src, b_src, g_ap, b_gconst, dst, b_dst, col):
        rstd_of(src, b_src, col)
        op("dve", lambda e: e.scalar_tensor_tensor(out=dst, in0=src, scalar=stat[:, col + 3:col + 4], in1=g_ap,
                                                   op0=ALU.mult, op1=ALU.mult),
           reads=[b_src, B_stat, b_gconst], writes=[b_dst])

    def transposes(src_bf, b_src, dstT, b_dstT):
        for k in range(8):
            op("pe", lambda e, k=k: e.transpose(out=ps_tr[:, k, :], in_=src_bf[:, k * 128:(k + 1) * 128], identity=ident[:]),
               reads=[b_src, B_id], writes=[B_ptr])
        op("act", lambda e: e.activation(out=dstT[:, :, :], in_=ps_tr[:], func=AF.Copy), reads=[B_ptr], writes=[b_dstT])

    A = Alloc(nc)
    sb = A.sb
    w_in_sb = sb("w_in_sb", [128, 8, IN_COLS], BF16)
    w_a_sb = sb("w_a_sb", [128, 6, D], BF16)
    w_b_sb = sb("w_b_sb", [128, 2, D], BF16)
    w_out_sb = sb("w_out_sb", [128, 8, D], BF16)
    w_glu_sb = sb("w_glu_sb", [128, 2, SSM_CH], BF16)
    B_w = Buf("weights")

    def load_weights():
        for kc in range(8):
            for c in range(3):
                dma("pool", lambda e, kc=kc, c=c: e.dma_start(
                    out=w_in_sb[:, kc, c * 1536:(c + 1) * 1536],
                    in_=w_in[kc * 128:(kc + 1) * 128, c * 1536:(c + 1) * 1536]), writes=[B_w])
        for kc in range(6):
            dma("pool", lambda e, kc=kc: e.dma_start(out=w_a_sb[:, kc, :], in_=w_a[kc * 128:(kc + 1) * 128, :]), writes=[B_w])
        for kc in range(2):
            dma("pool", lambda e, kc=kc: e.dma_start(out=w_b_sb[:, kc, :], in_=w_b[kc * 128:(kc + 1) * 128, :]), writes=[B_w])
            dma("pool", lambda e, kc=kc: e.dma_start(out=w_glu_sb[:, kc, :], in_=w_glu[kc * 128:(kc + 1) * 128, :]), writes=[B_w])
        for kc in range(8):
            dma("pool", lambda e, kc=kc: e.dma_start(out=w_out_sb[:, kc, :], in_=w_out[kc * 128:(kc + 1) * 128, :]), writes=[B_w])


    g_sb = sb("g_sb", [128, D])
    colp_sb = sb("colp_sb", [128, 48])
    B_c = Buf("consts")
    dma("sp", lambda e: e.dma_start(out=g_sb[:], in_=g_rep[0]), writes=[B_c])
    dma("sp", lambda e: e.dma_start(out=colp_sb[:], in_=colp[:, :]), writes=[B_c])
    CW = lambda c, k: colp_sb[:, 16 + c * 3 + k:17 + c * 3 + k]
    CBIAS = lambda c: colp_sb[:, 34 + c:35 + c]

    B_id = Buf("ident")
    op("pool", lambda e: e.iota(identf[:], [[1, 128]], base=0, channel_multiplier=-1,
                                allow_small_or_imprecise_dtypes=True), writes=[B_id])
    op("dve", lambda e: e.tensor_single_scalar(out=ident[:], in_=identf[:], scalar=0.0, op=ALU.is_equal),
       reads=[B_id], writes=[B_id])

    r_p = sb("r_p", [128, G])
    ctab = sb("ctab", [128, G, TB + 1])
    stab = sb("stab", [128, G, TB + 1])
    nsT = sb("nsT", [128, G])
    bpq = sb("bpq", [128, G, 2, 128], BF16)
    ctb = sb("ctb", [128, G, 128], BF16)
    B_s = Buf("ssmc")
    LP = 2 if NB % 2 == 0 else 1
    AXr = Alloc(nc)
    PT = AXr.sb("PT", [128, LP, G, 128], BF16, side="right")
    CRI = AXr.sb("CRI", [128, 2, G * P], F32, side="right")
    Mst = AXr.sb("Mst", [128, G, 128], F32, side="right")
    A00 = Alloc(nc)
    ctf = A00.sb("ctf", [128, G * 128])
    dma("sp", lambda e: e.dma_start(out=ctf[:], in_=ct_in[:, :]), writes=[B_s])
    op("dve", lambda e: e.tensor_scalar(out=ctf[64:128, :], in0=ctf[64:128, :], scalar1=-1.0, scalar2=None, op0=ALU.mult), reads=[B_s], writes=[B_s])
    op("dve", lambda e: e.tensor_copy(out=ctb[:].rearrange("p g m -> p (g m)"), in_=ctf[:]), reads=[B_s], writes=[B_s])
    S.barrier(dma=False)
    A00.close()
    A0 = Alloc(nc)
    sb0 = A0.sb
    spt = sb0("spt", [128, 3, G])
    HW = 8 * P
    srow = sb0("srow", [128, 3, HW])
    bt = sb0("bt", [128, 2, HW])
    ecol = sb0("ecol", [128, 4])
    dma("sp", lambda e: e.dma_start(out=spt[:], in_=ssm_pt.rearrange("a p g -> p a g")), writes=[B_s])
    for l in range(LP):
        op("pool", lambda e, l=l: e.iota(ecol[:, l:l + 1], [[0, 1]], base=128 * l + 127, channel_multiplier=-1,
                                         allow_small_or_imprecise_dtypes=True), writes=[B_s])

    thp_p = sb0("thp_p", [128, G])
    tmp_p = sb0("tmp_p", [128, 4, G])
    iota_t = sb0("iota_t", [128, TB + 1])
    ytmp = sb0("ytmp", [128, 3, TB + 1])
    yti = sb0("yti", [128, TB + 1], I32)
    rti = sb0("rti", [128, HW], I32)
    rt2 = sb0("rt2", [128, HW])
    R = [sb0("rw%d" % i, [128, HW]) for i in range(8)]

    def expm1_poly(eng, out, z, t):
        op(eng, lambda e: e.tensor_scalar(out=out, in0=z, scalar1=1.0 / 720, scalar2=1.0 / 120, op0=ALU.mult, op1=ALU.add), reads=[B_s], writes=[B_s])
        for c in (1.0 / 24, 1.0 / 6, 0.5, 1.0):
            op(eng, lambda e: e.tensor_tensor(out=t, in0=out, in1=z, op=ALU.mult), reads=[B_s], writes=[B_s])
            op(eng, lambda e, c=c: e.tensor_scalar(out=out, in0=t, scalar1=c, scalar2=None, op0=ALU.add), reads=[B_s], writes=[B_s])
        op(eng, lambda e: e.tensor_tensor(out=t, in0=out, in1=z, op=ALU.mult), reads=[B_s], writes=[B_s])
        op(eng, lambda e: e.tensor_copy(out=out, in_=t), reads=[B_s], writes=[B_s])

    def sin_turns(out, y, off, t, ti, t2):
        op("dve", lambda e: e.tensor_scalar(out=t, in0=y, scalar1=8.0 + off, scalar2=None, op0=ALU.add), reads=[B_s], writes=[B_s])
        op("dve", lambda e: e.tensor_copy(out=ti, in_=t), reads=[B_s], writes=[B_s])
        op("dve", lambda e: e.tensor_copy(out=t2, in_=ti), reads=[B_s], writes=[B_s])
        op("dve", lambda e: e.tensor_tensor(out=t, in0=t, in1=t2, op=ALU.subtract), reads=[B_s], writes=[B_s])
        op("dve", lambda e: e.tensor_single_scalar(out=t2, in_=t, scalar=0.5, op=ALU.is_gt), reads=[B_s], writes=[B_s])
        op("dve", lambda e: e.tensor_tensor(out=t, in0=t, in1=t2, op=ALU.subtract), reads=[B_s], writes=[B_s])
        op("act", lambda e: e.activation(out=out, in_=t, func=AF.Sin, scale=TWO_PI), reads=[B_s], writes=[B_s])

    dt_p = tmp_p[:, 0, :]
    z_p = tmp_p[:, 1, :]
    e_p = tmp_p[:, 2, :]
    t_p = tmp_p[:, 3, :]
    op("act", lambda e: e.activation(out=dt_p, in_=spt[:, 2, :], func=AF.Exp), reads=[B_s], writes=[B_s])
    op("dve", lambda e: e.tensor_tensor(out=z_p, in0=spt[:, 0, :], in1=dt_p, op=ALU.mult), reads=[B_s], writes=[B_s])
    expm1_poly("dve", e_p, z_p, t_p)
    op("dve", lambda e: e.tensor_scalar(out=r_p[:], in0=e_p, scalar1=1.0, scalar2=None, op0=ALU.add), reads=[B_s], writes=[B_s])
    op("dve", lambda e: e.tensor_tensor(out=t_p, in0=spt[:, 1, :], in1=dt_p, op=ALU.mult), reads=[B_s], writes=[B_s])
    op("dve", lambda e: e.tensor_scalar(out=thp_p[:], in0=t_p, scalar1=1.0 / TWO_PI, scalar2=None, op0=ALU.mult), reads=[B_s], writes=[B_s])
    op("pool", lambda e: e.iota(iota_t[:], [[1, TB + 1]], base=0, channel_multiplier=0,
                                allow_small_or_imprecise_dtypes=True), writes=[B_s])
    load_weights()
    for g in range(G):
        op("dve", lambda e, g=g: e.tensor_scalar(out=ytmp[:, 0, :], in0=iota_t[:], scalar1=thp_p[:, g:g + 1], scalar2=None, op0=ALU.mult), reads=[B_s], writes=[B_s])
        sin_turns(stab[:, g, :], ytmp[:, 0, :], 0.0, ytmp[:, 1, :], yti[:], ytmp[:, 2, :])
        sin_turns(ctab[:, g, :], ytmp[:, 0, :], 0.25, ytmp[:, 1, :], yti[:], ytmp[:, 2, :])
    op("dve", lambda e: e.tensor_scalar(out=nsT[:], in0=stab[:, :, TB], scalar1=-1.0, scalar2=None, op0=ALU.mult), reads=[B_s], writes=[B_s])

    for hh in range(2):
        gs = slice(8 * hh, 8 * hh + 8)
        for a in range(3):
            dma("sp", lambda e, a=a, hh=hh: e.dma_start(out=srow[:, a, :], in_=ssm_row[a][:, hh * HW:(hh + 1) * HW]), reads=[B_s], writes=[B_s])
        for a in range(2):
            dma("sp", lambda e, a=a, hh=hh: e.dma_start(out=bt[:, a, :], in_=bt_in[a][:, hh * HW:(hh + 1) * HW]), reads=[B_s], writes=[B_s])
        lr_r, li_r = srow[:, 0, :], srow[:, 1, :]
        dt_r, z_r, em1_r, t_r, thp_r, sn_r, cs_r, sh_r = [R[i][:] for i in range(8)]
        op("act", lambda e: e.activation(out=dt_r, in_=srow[:, 2, :], func=AF.Exp), reads=[B_s], writes=[B_s])
        op("dve", lambda e: e.tensor_tensor(out=z_r, in0=lr_r, in1=dt_r, op=ALU.mult), reads=[B_s], writes=[B_s])
        expm1_poly("dve", em1_r, z_r, t_r)
        op("dve", lambda e: e.tensor_tensor(out=t_r, in0=li_r, in1=dt_r, op=ALU.mult), reads=[B_s], writes=[B_s])
        op("dve", lambda e: e.tensor_scalar(out=thp_r, in0=t_r, scalar1=1.0 / TWO_PI, scalar2=None, op0=ALU.mult), reads=[B_s], writes=[B_s])
        y_r, mg_r = R[5][:], R[0][:]
        for l in range(LP):
            op("dve", lambda e, l=l: e.tensor_scalar(out=y_r, in0=thp_r, scalar1=ecol[:, l:l + 1], scalar2=None, op0=ALU.mult), reads=[B_s], writes=[B_s])
            sin_turns(R[7][:], y_r, 0.0, t_r, rti[:], rt2[:])
            sin_turns(R[6][:], y_r, 0.25, t_r, rti[:], rt2[:])
            op("act", lambda e, l=l: e.activation(out=mg_r, in_=z_r, func=AF.Exp, scale=ecol[:, l:l + 1]), reads=[B_s], writes=[B_s])
            op("dve", lambda e, l=l, gs=gs: e.tensor_tensor(out=PT[:, l, gs, 0:64], in0=mg_r.rearrange("p (g s) -> p g s", g=8), in1=R[6][:].rearrange("p (g s) -> p g s", g=8), op=ALU.mult), reads=[B_s], writes=[B_s])
            op("dve", lambda e, l=l, gs=gs: e.tensor_tensor(out=PT[:, l, gs, 64:128], in0=mg_r.rearrange("p (g s) -> p g s", g=8), in1=R[7][:].rearrange("p (g s) -> p g s", g=8), op=ALU.mult), reads=[B_s], writes=[B_s])
        op("dve", lambda e: e.tensor_scalar(out=y_r, in0=thp_r, scalar1=float(128 * LP), scalar2=None, op0=ALU.mult), reads=[B_s], writes=[B_s])
        sin_turns(R[7][:], y_r, 0.0, t_r, rti[:], rt2[:])
        sin_turns(R[6][:], y_r, 0.25, t_r, rti[:], rt2[:])
        op("act", lambda e: e.activation(out=mg_r, in_=z_r, func=AF.Exp, scale=float(128 * LP)), reads=[B_s], writes=[B_s])
        op("dve", lambda e, hh=hh: e.tensor_tensor(out=CRI[:, 0, hh * HW:(hh + 1) * HW], in0=mg_r, in1=R[6][:], op=ALU.mult), reads=[B_s], writes=[B_s])
        op("dve", lambda e, hh=hh: e.tensor_tensor(out=CRI[:, 1, hh * HW:(hh + 1) * HW], in0=mg_r, in1=R[7][:], op=ALU.mult), reads=[B_s], writes=[B_s])
        op("act", lambda e: e.activation(out=dt_r, in_=srow[:, 2, :], func=AF.Exp), reads=[B_s], writes=[B_s])
        sin_turns(sn_r, thp_r, 0.0, t_r, rti[:], rt2[:])
        sin_turns(cs_r, thp_r, 0.25, t_r, rti[:], rt2[:])
        op("dve", lambda e: e.tensor_scalar(out=dt_r, in0=thp_r, scalar1=0.5, scalar2=None, op0=ALU.mult), reads=[B_s], writes=[B_s])
        sin_turns(sh_r, dt_r, 0.0, t_r, rti[:], rt2[:])
        op("dve", lambda e: e.tensor_tensor(out=z_r, in0=em1_r, in1=cs_r, op=ALU.mult), reads=[B_s], writes=[B_s])
        op("dve", lambda e: e.tensor_tensor(out=t_r, in0=sh_r, in1=sh_r, op=ALU.mult), reads=[B_s], writes=[B_s])
        op("dve", lambda e: e.scalar_tensor_tensor(out=z_r, in0=t_r, scalar=-2.0, in1=z_r, op0=ALU.mult, op1=ALU.add), reads=[B_s], writes=[B_s])
        op("dve", lambda e: e.scalar_tensor_tensor(out=sn_r, in0=em1_r, scalar=1.0, in1=sn_r, op0=ALU.add, op1=ALU.mult), reads=[B_s], writes=[B_s])
        nr_r, ni_r = z_r, sn_r
        op("dve", lambda e: e.tensor_tensor(out=cs_r, in0=lr_r, in1=lr_r, op=ALU.mult), reads=[B_s], writes=[B_s])
        op("dve", lambda e: e.tensor_tensor(out=t_r, in0=li_r, in1=li_r, op=ALU.mult), reads=[B_s], writes=[B_s])
        op("dve", lambda e: e.tensor_tensor(out=cs_r, in0=cs_r, in1=t_r, op=ALU.add), reads=[B_s], writes=[B_s])
        op("dve", lambda e: e.reciprocal(out=cs_r, in_=cs_r), reads=[B_s], writes=[B_s])
        rden = cs_r
        op("dve", lambda e: e.tensor_tensor(out=em1_r, in0=nr_r, in1=lr_r, op=ALU.mult), reads=[B_s], writes=[B_s])
        op("dve", lambda e: e.tensor_tensor(out=t_r, in0=ni_r, in1=li_r, op=ALU.mult), reads=[B_s], writes=[B_s])
        op("dve", lambda e: e.tensor_tensor(out=em1_r, in0=em1_r, in1=t_r, op=ALU.add), reads=[B_s], writes=[B_s])
        op("dve", lambda e: e.tensor_tensor(out=em1_r, in0=em1_r, in1=rden, op=ALU.mult), reads=[B_s], writes=[B_s])
        op("dve", lambda e: e.tensor_tensor(out=sh_r, in0=ni_r, in1=lr_r, op=ALU.mult), reads=[B_s], writes=[B_s])
        op("dve", lambda e: e.tensor_tensor(out=t_r, in0=nr_r, in1=li_r, op=ALU.mult), reads=[B_s], writes=[B_s])
        op("dve", lambda e: e.tensor_tensor(out=sh_r, in0=sh_r, in1=t_r, op=ALU.subtract), reads=[B_s], writes=[B_s])
        op("dve", lambda e: e.tensor_tensor(out=sh_r, in0=sh_r, in1=rden, op=ALU.mult), reads=[B_s], writes=[B_s])
        k_re, k_im = em1_r, sh_r
        op("dve", lambda e: e.tensor_tensor(out=z_r, in0=k_re, in1=bt[:, 0, :], op=ALU.mult), reads=[B_s], writes=[B_s])
        op("dve", lambda e: e.tensor_tensor(out=t_r, in0=k_im, in1=bt[:, 1, :], op=ALU.mult), reads=[B_s], writes=[B_s])
        op("dve", lambda e: e.tensor_tensor(out=z_r, in0=z_r, in1=t_r, op=ALU.subtract), reads=[B_s], writes=[B_s])
        op("dve", lambda e: e.tensor_tensor(out=sn_r, in0=k_re, in1=bt[:, 1, :], op=ALU.mult), reads=[B_s], writes=[B_s])
        op("dve", lambda e: e.tensor_tensor(out=t_r, in0=k_im, in1=bt[:, 0, :], op=ALU.mult), reads=[B_s], writes=[B_s])
        op("dve", lambda e: e.tensor_tensor(out=sn_r, in0=sn_r, in1=t_r, op=ALU.add), reads=[B_s], writes=[B_s])
        bre3 = R[1][:].rearrange("p (g s) -> p g s", g=8)
        bim3 = R[5][:].rearrange("p (g s) -> p g s", g=8)
        op("dve", lambda e, gs=gs: e.tensor_copy(out=bpq[:, gs, 0, 0:64], in_=bre3), reads=[B_s], writes=[B_s])
        op("dve", lambda e, gs=gs: e.tensor_copy(out=bpq[:, gs, 0, 64:128], in_=bim3), reads=[B_s], writes=[B_s])
        op("dve", lambda e, gs=gs: e.tensor_copy(out=bpq[:, gs, 1, 0:64], in_=bim3), reads=[B_s], writes=[B_s])
        op("dve", lambda e, gs=gs: e.tensor_scalar(out=bpq[:, gs, 1, 64:128], in0=bre3, scalar1=-1.0, scalar2=None, op0=ALU.mult), reads=[B_s], writes=[B_s])

    S.barrier()
    A0.close()
    x_tok = [sb("x_tok%d" % i, [128, D]) for i in range(3)]
    B_x = [Buf("x_tok%d" % i) for i in range(3)]
    h_bf = sb("h_bf", [128, 1, D], BF16)
    B_h = [Buf("h0")]
    hT2 = [sb("hT%d" % i, [128, 8, TB], BF16) for i in range(2)]
    B_hT2 = [Buf("hT0"), Buf("hT1")]
    v_sb = sb("v_sb", [128, 6, TB + 2])
    B_v = [Buf("v%d" % c) for c in range(6)]
    cc_sb = sb("cc_sb", [128, 2, TB])
    B_cc = [Buf("cc0"), Buf("cc1")]
    init = sb("init", [128, G, 2])
    B_init = [Buf("init%d" % g) for g in range(G)]
    op("pool", lambda e: e.memset(v_sb[:], 0.0), writes=B_v)
    op("pool", lambda e: e.memset(init[:], 0.0), writes=B_init)

    blocks = [("pre", i) for i in range(NB)] + [("main", i) for i in range(NB)]

    def proj(par, mt):
        ps, b_ps = next_ps()
        mm_group(ps[:, 0:TB], b_ps,
                 [(w_in_sb[:, k, mt * 128:(mt + 1) * 128], hT2[par][:, k, :]) for k in range(8)], [B_w, B_hT2[par]])
        return ps, b_ps

    def alloc_main():
        g = {}
        g["ct1"] = sb("ct1", [128, 2, TB]); g["ct2"] = sb("ct2", [128, 2, TB])
        g["outA2"] = [sb("outA%d" % i, [128, 6, TB], BF16) for i in range(2)]
        g["uT2"] = [sb("uT%d" % i, [128, 2, TB], BF16) for i in range(2)]
        g["u_f2"] = [sb("u_f%d" % i, [128, 2, TB]) for i in range(2)]
        g["gates2"] = [sb("gates%d" % i, [128, 16, TB], BF16) for i in range(2)]
        g["st"] = [[sb("st%d_%d" % (i, j), [128, TB]) for j in range(NST)] for i in range(NSB)]
        g["s_bf"] = sb("s_bf", [128, NSB, TB], BF16)
        g["itmp"] = sb("itmp", [128, G, 2])
        g["ctmp"] = sb("ctmp", [128, 4, G])
        g["y_sb"] = sb("y_sb", [128, 2, TB]); g["z_f"] = sb("z_f", [128, 2, TB]); g["z_b"] = sb("z_b", [128, 2, TB], BF16)
        g["sg"] = sb("sg", [128, 2, TB]); g["outB"] = sb("outB", [128, 2, TB], BF16)
        g["m1"] = sb("m1", [128, 1, TB]); g["m2"] = sb("m2", [128, 1, TB]); g["merged"] = sb("merged", [128, 8, TB], BF16)
        g["stg32"] = [sb("stg32_%d" % i, [128, 512]) for i in range(2)]
        g["stgb"] = [sb("stgb_%d" % i, [128, 512], BF16) for i in range(2)]
        return g

    NSB = 3
    NST = 10
    B_ct = [Buf("ct0"), Buf("ct1")]
    B_outA2 = [Buf("outA0"), Buf("outA1")]
    B_u2 = [Buf("u0"), Buf("u1")]
    B_g2 = [Buf("gates0"), Buf("gates1")]
    B_st = [[Buf("st%d_%d" % (i, j)) for j in range(NST)] for i in range(NSB)]
    B_sbf = [Buf("sbf%d" % i) for i in range(NSB)]
    B_y = [Buf("y0"), Buf("y1")]
    B_outB = Buf("outB")
    B_m = [Buf("m0"), Buf("m1")]
    B_mg = Buf("merged")
    B_wl = Buf("wl")
    B_ctmp = Buf("ctmp")
    B_yacc = [Buf("yacc0"), Buf("yacc1")]
    B_stg = [Buf("stg0"), Buf("stg1")]
    pre_chunks = [(tb, ch, hf) for tb in range(2) for ch in range(NE // 128) for hf in range(2)]
    pre_i = [0]
    n_pre_per_blk = -(-len(pre_chunks) // NB)
    MA = {}
    B_stg32 = [Buf("stg32_0"), Buf("stg32_1")]

    def precast_gen(n):
        if SKIP_PRECAST:
            return
        stg32, stgb = MA["stg32"], MA["stgb"]

        def load(i):
            tb, ch, hf = pre_chunks[i]
            src = (exp_u if tb == 0 else exp_v)[ch * 128:(ch + 1) * 128, hf * 512:(hf + 1) * 512]
            dma("act", lambda e, src=src, j=i % 2: e.dma_start(out=stg32[j][:], in_=src), writes=[B_stg32[i % 2]])

        for _ in range(n):
            i = pre_i[0]
            if i >= len(pre_chunks):
                return
            pre_i[0] += 1
            if i == 0:
                load(0)
            if i + 1 < len(pre_chunks):
                pass
            tb, ch, hf = pre_chunks[i]
            j = i % 2
            dst = uv_d[ch * 128:(ch + 1) * 128, tb * D + hf * 512:tb * D + (hf + 1) * 512]
            op("act", lambda e, j=j: e.activation(out=stgb[j][:], in_=stg32[j][:], func=AF.Copy), reads=[B_stg32[j]], writes=[B_stg[j]])
            if i + 1 < len(pre_chunks):
                load(i + 1)
            dma("act", lambda e, dst=dst, j=j: e.dma_start(out=dst, in_=stgb[j][:]), reads=[B_stg[j]], writes=[B_uv])
            yield

    def load_norm_T(bi):
        kind, blk = blocks[bi]
        par = bi % 2
        xsrc = x_main if kind == "main" else x_prev
        xt, b_xt = x_tok[bi % 3], B_x[bi % 3]
        dma("sp", lambda e: e.dma_start(out=xt[:], in_=xsrc[blk * TB:(blk + 1) * TB, :]), writes=[b_xt])
        norm_tile(xt[:], b_xt, g_sb[:], B_c, h_bf[:, 0, :], B_h[0], 4 * par)
        transposes(h_bf[:, 0, :], B_h[0], hT2[par], B_hT2[par])

    def front(bi):
        kind, blk = blocks[bi]
        par = bi % 2
        load_norm_T(bi)
        yield
        for m in range(2):
            ps_u, b_u = proj(par, 18 + m)
            op("act", lambda e, m=m, ps_u=ps_u: e.activation(out=u_f2[par][:, m, :], in_=ps_u[:, 0:TB], func=AF.Copy), reads=[b_u], writes=[B_u2[par]])
            op("act", lambda e, m=m, ps_u=ps_u: e.activation(out=uT2[par][:, m, :], in_=ps_u[:, 0:TB], func=AF.Copy), reads=[b_u], writes=[B_u2[par]])
            yield

    def delayed(gen, n):
        for _ in range(n):
            yield
        for _ in gen:
            yield

    def convgate(bi):
        kind, blk = blocks[bi]
        par = bi % 2
        is_main = kind == "main"
        last_pre = (kind == "pre" and blk == NB - 1)
        if is_main:
            outA, B_outA = outA2[par], B_outA2[par]
            gates, B_g = gates2[par], B_g2[par]
        if is_main or last_pre:
            for c in range(6):
                j = c % 2
                ps_c, b_c = proj(par, 6 + c)
                ps_x, b_xx = proj(par, 12 + c)
                op("act", lambda e, ps_c=ps_c, j=j: e.activation(out=cc_sb[:, j, :], in_=ps_c[:, 0:TB], func=AF.Copy),
                   reads=[b_c], writes=[B_cc[j]])
                op("dve", lambda e, ps_x=ps_x, j=j, c=c: e.tensor_tensor(out=v_sb[:, c, 2:TB + 2], in0=cc_sb[:, j, :], in1=ps_x[:, 0:TB], op=ALU.mult),
                   reads=[B_cc[j], b_xx], writes=[B_v[c]])
                yield
                if is_main:
                    ps_b, b_b = proj(par, c)
                    op("pool", lambda e, c=c, j=j: e.tensor_scalar(out=ct1[:, j, :], in0=v_sb[:, c, 2:TB + 2], scalar1=CW(c, 2), scalar2=CBIAS(c),
                                                                   op0=ALU.mult, op1=ALU.add), reads=[B_v[c], B_c], writes=[B_ct[j]])
                    op("dve", lambda e, c=c, j=j: e.scalar_tensor_tensor(out=ct2[:, j, :], in0=v_sb[:, c, 1:TB + 1], scalar=CW(c, 1), in1=ct1[:, j, :],
                                                                         op0=ALU.mult, op1=ALU.add), reads=[B_v[c], B_c, B_ct[j]], writes=[B_ct[j]])
                    op("dve", lambda e, c=c, j=j: e.scalar_tensor_tensor(out=ct1[:, j, :], in0=v_sb[:, c, 0:TB], scalar=CW(c, 0), in1=ct2[:, j, :],
                                                                         op0=ALU.mult, op1=ALU.add), reads=[B_v[c], B_c, B_ct[j]], writes=[B_ct[j]])
                    op("dve", lambda e, c=c, j=j, ps_b=ps_b: e.tensor_tensor(out=outA[:, c, :], in0=ct1[:, j, :], in1=ps_b[:, 0:TB], op=ALU.mult),
                       reads=[B_ct[j], b_b], writes=[B_outA])
                op("pool", lambda e, c=c: e.tensor_copy(out=v_sb[:, c, 0:2], in_=v_sb[:, c, TB:TB + 2]), reads=[B_v[c]], writes=[B_v[c]])
                yield
        if is_main:
            for t in range(16):
                ps_g, b_gg = proj(par, 20 + t)
                op("act", lambda e, t=t, ps_g=ps_g: e.activation(out=gates[:, t, :], in_=ps_g[:, 0:TB], func=AF.Sigmoid,
                                                                 bias=colp_sb[:, t:t + 1], scale=1.0), reads=[b_gg, B_c], writes=[B_g])
                if t % 2 == 1:
                    yield

    def ssm(bi):
        kind, blk = blocks[bi]
        par = bi % 2
        is_main = kind == "main"
        uT, B_u = uT2[par], B_u2[par]
        pq = {}

        def stage0(g):
            ps_p, b_p = ps_mm[3 + (g % 2)], B_ps[3 + (g % 2)]
            m = g // 8
            mm_group(ps_p[:, 0:TB], b_p, [(bpq[:, g, 0, :], uT[:, m, :])], [B_s, B_u])
            mm_group(ps_p[:, TB:2 * TB], b_p, [(bpq[:, g, 1, :], uT[:, m, :])], [B_s, B_u])
            pq[g] = (ps_p, b_p)

        def bufs(g):
            si = g % NSB
            return [st[si][j][:] for j in range(NST)], B_st[si], si

        def stage1(g):
            (R1, R2, W1, W2, T1, T2, T3, T4, T5, T6), (bR1, bR2, bW1, bW2, bT1, bT2, bT3, bT4, bT5, bT6), si = bufs(g)
            ps_p, b_p = pq.pop(g)
            Pp, Qp = ps_p[:, 0:TB], ps_p[:, TB:2 * TB]
            C_g, S_g = ctab[:, g, 0:TB], stab[:, g, 0:TB]
            op("dve", lambda e: e.tensor_tensor(out=T1, in0=C_g, in1=Pp, op=ALU.mult), reads=[B_s, b_p], writes=[bT1])
            op("dve", lambda e: e.tensor_tensor(out=T2, in0=S_g, in1=Qp, op=ALU.mult), reads=[B_s, b_p], writes=[bT2])
            op("dve", lambda e: e.tensor_tensor(out=T3, in0=C_g, in1=Qp, op=ALU.mult), reads=[B_s, b_p], writes=[bT3])
            op("dve", lambda e: e.tensor_tensor(out=T4, in0=S_g, in1=Pp, op=ALU.mult), reads=[B_s, b_p], writes=[bT4])

        def stage2(g):
            (R1, R2, W1, W2, T1, T2, T3, T4, T5, T6), (bR1, bR2, bW1, bW2, bT1, bT2, bT3, bT4, bT5, bT6), si = bufs(g)
            op("dve", lambda e: e.tensor_tensor(out=R1, in0=T1, in1=T2, op=ALU.add), reads=[bT1, bT2], writes=[bR1])
            op("pool", lambda e: e.tensor_tensor(out=R2, in0=T3, in1=T4, op=ALU.subtract), reads=[bT3, bT4], writes=[bR2])

        def stage3(g):
            (R1, R2, W1, W2, T1, T2, T3, T4, T5, T6), (bR1, bR2, bW1, bW2, bT1, bT2, bT3, bT4, bT5, bT6), si = bufs(g)
            rb = r_p[:, g:g + 1].to_broadcast([128, TB])
            op("dve", lambda e: e.tensor_tensor_scan(out=W1, data0=rb, data1=R1, initial=init[:, g, 0:1], op0=ALU.mult, op1=ALU.add),
               reads=[B_s, bR1, B_init[g]], writes=[bW1])
            op("dve", lambda e: e.tensor_tensor_scan(out=W2, data0=rb, data1=R2, initial=init[:, g, 1:2], op0=ALU.mult, op1=ALU.add),
               reads=[B_s, bR2, B_init[g]], writes=[bW2])

        def stage4(g):
            (R1, R2, W1, W2, T1, T2, T3, T4, T5, T6), (bR1, bR2, bW1, bW2, bT1, bT2, bT3, bT4, bT5, bT6), si = bufs(g)
            C_g, S_g = ctab[:, g, 0:TB], stab[:, g, 0:TB]
            op("act", lambda e: e.activation(out=itmp[:, g, 0:1], in_=W1[:, TB - 1:TB], func=AF.Copy), reads=[bW1], writes=[B_wl])
            op("act", lambda e: e.activation(out=itmp[:, g, 1:2], in_=W2[:, TB - 1:TB], func=AF.Copy), reads=[bW2], writes=[B_wl])
            op("pool", lambda e: e.tensor_tensor(out=T5, in0=C_g, in1=W1, op=ALU.mult), reads=[B_s, bW1], writes=[bT5])
            op("pool", lambda e: e.tensor_tensor(out=T6, in0=S_g, in1=W2, op=ALU.mult), reads=[B_s, bW2], writes=[bT6])
            op("pool", lambda e: e.tensor_tensor(out=s_bf[:, si, :], in0=T5, in1=T6, op=ALU.subtract), reads=[bT5, bT6], writes=[B_sbf[si]])

        def stage5(g):
            si = g % NSB
            m = g // 8
            op("pe", lambda e: e.matmul(ps_acc[m][:, 0:TB], lhsT=ctb[:, g, :], rhs=s_bf[:, si, :], start=(g % 8 == 0), stop=(g % 8 == 7)),
               reads=[B_s, B_sbf[si]], writes=[B_yacc[m]])

        stages = [stage0, stage1, stage2, stage3, stage4, stage5]
        for step in range(G + len(stages) - 1):
            for k, fn in enumerate(stages):
                if 0 <= step - k < G:
                    fn(step - k)
            yield
        cT, sT = ctab[:, :, TB], stab[:, :, TB]
        op("dve", lambda e: e.tensor_tensor(out=ctmp[:, 0, :], in0=cT, in1=itmp[:, :, 0], op=ALU.mult), reads=[B_s, B_wl], writes=[B_ctmp])
        op("dve", lambda e: e.tensor_tensor(out=ctmp[:, 1, :], in0=sT, in1=itmp[:, :, 1], op=ALU.mult), reads=[B_s, B_wl], writes=[B_ctmp])
        op("dve", lambda e: e.tensor_tensor(out=init[:, :, 0], in0=ctmp[:, 0, :], in1=ctmp[:, 1, :], op=ALU.subtract), reads=[B_ctmp] + B_init, writes=B_init)
        op("dve", lambda e: e.tensor_tensor(out=ctmp[:, 2, :], in0=cT, in1=itmp[:, :, 1], op=ALU.mult), reads=[B_s, B_wl], writes=[B_ctmp])
        op("dve", lambda e: e.tensor_tensor(out=ctmp[:, 3, :], in0=sT, in1=itmp[:, :, 0], op=ALU.mult), reads=[B_s, B_wl], writes=[B_ctmp])
        op("dve", lambda e: e.tensor_tensor(out=init[:, :, 1], in0=ctmp[:, 2, :], in1=ctmp[:, 3, :], op=ALU.add), reads=[B_ctmp] + B_init, writes=B_init)
        yield

    def tail(bi):
        kind, blk = blocks[bi]
        if kind != "main":
            return
        par = bi % 2
        xt, b_xt = x_tok[bi % 3], B_x[bi % 3]
        outA, B_outA = outA2[par], B_outA2[par]
        gates, B_g = gates2[par], B_g2[par]
        u_f, B_u = u_f2[par], B_u2[par]
        for m in range(2):
            op("dve", lambda e, m=m: e.scalar_tensor_tensor(out=y_sb[:, m, :], in0=u_f[:, m, :], scalar=colp_sb[:, 42 + m:43 + m], in1=ps_acc[m][:, 0:TB],
                                                            op0=ALU.mult, op1=ALU.add), reads=[B_u, B_c, B_yacc[m]], writes=[B_y[m]])
            if debug:
                dma("sp", lambda e, m=m: e.dma_start(out=dbg_d[:, m * S_half + blk * TB:m * S_half + (blk + 1) * TB], in_=y_sb[:, m, :]), reads=[B_y[m]])
            op("act", lambda e, m=m: e.activation(out=z_f[:, m, :], in_=y_sb[:, m, :], func=AF.Gelu), reads=[B_y[m]], writes=[B_y[m]])
            op("pool", lambda e, m=m: e.tensor_copy(out=z_b[:, m, :], in_=z_f[:, m, :]), reads=[B_y[m]], writes=[B_y[m]])
            yield
        for mo in range(2):
            ps_g2, b_g2 = next_ps()
            mm_group(ps_g2[:, 0:TB], b_g2, [(w_glu_sb[:, mi, mo * 128:(mo + 1) * 128], z_b[:, mi, :]) for mi in range(2)], [B_w, B_y[0], B_y[1]])
            op("act", lambda e, mo=mo, ps_g2=ps_g2: e.activation(out=sg[:, mo, :], in_=ps_g2[:, 0:TB], func=AF.Sigmoid,
                                                                 bias=colp_sb[:, 40 + mo:41 + mo], scale=1.0), reads=[b_g2, B_c], writes=[B_y[mo]])
            op("pool", lambda e, mo=mo: e.tensor_tensor(out=outB[:, mo, :], in0=z_f[:, mo, :], in1=sg[:, mo, :], op=ALU.mult), reads=[B_y[mo]], writes=[B_outB])
            yield
        for dm in range(8):
            j = 0
            ps_a, b_a = next_ps()
            mm_group(ps_a[:, 0:TB], b_a, [(w_a_sb[:, c, dm * 128:(dm + 1) * 128], outA[:, c, :]) for c in range(6)], [B_w, B_outA])
            mm_group(ps_a[:, TB:2 * TB], b_a, [(w_b_sb[:, mi, dm * 128:(dm + 1) * 128], outB[:, mi, :]) for mi in range(2)], [B_w, B_outB])
            op("dve", lambda e, dm=dm, j=j, ps_a=ps_a: e.tensor_tensor(out=m1[:, j, :], in0=gates[:, dm, :], in1=ps_a[:, 0:TB], op=ALU.mult), reads=[B_g, b_a], writes=[B_m[j]])
            op("dve", lambda e, dm=dm, j=j, ps_a=ps_a: e.tensor_tensor(out=m2[:, j, :], in0=gates[:, 8 + dm, :], in1=ps_a[:, TB:2 * TB], op=ALU.mult), reads=[B_g, b_a], writes=[B_m[j]])
            op("pool", lambda e, dm=dm, j=j: e.tensor_tensor(out=merged[:, dm, :], in0=m1[:, j, :], in1=m2[:, j, :], op=ALU.add), reads=[B_m[j]], writes=[B_mg])
            yield
        for hf in range(2):
            ps_o, b_o = next_ps()
            mm_group(ps_o[:, :], b_o, [(merged[:, k, :], w_out_sb[:, k, hf * 512:(hf + 1) * 512]) for k in range(8)], [B_w, B_mg])
            op("dve", lambda e, hf=hf, ps_o=ps_o: e.tensor_tensor(out=xt[:, hf * 512:(hf + 1) * 512], in0=xt[:, hf * 512:(hf + 1) * 512],
                                                                  in1=ps_o[:, :], op=ALU.add), reads=[b_xt, b_o], writes=[b_xt])
            yield
        dma("sp", lambda e: e.dma_start(out=x1_d[blk * TB:(blk + 1) * TB, :], in_=xt[:]), reads=[b_xt], writes=[B_x1d])

    def interleave(gens):
        gens = [g for g in gens if g is not None]
        while gens:
            alive = []
            for g in gens:
                try:
                    next(g)
                    alive.append(g)
                except StopIteration:
                    pass
            gens = alive

    Apre = Alloc(nc)
    u_tok2 = [Apre.sb("u_tok%d" % i, [128, 256], BF16) for i in range(2)]
    B_ut = [Buf("ut0"), Buf("ut1")]
    pt_t = [Apre.sb("pt_t%d" % i, [128, 4, 64]) for i in range(4)]
    B_ptt = [Buf("ptt%d" % i) for i in range(4)]
    Uc = Apre.sb("Uc", [128, 8, 128])
    Us = Apre.sb("Us", [128, 8, 128])
    ta = Apre.sb("ta", [128, 8, 64])
    tb_ = Apre.sb("tb_", [128, 8, 64])
    ones2 = Apre.sb("ones2", [128, 2])
    S_sb = Apre.sb("S_sb", [128, 2, G])
    tq = Apre.sb("tq", [128, 2, G])
    B_fin = Buf("fin")
    B_M = Buf("Mst")
    zbanks = [ps_acc[0], ps_acc[1], ps_mm[3], ps_mm[4]]
    B_zb = [Buf("zb%d" % i) for i in range(4)]
    ps_pool[:] = [0, 1, 2]
    op("pool", lambda e: e.memset(Mst[:], 0.0), writes=[B_M])
    op("pool", lambda e: e.memset(ones2[:], 1.0), writes=[B_fin])
    def pre_B(j):
        par = j % 2
        ps_u, b_u = next_ps()
        mm_group(ps_u[:, 0:256], b_u, [(hT2[par][:, k, :], w_in_sb[:, k, 2304:2560]) for k in range(8)], [B_w, B_hT2[par]])
        op("act", lambda e, par=par, ps_u=ps_u: e.activation(out=u_tok2[par][:], in_=ps_u[:, 0:256], func=AF.Copy), reads=[b_u], writes=[B_ut[par]])
        if j == NB - 1:
            for _ in convgate(j):
                pass

    def pre_C(j):
        par = j % 2
        l = (LP - 1) - (j % LP)
        for zi in range(4):
            m = zi // 2
            op("pe", lambda e, zi=zi, m=m, l=l, par=par, j=j: e.matmul(zbanks[zi][:, :], lhsT=u_tok2[par][:, m * 128:(m + 1) * 128],
                                                                   rhs=PT[:, l, 4 * zi:4 * zi + 4, :].rearrange("p g c -> p (g c)"),
                                                                   start=(j % LP == 0), stop=(j % LP == LP - 1)),
               reads=[B_ut[par], B_s], writes=[B_zb[zi]])
        if j % LP == LP - 1:
            for zi in range(4):
                gsl = slice(4 * zi, 4 * zi + 4)
                Mre, Mim = Mst[:, gsl, 0:64], Mst[:, gsl, 64:128]
                cr = CRI[:, 0, 256 * zi:256 * (zi + 1)].rearrange("p (g s) -> p g s", g=4)
                ci = CRI[:, 1, 256 * zi:256 * (zi + 1)].rearrange("p (g s) -> p g s", g=4)
                Z3 = zbanks[zi][:, :].rearrange("p (g c) -> p g c", g=4)
                Zre, Zim = Z3[:, :, 0:64], Z3[:, :, 64:128]
                t1, t2, t3, t4 = [t[:] for t in pt_t]
                b1, b2, b3, b4 = B_ptt
                op("pool", lambda e, cr=cr, Mre=Mre, t1=t1: e.tensor_tensor(out=t1, in0=cr, in1=Mre, op=ALU.mult), reads=[B_s, B_M], writes=[b1])
                op("pool", lambda e, ci=ci, Mim=Mim, t2=t2: e.tensor_tensor(out=t2, in0=ci, in1=Mim, op=ALU.mult), reads=[B_s, B_M], writes=[b2])
                op("pool", lambda e, cr=cr, Mim=Mim, t3=t3: e.tensor_tensor(out=t3, in0=cr, in1=Mim, op=ALU.mult), reads=[B_s, B_M], writes=[b3])
                op("pool", lambda e, ci=ci, Mre=Mre, t4=t4: e.tensor_tensor(out=t4, in0=ci, in1=Mre, op=ALU.mult), reads=[B_s, B_M], writes=[b4])
                op("pool", lambda e, t1=t1, t2=t2: e.tensor_tensor(out=t1, in0=t1, in1=t2, op=ALU.subtract), reads=[b1, b2], writes=[b1])
                op("pool", lambda e, t3=t3, t4=t4: e.tensor_tensor(out=t3, in0=t3, in1=t4, op=ALU.add), reads=[b3, b4], writes=[b3])
                op("dve", lambda e, Mre=Mre, t1=t1, Zre=Zre: e.tensor_tensor(out=Mre, in0=t1, in1=Zre, op=ALU.add), reads=[b1, B_zb[zi]], writes=[B_M])
                op("dve", lambda e, Mim=Mim, t3=t3, Zim=Zim: e.tensor_tensor(out=Mim, in0=t3, in1=Zim, op=ALU.add), reads=[b3, B_zb[zi]], writes=[B_M])

    hpre_bf = Apre.sb("hpre_bf", [128, 2, D], BF16)
    B_hp = [Buf("hp0"), Buf("hp1")]

    def pre_N(j):
        xt, b_xt = x_tok[j % 3], B_x[j % 3]
        dma("sp", lambda e: e.dma_start(out=xt[:], in_=x_prev[j * TB:(j + 1) * TB, :]), writes=[b_xt])
        norm_tile(xt[:], b_xt, g_sb[:], B_c, hpre_bf[:, j % 2, :], B_hp[j % 2], 4 * (j % 2))

    def pre_T(j):
        transposes(hpre_bf[:, j % 2, :], B_hp[j % 2], hT2[j % 2], B_hT2[j % 2])

    for k in range(NB + 3):
        if k < NB:
            pre_N(k)
        if 0 <= k - 1 < NB:
            pre_T(k - 1)
        if 0 <= k - 2 < NB:
            pre_B(k - 2)
        if 0 <= k - 3 < NB:
            pre_C(k - 3)
    ps_S, b_S = next_ps()
    for m in range(2):
        gsl = slice(8 * m, 8 * m + 8)
        Bre, Bim = bpq[:, gsl, 0, 0:64], bpq[:, gsl, 0, 64:128]
        Mre, Mim = Mst[:, gsl, 0:64], Mst[:, gsl, 64:128]
        op("dve", lambda e, Bre=Bre, Mre=Mre: e.tensor_tensor(out=ta[:], in0=Bre, in1=Mre, op=ALU.mult), reads=[B_s, B_M], writes=[B_fin])
        op("dve", lambda e, Bim=Bim, Mim=Mim: e.tensor_tensor(out=tb_[:], in0=Bim, in1=Mim, op=ALU.mult), reads=[B_s, B_M], writes=[B_fin])
        op("dve", lambda e: e.tensor_tensor(out=Uc[:, :, 0:64], in0=ta[:], in1=tb_[:], op=ALU.subtract), reads=[B_fin], writes=[B_fin])
        op("dve", lambda e, Bre=Bre, Mim=Mim: e.tensor_tensor(out=ta[:], in0=Bre, in1=Mim, op=ALU.mult), reads=[B_s, B_M, B_fin], writes=[B_fin])
        op("dve", lambda e, Bim=Bim, Mre=Mre: e.tensor_tensor(out=tb_[:], in0=Bim, in1=Mre, op=ALU.mult), reads=[B_s, B_M, B_fin], writes=[B_fin])
        op("dve", lambda e: e.tensor_tensor(out=Uc[:, :, 64:128], in0=ta[:], in1=tb_[:], op=ALU.add), reads=[B_fin], writes=[B_fin])
        op("dve", lambda e: e.tensor_copy(out=Us[:, :, 0:64], in_=Uc[:, :, 64:128]), reads=[B_fin], writes=[B_fin])
        op("dve", lambda e: e.tensor_scalar(out=Us[:, :, 64:128], in0=Uc[:, :, 0:64], scalar1=-1.0, scalar2=None, op0=ALU.mult), reads=[B_fin], writes=[B_fin])
        for gl_ in range(8):
            g = 8 * m + gl_
            op("pe", lambda e, gl_=gl_, g=g: e.matmul(ps_S[:, 2 * g:2 * g + 2], lhsT=Uc[:, gl_, :], rhs=ones2[:, :], start=True, stop=True), reads=[B_fin], writes=[b_S])
            op("pe", lambda e, gl_=gl_, g=g: e.matmul(ps_S[:, 64 + 2 * g:64 + 2 * g + 2], lhsT=Us[:, gl_, :], rhs=ones2[:, :], start=True, stop=True), reads=[B_fin], writes=[b_S])
    for a in range(2):
        op("act", lambda e, a=a: e.activation(out=S_sb[:, a, :], in_=ps_S[:, 64 * a:64 * a + 32].rearrange("p (g t) -> p g t", t=2)[:, :, 0], func=AF.Copy),
           reads=[b_S, B_fin], writes=[B_fin])
    c1, s1 = ctab[:, :, 1], stab[:, :, 1]
    op("dve", lambda e: e.tensor_tensor(out=tq[:, 0, :], in0=c1, in1=S_sb[:, 0, :], op=ALU.mult), reads=[B_s, B_fin], writes=[B_fin])
    op("dve", lambda e: e.tensor_tensor(out=tq[:, 1, :], in0=s1, in1=S_sb[:, 1, :], op=ALU.mult), reads=[B_s, B_fin], writes=[B_fin])
    op("dve", lambda e: e.tensor_tensor(out=init[:, :, 0], in0=tq[:, 0, :], in1=tq[:, 1, :], op=ALU.subtract), reads=[B_fin] + B_init, writes=B_init)
    op("dve", lambda e: e.tensor_tensor(out=tq[:, 0, :], in0=c1, in1=S_sb[:, 1, :], op=ALU.mult), reads=[B_s, B_fin] + B_init, writes=[B_fin])
    op("dve", lambda e: e.tensor_tensor(out=tq[:, 1, :], in0=s1, in1=S_sb[:, 0, :], op=ALU.mult), reads=[B_s, B_fin], writes=[B_fin])
    op("dve", lambda e: e.tensor_tensor(out=init[:, :, 1], in0=tq[:, 0, :], in1=tq[:, 1, :], op=ALU.add), reads=[B_fin] + B_init, writes=B_init)
    S.barrier()
    Apre.close()
    AXr.close()
    ps_pool[:] = [0, 1, 2]

    MA.update(alloc_main())
    ct1, ct2, outA2, uT2, u_f2, gates2, st, s_bf, itmp, ctmp = [MA[k] for k in ("ct1", "ct2", "outA2", "uT2", "u_f2", "gates2", "st", "s_bf", "itmp", "ctmp")]
    y_sb, z_f, z_b, sg, outB, m1, m2, merged = [MA[k] for k in ("y_sb", "z_f", "z_b", "sg", "outB", "m1", "m2", "merged")]
    nblk = len(blocks)
    for _ in front(NB):
        pass
    for bi in range(NB, nblk + 1):
        gl = []
        if bi < nblk:
            gl.append(ssm(bi))
            gl.append(convgate(bi))
        if bi - 1 >= NB:
            gl.append(tail(bi - 1))
        if bi + 1 < nblk:
            gl.append(delayed(front(bi + 1), 12))
        if bi < nblk:
            gl.append(precast_gen(n_pre_per_blk))
        interleave(gl)

    S.barrier()
    A.close()
    ps_pool[:] = list(range(NPS))
    if do_b:
        Bq = Alloc(nc)
        sb = Bq.sb
        NT = S_half // 128
        w_q_sb = sb("w_q_sb", [128, 8, NQ], BF16)
        keys_sb = sb("keys_sb", [128, 2, 128], BF16)
        g2_sb = sb("g2_sb", [128, 2, D])
        B_wq = Buf("wq")
        for kc in range(8):
            dma("pool", lambda e, kc=kc: e.dma_start(out=w_q_sb[:, kc, :], in_=w_q[kc * 128:(kc + 1) * 128, :]), writes=[B_wq])
        for c in range(2):
            dma("pool", lambda e, c=c: e.dma_start(out=keys_sb[:, c, :], in_=keysT[c]), writes=[B_wq])
            dma("sp", lambda e, c=c: e.dma_start(out=g2_sb[:, c, :], in_=g_rep[1 + c]), writes=[B_wq])
        iota4 = sb("iota4", [128, 8, 16, 16])
        op("pool", lambda e: e.iota(iota4[:].rearrange("p h k a -> p (h k) a"), [[0, 128], [1, 16]], base=0, channel_multiplier=0,
                                    allow_small_or_imprecise_dtypes=True), writes=[B_wq])
        x1t = [sb("x1t%d" % i, [128, D]) for i in range(3)]
        B_x1 = [Buf("x1t0"), Buf("x1t1"), Buf("x1t2")]
        xn_f2 = [sb("xn_f%d" % i, [128, D]) for i in range(2)]
        B_xn2 = [Buf("xn0"), Buf("xn1")]
        xn_b = sb("xn_b", [128, D], BF16)
        B_xnb = Buf("xn_b")
        xnT = sb("xnT", [128, 8, 128], BF16)
        B_xnT = Buf("xnT")
        qT = sb("qT", [128, 16, 128], BF16)
        B_qT = Buf("qT")
        s_sb = sb("s_sb", [128, 16, 128])
        B_ssb = Buf("s_sb")
        srep = sb("srep", [128, 128])
        v12 = sb("v12", [128, 8, 2, 16])
        i12u = sb("i12u", [128, 8, 2, 16], U32)
        i12f = sb("i12f", [128, 8, 2, 16])
        cand = sb("cand", [128, 8, 16, 16])
        crep = sb("crep", [128, 256])
        best = sb("best", [128, 8, 16])
        posu = sb("posu", [128, 8, 16], U32)
        pau = sb("pau", [128, 8, 16], U32)
        pbu = sb("pbu", [128, 8, 16], U32)
        paf = sb("paf", [128, 8, 16])
        pbf = sb("pbf", [128, 8, 16])
        eq = sb("eq", [128, 8, 16, 16])
        If = sb("If", [128, 8, 16])
        Jf = sb("Jf", [128, 8, 16])
        eg = sb("eg", [128, 8, 16])
        zs = sb("zs", [128, 8])
        eidx2 = [sb("eidx%d" % i, [128, 128], I32) for i in range(2)]
        gate2 = [sb("gate%d" % i, [128, 128]) for i in range(2)]
        B_eg2 = [Buf("eg0"), Buf("eg1")]
        hpre = sb("hpre", [128, 128])
        B_tk = Buf("topk")
        junk_b = sb("junk_b", [128, D], BF16)
        B_jf = Buf("junk_b")
        NGB = 20
        gbuf = [sb("gbuf%d" % i, [128, 2 * D], BF16) for i in range(NGB)]
        B_gb = [Buf("gb%d" % i) for i in range(NGB)]
        gb_rr = [0]
        NHC = 4
        B_hc = [Buf("hc%d" % i) for i in range(NHC)]
        wcol = sb("wcol", [128, 2, 128])
        NDG = 6
        diag = [sb("diag%d" % i, [128, 128], BF16) for i in range(NDG)]
        B_dg = [Buf("dg%d" % i) for i in range(NDG)]
        o_t = sb("o_t", [128, D])
        B_ot = Buf("o_t")

        def prep_gen(t):
            par = t % 2
            xt, b_xt = x1t[t % 3], B_x1[t % 3]
            xn_f, B_xn = xn_f2[par], B_xn2[par]
            eidx, gate, B_eg = eidx2[par], gate2[par], B_eg2[par]
            dma("sp", lambda e: e.dma_start(out=xt[:], in_=x1_d[t * 128:(t + 1) * 128, :]), reads=[B_x1d], writes=[b_xt])
            norm_tile(xt[:], b_xt, g2_sb[:, 0, :], B_wq, xn_f[:], B_xn, 0)
            op("act", lambda e: e.activation(out=xn_b[:], in_=xn_f[:], func=AF.Copy), reads=[B_xn], writes=[B_xnb])
            yield
            transposes(xn_b[:], B_xnb, xnT, B_xnT)
            yield
            pend = None
            for qt in range(16):
                ps_q, b_q = next_ps()
                mm_group(ps_q[:, 0:128], b_q, [(w_q_sb[:, k, qt * 128:(qt + 1) * 128], xnT[:, k, :]) for k in range(8)], [B_wq, B_xnT])
                if pend is not None:
                    pend()
                pend = (lambda qt=qt, ps_q=ps_q, b_q=b_q: op("act", lambda e: e.activation(out=qT[:, qt, :], in_=ps_q[:, 0:128], func=AF.Copy), reads=[b_q], writes=[B_qT]))
                yield
            pend()
            yield
            pend = None
            for bnk in range(4):
                ps_s, b_s = next_ps()
                for j in range(4):
                    idx = bnk * 4 + j
                    mm_group(ps_s[:, j * 128:(j + 1) * 128], b_s, [(qT[:, idx, :], keys_sb[:, idx % 2, :])], [B_qT, B_wq])
                if pend is not None:
                    pend()
                pend = (lambda bnk=bnk, ps_s=ps_s, b_s=b_s: op("act", lambda e: e.activation(out=s_sb[:, bnk * 4:(bnk + 1) * 4, :].rearrange("p a n -> p (a n)"), in_=ps_s[:, :], func=AF.Copy),
                                                                 reads=[b_s], writes=[B_ssb]))
                yield
            pend()
            yield
            for h in range(8):
                for c in range(2):
                    sv = s_sb[:, 2 * h + c, :]
                    op("dve", lambda e, h=h, c=c, sv=sv: e.max(out=v12[:, h, c, 0:8], in_=sv), reads=[B_ssb], writes=[B_tk])
                    op("dve", lambda e, h=h, c=c, sv=sv: e.match_replace(out=srep[:], in_to_replace=v12[:, h, c, 0:8], in_values=sv, imm_value=-1e30), reads=[B_ssb, B_tk], writes=[B_tk])
                    yield
                    op("dve", lambda e, h=h, c=c: e.max(out=v12[:, h, c, 8:16], in_=srep[:]), reads=[B_tk], writes=[B_tk])
                    op("dve", lambda e, h=h, c=c, sv=sv: e.max_index(out=i12u[:, h, c, 0:8], in_max=v12[:, h, c, 0:8], in_values=sv), reads=[B_ssb, B_tk], writes=[B_tk])
                    yield
                    op("dve", lambda e, h=h, c=c: e.max_index(out=i12u[:, h, c, 8:16], in_max=v12[:, h, c, 8:16], in_values=srep[:]), reads=[B_tk], writes=[B_tk])
                    yield
            op("dve", lambda e: e.tensor_copy(out=i12f[:], in_=i12u[:]), reads=[B_tk], writes=[B_tk])
            v1b = v12[:, :, 0, :].rearrange("p h (a o) -> p h a o", o=1).to_broadcast([128, 8, 16, 16])
            v2b = v12[:, :, 1, :].rearrange("p h (o b) -> p h o b", o=1).to_broadcast([128, 8, 16, 16])
            op("dve", lambda e: e.tensor_tensor(out=cand[:], in0=v1b, in1=v2b, op=ALU.add), reads=[B_tk], writes=[B_tk])
            yield
            for h in range(8):
                ch = cand[:, h, :, :].rearrange("p a b -> p (a b)")
                op("dve", lambda e, h=h, ch=ch: e.max(out=best[:, h, 0:8], in_=ch), reads=[B_tk], writes=[B_tk])
                op("dve", lambda e, h=h, ch=ch: e.match_replace(out=crep[:], in_to_replace=best[:, h, 0:8], in_values=ch, imm_value=-1e30), reads=[B_tk], writes=[B_tk])
                yield
                op("dve", lambda e, h=h: e.max(out=best[:, h, 8:16], in_=crep[:]), reads=[B_tk], writes=[B_tk])
                op("dve", lambda e, h=h, ch=ch: e.max_index(out=posu[:, h, 0:8], in_max=best[:, h, 0:8], in_values=ch), reads=[B_tk], writes=[B_tk])
                yield
                op("dve", lambda e, h=h: e.max_index(out=posu[:, h, 8:16], in_max=best[:, h, 8:16], in_values=crep[:]), reads=[B_tk], writes=[B_tk])
                yield
            op("dve", lambda e: e.tensor_single_scalar(out=pau[:], in_=posu[:], scalar=4, op=ALU.logical_shift_right), reads=[B_tk], writes=[B_tk])
            op("dve", lambda e: e.tensor_single_scalar(out=pbu[:], in_=posu[:], scalar=15, op=ALU.bitwise_and), reads=[B_tk], writes=[B_tk])
            yield
            op("dve", lambda e: e.tensor_copy(out=paf[:], in_=pau[:]), reads=[B_tk], writes=[B_tk])
            op("dve", lambda e: e.tensor_copy(out=pbf[:], in_=pbu[:]), reads=[B_tk], writes=[B_tk])
            yield
            for (pf, c, dst) in ((paf, 0, If), (pbf, 1, Jf)):
                pfb = pf[:].rearrange("p h (k o) -> p h k o", o=1).to_broadcast([128, 8, 16, 16])
                ifb = i12f[:, :, c, :].rearrange("p h (o a) -> p h o a", o=1).to_broadcast([128, 8, 16, 16])
                op("dve", lambda e, pfb=pfb: e.tensor_tensor(out=eq[:], in0=pfb, in1=iota4[:], op=ALU.is_equal), reads=[B_tk, B_wq], writes=[B_tk])
                yield
                op("dve", lambda e, ifb=ifb: e.tensor_tensor(out=eq[:], in0=eq[:], in1=ifb, op=ALU.mult), reads=[B_tk], writes=[B_tk])
                yield
                op("dve", lambda e, dst=dst: e.tensor_reduce(out=dst[:].rearrange("p h k -> p (h k)"), in_=eq[:].rearrange("p h k a -> p (h k) a"), axis=AX.X, op=ALU.add),
                   reads=[B_tk], writes=[B_tk])
                yield
            op("dve", lambda e: e.scalar_tensor_tensor(out=paf[:], in0=If[:], scalar=128.0, in1=Jf[:], op0=ALU.mult, op1=ALU.add), reads=[B_tk], writes=[B_tk])
            op("dve", lambda e: e.tensor_copy(out=eidx[:], in_=paf[:].rearrange("p h k -> p (h k)")), reads=[B_tk, B_eg], writes=[B_eg])
            yield
            bmx = best[:, :, 0:1].to_broadcast([128, 8, 16])
            op("dve", lambda e: e.tensor_tensor(out=eg[:], in0=best[:], in1=bmx, op=ALU.subtract), reads=[B_tk], writes=[B_tk])
            op("act", lambda e: e.activation(out=eg[:], in_=eg[:], func=AF.Exp), reads=[B_tk], writes=[B_tk])
            op("dve", lambda e: e.tensor_reduce(out=zs[:], in_=eg[:], axis=AX.X, op=ALU.add), reads=[B_tk], writes=[B_tk])
            op("dve", lambda e: e.reciprocal(out=zs[:], in_=zs[:]), reads=[B_tk], writes=[B_tk])
            zb = zs[:].rearrange("p (h o) -> p h o", o=1).to_broadcast([128, 8, 16])
            op("dve", lambda e: e.tensor_tensor(out=gate[:].rearrange("p (h k) -> p h k", h=8), in0=eg[:], in1=zb, op=ALU.mult), reads=[B_tk, B_eg], writes=[B_eg])
            yield

        def run_gen(g, n=None):
            if g is None:
                return
            k = 0
            for _ in g:
                k += 1
                if n is not None and k >= n:
                    return

        gcur = prep_gen(0)
        run_gen(gcur)
        def finalize(t):
            xt, b_xt = x1t[t % 3], B_x1[t % 3]
            for hf in range(2):
                op("dve", lambda e, hf=hf, xt=xt: e.tensor_tensor(out=xt[:, hf * 512:(hf + 1) * 512], in0=xt[:, hf * 512:(hf + 1) * 512], in1=ps_acc[hf][:, :], op=ALU.add),
                   reads=[b_xt, B_pacc], writes=[b_xt])
            norm_tile(xt[:], b_xt, g2_sb[:, 1, :], B_wq, o_t[:], B_ot, 4)
            dma("sp", lambda e, t=t: e.dma_start(out=out_d[t * 128:(t + 1) * 128, :], in_=o_t[:]), reads=[B_ot])

        for t in range(NT):
            par = t % 2
            xt, b_xt = x1t[t % 3], B_x1[t % 3]
            xn_f, B_xn = xn_f2[par], B_xn2[par]
            eidx, gate, B_eg = eidx2[par], gate2[par], B_eg2[par]
            gnext = prep_gen(t + 1) if t + 1 < NT else None
            GS = 8
            slot_gb = {}
            for k in range(128 // GS + 1):
                if k == 1 and t >= 1:
                    finalize(t - 1)
                if k >= 1:
                    kk = k - 1
                    b_hc = B_hc[kk % NHC]
                    cs = slice(kk * GS, (kk + 1) * GS)
                    op("act", lambda e, cs=cs: e.activation(out=wcol[:, 0, cs], in_=hpre[:, cs], func=AF.Gelu), reads=[b_hc], writes=[b_hc])
                    for sl in range(kk * GS, (kk + 1) * GS):
                        gb, b_gb = slot_gb.pop(sl)
                        dg, b_dg = diag[sl % NDG], B_dg[sl % NDG]
                        op("act", lambda e, sl=sl, gate=gate: e.activation(out=wcol[:, 1, sl:sl + 1], in_=wcol[:, 0, sl:sl + 1], func=AF.Copy, scale=gate[:, sl:sl + 1]),
                           reads=[b_hc, B_eg], writes=[b_hc])
                        op("act", lambda e, sl=sl, dg=dg: e.activation(out=dg[:], in_=ident[:], func=AF.Copy, scale=wcol[:, 1, sl:sl + 1]),
                           reads=[b_hc, B_id], writes=[b_dg])
                        for hf in range(2):
                            op("pe", lambda e, sl=sl, dg=dg, gb=gb, hf=hf: e.matmul(ps_acc[hf][:, :], lhsT=dg[:], rhs=gb[:, D + hf * 512:D + (hf + 1) * 512],
                                                                                 start=(sl == 0), stop=(sl == 127)), reads=[b_dg, b_gb], writes=[B_pacc])
                if k < 128 // GS:
                    for sl in range(k * GS, (k + 1) * GS):
                        gi = gb_rr[0] % NGB
                        gb_rr[0] += 1
                        gb, b_gb = gbuf[gi], B_gb[gi]
                        slot_gb[sl] = (gb, b_gb)
                        b_hc = B_hc[k % NHC]
                        dma("pool", lambda e, gb=gb, sl=sl, eidx=eidx: e.indirect_dma_start(out=gb[:], out_offset=None, in_=uv_d[:, :],
                            in_offset=bass.IndirectOffsetOnAxis(ap=eidx[:, sl:sl + 1], axis=0)), reads=[B_eg, B_uv], writes=[b_gb])
                        op("dve", lambda e, gb=gb, sl=sl, xn_f=xn_f: e.scalar_tensor_tensor(out=junk_b[:], in0=gb[:, 0:D], scalar=1.0, in1=xn_f[:], op0=ALU.mult, op1=ALU.mult,
                                                                                            accum_out=hpre[:, sl:sl + 1]), reads=[b_gb, B_xn, b_hc], writes=[B_jf, b_hc])
                        run_gen(gnext, 1)
            run_gen(gnext)
        finalize(NT - 1)
        S.barrier()
        Bq.close()
    P0.close()
    S.final_wait("sp")
    S.emit()
    S.close()
    return nc


def prep_shared(inp):
    f = np.float32
    sh = {}
    sh["w_in"] = np.ascontiguousarray(inp["w_in"][0], f)
    sh["w_a"] = np.ascontiguousarray(inp["w_branch_a"][0], f)
    sh["w_b"] = np.ascontiguousarray(inp["w_branch_b"][0], f)
    sh["w_out"] = np.ascontiguousarray(inp["w_out"][0], f)
    sh["w_glu"] = np.ascontiguousarray(inp["w_glu"][0], f)
    sh["w_q"] = np.ascontiguousarray(inp["w_q"][0], f)
    sh["keysT"] = np.ascontiguousarray(np.stack([inp["sub_keys_1"][0].T, inp["sub_keys_2"][0].T]), f)
    sh["g_rep"] = np.ascontiguousarray(np.stack([np.broadcast_to(inp["g_mix"][0], (128, D)),
                                                 np.broadcast_to(inp["g_ffn"][0], (128, D)),
                                                 np.broadcast_to(inp["g_final"], (128, D))]), f)
    colp = np.zeros((128, 48), f)
    colp[:, 0:16] = inp["b_gate"][0].reshape(16, 128).T
    cw = inp["conv_w"][0].reshape(3, 6, 128)
    colp[:, 16:34] = cw.transpose(2, 1, 0).reshape(128, 18)
    colp[:, 34:40] = inp["conv_b"][0].reshape(6, 128).T
    colp[:, 40:42] = inp["b_glu"][0].reshape(2, 128).T
    colp[:, 42:44] = inp["ssm_d"][0].reshape(2, 128).T
    sh["colp"] = colp
    lr, li, ld = inp["lam_re"][0], inp["lam_im"][0], inp["log_dt"][0]
    pt = np.zeros((3, 128, G), f)
    pt[0] = np.concatenate([lr.T, lr.T], 0)
    pt[1] = np.concatenate([li.T, li.T], 0)
    pt[2] = np.broadcast_to(ld[None, :], (128, G))
    sh["ssm_pt"] = pt
    row = np.zeros((3, 128, G * P), f)
    row[0] = np.broadcast_to(lr.reshape(1, -1), (128, G * P))
    row[1] = np.broadcast_to(li.reshape(1, -1), (128, G * P))
    row[2] = np.broadcast_to(np.repeat(ld, P).reshape(1, -1), (128, G * P))
    sh["ssm_row"] = row
    btm = np.zeros((2, 128, G, P), f)
    ctm = np.zeros((128, G, 128), f)
    for g in range(G):
        r0 = 16 * (g % 8)
        btm[0, r0:r0 + 16, g, :] = inp["ssm_b_re"][0][g].T
        btm[1, r0:r0 + 16, g, :] = inp["ssm_b_im"][0][g].T
        ctm[0:64, g, r0:r0 + 16] = inp["ssm_c_re"][0][g].T
        ctm[64:128, g, r0:r0 + 16] = inp["ssm_c_im"][0][g].T
    sh["bt_in"] = btm.reshape(2, 128, G * P)
    sh["ct_in"] = ctm.reshape(128, G * 128)
    sh["exp_u"] = np.ascontiguousarray(inp["expert_u"][0], f)
    sh["exp_v"] = np.ascontiguousarray(inp["expert_v"][0], f)
    return sh


def make_in_maps(inp, S_half, n_cores):
    sh = prep_shared(inp)
    x = np.asarray(inp["x"], np.float32)
    maps = []
    for c in range(n_cores):
        b, hf = c // 2, c % 2
        m = dict(sh)
        m["x_main"] = np.ascontiguousarray(x[b, hf * S_half:(hf + 1) * S_half])
        m["x_prev"] = np.ascontiguousarray(x[b, 0:S_half]) if hf == 1 else np.zeros((S_half, D), np.float32)
        maps.append(m)
    return maps


def kernel(**inputs):
    x = np.asarray(inputs["x"])
    bsz, seq, _ = x.shape
    S_half = seq // 2
    n_cores = 2 * bsz
    nc = build_nc(S_half)
    maps = make_in_maps(inputs, S_half, n_cores)
    res = run_bass_kernel_spmd(nc, maps, core_ids=list(range(n_cores)))
    out = np.zeros((bsz, seq, D), np.float32)
    for c in range(n_cores):
        b, hf = c // 2, c % 2
        out[b, hf * S_half:(hf + 1) * S_half] = res.results[c]["out"]
    return out
```
